# Optimizing a Trainium2 kernel written in Bass

```python
import jax, jax.numpy as jnp
from jax import lax
import numpy as np

D_MODEL = 1024
BATCH = 8
SEQ = 2048
DEPTH = 4

GRID_W = 64
CTX_LEN = 256

A_WIDTH = 512
A_GROUPS = 4
CHUNK = 128
MLA_HEADS = 8
QK_NOPE = 64
QK_ROPE = 32
V_DIM = 64
Q_RANK = 256
KV_RANK = 128
ROPE_BASE = 10000.0
Q_BLOCK = 128
C_WIDTH = 512
C_BLOCKS = 8
C_BLOCK_W = C_WIDTH // C_BLOCKS
CONV_W = 4
CONV_LEFT = 2
LRU_C = 8.0
N_BRANCH = 3
BRANCH_W = 512
OFF_AU = 0
OFF_AV = OFF_AU + A_WIDTH
OFF_CQ = OFF_AV + A_WIDTH
OFF_CKV = OFF_CQ + Q_RANK
OFF_KR = OFF_CKV + KV_RANK
OFF_CX = OFF_KR + QK_ROPE
OFF_CG = OFF_CX + C_WIDTH
OFF_GATE = OFF_CG + C_WIDTH
N_IN = OFF_GATE + N_BRANCH * D_MODEL
N_EXPERTS = 16
N_GROUPS = 4
EXPERTS_PER_GROUP = N_EXPERTS // N_GROUPS
TOP_K = 2
D_EXPERT = 512
ROUTED_SCALE = 2.5
EPS = 1e-6

kernel_name = 'hybrid_gated_mixer_moe_trunk'


def layer_norm(x, g, b):
    x32 = x.astype(jnp.float32)
    mu = jnp.mean(x32, axis=-1, keepdims=True)
    var = jnp.mean(jnp.square(x32 - mu), axis=-1, keepdims=True)
    return ((x32 - mu) * lax.rsqrt(var + EPS)).astype(x.dtype) * g + b


def rms_norm(x, g):
    x32 = x.astype(jnp.float32)
    ms = jnp.mean(jnp.square(x32), axis=-1, keepdims=True)
    return (x32 * lax.rsqrt(ms + EPS)).astype(x.dtype) * g


def axial_rope(rows):
    row = jnp.repeat(jnp.arange(rows, dtype=jnp.float32), GRID_W)
    col = jnp.tile(jnp.arange(GRID_W, dtype=jnp.float32), rows)
    n_freq = QK_ROPE // 4
    inv = ROPE_BASE ** (-jnp.arange(n_freq, dtype=jnp.float32) / n_freq)
    ang = jnp.stack([row[:, None] * inv, col[:, None] * inv], axis=1)
    return jnp.cos(ang), jnp.sin(ang)


def apply_rope(x, cos, sin):
    shp = x.shape
    xr = x.reshape(shp[:-1] + (2, 2, QK_ROPE // 4))
    x1, x2 = xr[..., 0, :], xr[..., 1, :]
    c = cos[None, :, None]
    s = sin[None, :, None]
    out = jnp.stack([x1 * c - x2 * s, x2 * c + x1 * s], axis=-2)
    return out.reshape(shp).astype(x.dtype)


def chunk_gmlp(u_raw, v_raw, p):
    bsz, n, _ = v_raw.shape
    u = jax.nn.gelu(u_raw)
    v = layer_norm(jax.nn.gelu(v_raw), p['a_ln_g'], p['a_ln_b'])
    vc = v.reshape(bsz, n // CHUNK, CHUNK, A_GROUPS, A_WIDTH // A_GROUPS)
    mixed = jnp.einsum('gpq,bcqgd->bcpgd', p['w_s'], vc) + p['b_s'].T[None, None, :, :, None]
    return u * mixed.reshape(bsz, n, A_WIDTH)


def mla_queries(cq, p, rope):
    q = rms_norm(cq, p['q_norm_g']) @ p['w_uq']
    q = q.reshape(q.shape[:-1] + (MLA_HEADS, QK_NOPE + QK_ROPE))
    q_nope, q_rope = q[..., :QK_NOPE], q[..., QK_NOPE:]
    if rope is not None:
        q_rope = apply_rope(q_rope, *rope)
    return jnp.concatenate([q_nope, q_rope], axis=-1)


def mla_keys_values(ckv, kr, p, rope):
    kv = rms_norm(ckv, p['kv_norm_g']) @ p['w_ukv']
    kv = kv.reshape(kv.shape[:-1] + (MLA_HEADS, QK_NOPE + V_DIM))
    k_nope, v = kv[..., :QK_NOPE], kv[..., QK_NOPE:]
    k_rope = kr[..., None, :]
    if rope is not None:
        k_rope = apply_rope(k_rope, *rope)
    k_rope = jnp.broadcast_to(k_rope, k_nope.shape[:-1] + (QK_ROPE,))
    return jnp.concatenate([k_nope, k_rope], axis=-1), v


def block_attention(q, k, v):
    bsz, n, h, dq = q.shape
    nb = n // Q_BLOCK
    scale = dq ** -0.5
    qb = q.reshape(bsz, nb, Q_BLOCK, h, dq).transpose(1, 0, 2, 3, 4)

    def one(qblk):
        s = jnp.einsum('bqhd,bkhd->bhqk', qblk, k).astype(jnp.float32) * scale
        pr = jax.nn.softmax(s, axis=-1).astype(v.dtype)
        return jnp.einsum('bhqk,bkhd->bqhd', pr, v)

    out = lax.map(one, qb)
    return out.transpose(1, 0, 2, 3, 4).reshape(bsz, n, h * v.shape[-1])


def depthwise_conv(x, w, b):
    n = x.shape[1]
    xp = jnp.pad(x, ((0, 0), (CONV_LEFT, CONV_W - 1 - CONV_LEFT), (0, 0)))
    out = b
    for k in range(CONV_W):
        out = out + xp[:, k:k + n] * w[k]
    return out


def block_diag(x, w, b):
    xb = x.reshape(x.shape[:-1] + (C_BLOCKS, C_BLOCK_W))
    return jnp.einsum('bnhi,hij->bnhj', xb, w).reshape(x.shape) + b


def rglru_coeffs(x, w_r, b_r, w_i, b_i, lam):
    r = jax.nn.sigmoid(block_diag(x, w_r, b_r))
    i = jax.nn.sigmoid(block_diag(x, w_i, b_i))
    log_a = LRU_C * r * jax.nn.log_sigmoid(lam)
    a = jnp.exp(log_a)
    return a, jnp.sqrt(-jnp.expm1(2.0 * log_a)) * (i * x)


def linear_scan(a, bx, h0, reverse):
    first = -1 if reverse else 0
    bx = bx.at[:, first].add(a[:, first] * h0)

    def comb(l, r):
        return (l[0] * r[0], r[0] * l[1] + r[1])

    _, h = lax.associative_scan(comb, (a, bx), axis=1, reverse=reverse)
    return h


def merge(y_a, y_b, y_c, gate_raw, p):
    g = jax.nn.sigmoid(gate_raw.astype(jnp.float32)).astype(y_a.dtype)
    g = g.reshape(gate_raw.shape[:-1] + (N_BRANCH, D_MODEL))
    ys = jnp.stack([y_a, y_b, y_c], axis=-2)
    br = jnp.einsum('bnkc,kcd->bnkd', ys, p['w_br'])
    return jnp.sum(g * br, axis=-2) @ p['w_o']


def token_mixer(h_lat, h_ctx, p, rope, ctx_out):
    pl = h_lat @ p['w_in']
    if ctx_out:
        pc, base = h_ctx @ p['w_in'], 0
    else:
        pc, base = h_ctx @ p['w_in'][:, OFF_CKV:OFF_CG], OFF_CKV

    def lat(off, width):
        return pl[..., off:off + width]

    def ctxc(off, width):
        return pc[..., off - base:off - base + width]

    y_a_lat = chunk_gmlp(lat(OFF_AU, A_WIDTH), lat(OFF_AV, A_WIDTH), p)
    k_ctx, v_ctx = mla_keys_values(ctxc(OFF_CKV, KV_RANK), ctxc(OFF_KR, QK_ROPE), p, None)
    k_lat, v_lat = mla_keys_values(lat(OFF_CKV, KV_RANK), lat(OFF_KR, QK_ROPE), p, rope)
    q_lat = mla_queries(lat(OFF_CQ, Q_RANK), p, rope)
    y_b_lat = block_attention(q_lat, jnp.concatenate([k_ctx, k_lat], axis=1),
                              jnp.concatenate([v_ctx, v_lat], axis=1))
    xc_ctx = depthwise_conv(ctxc(OFF_CX, C_WIDTH).astype(jnp.float32), p['conv_w'], p['conv_b'])
    xc_lat = depthwise_conv(lat(OFF_CX, C_WIDTH).astype(jnp.float32), p['conv_w'], p['conv_b'])
    rec_lat = 0.0
    rec_ctx = 0.0
    for d, reverse in enumerate((False, True)):
        coeff = (p['w_r'][d], p['b_r'][d], p['w_i'][d], p['b_i'][d], p['lru_lambda'][d])
        a, bx = rglru_coeffs(xc_ctx, *coeff)
        hc = linear_scan(a, bx, jnp.zeros_like(xc_ctx[:, 0]), reverse)
        h_last = hc[:, 0] if reverse else hc[:, -1]
        a, bx = rglru_coeffs(xc_lat, *coeff)
        rec_lat = rec_lat + linear_scan(a, bx, h_last, reverse)
        if ctx_out:
            rec_ctx = rec_ctx + hc
    y_c_lat = jax.nn.gelu(lat(OFF_CG, C_WIDTH)) * rec_lat.astype(h_lat.dtype)
    y_lat = merge(y_a_lat, y_b_lat, y_c_lat, lat(OFF_GATE, N_BRANCH * D_MODEL), p)
    if not ctx_out:
        return y_lat, None
    y_a_ctx = chunk_gmlp(ctxc(OFF_AU, A_WIDTH), ctxc(OFF_AV, A_WIDTH), p)
    y_b_ctx = block_attention(mla_queries(ctxc(OFF_CQ, Q_RANK), p, None), k_ctx, v_ctx)
    y_c_ctx = jax.nn.gelu(ctxc(OFF_CG, C_WIDTH)) * rec_ctx.astype(h_ctx.dtype)
    y_ctx = merge(y_a_ctx, y_b_ctx, y_c_ctx, ctxc(OFF_GATE, N_BRANCH * D_MODEL), p)
    return y_lat, y_ctx


def moe(h, w_router, router_bias, w_gate, w_up, w_down):
    n_tok = h.shape[0]
    scores = jax.nn.sigmoid((h @ w_router).astype(jnp.float32))
    sel = (scores + router_bias.astype(jnp.float32)).reshape(n_tok, N_GROUPS, EXPERTS_PER_GROUP)
    group_score = jnp.sum(lax.top_k(sel, 2)[0], axis=-1)
    best = jnp.argmax(group_score, axis=-1)
    in_group = jnp.take_along_axis(sel, best[:, None, None], axis=1)[:, 0]
    _, local = lax.top_k(in_group, TOP_K)
    expert_idx = best[:, None] * EXPERTS_PER_GROUP + local
    w = jnp.take_along_axis(scores, expert_idx, axis=-1)
    w = ROUTED_SCALE * w / jnp.sum(w, axis=-1, keepdims=True)
    combine = jnp.sum(jax.nn.one_hot(expert_idx, N_EXPERTS, dtype=jnp.float32) * w[..., None], axis=1)
    combine = combine.astype(h.dtype)

    def expert(acc, ew):
        wg, wu, wd, cw = ew
        y = (jax.nn.silu(h @ wg) * (h @ wu)) @ wd
        return acc + cw[:, None] * y, None

    out, _ = lax.scan(expert, jnp.zeros_like(h), (w_gate, w_up, w_down, combine.T))
    return out


def setup_inputs(seed: int = 0) -> dict:
    key = jax.random.key(seed)
    ks = iter(jax.random.split(key, 40))
    f32 = jnp.float32
    beta = (8.0 * DEPTH) ** -0.25

    def nrm(shape, scale):
        return jax.random.normal(next(ks), shape, f32) * scale

    def gain(shape):
        return 1.0 + nrm(shape, 0.02)

    a0 = jax.random.uniform(next(ks), (DEPTH, 2, C_WIDTH), f32, 0.9, 0.999)
    s0 = a0 ** (1.0 / LRU_C)
    lru_lambda = jnp.log(s0) - jnp.log1p(-s0)
    return {
        'x': nrm((BATCH, SEQ, D_MODEL), 1.0),
        'c': nrm((BATCH, D_MODEL), 1.0),
        'ctx': nrm((BATCH, CTX_LEN, D_MODEL), 1.0),
        'c_ctx': nrm((D_MODEL,), 1.0),
        'w_ada': nrm((DEPTH, D_MODEL, 6 * D_MODEL), 0.5 * D_MODEL ** -0.5),
        'b_ada': nrm((DEPTH, 6 * D_MODEL), 0.02),
        'w_in': nrm((DEPTH, D_MODEL, N_IN), D_MODEL ** -0.5),
        'a_ln_g': gain((DEPTH, A_WIDTH)),
        'a_ln_b': nrm((DEPTH, A_WIDTH), 0.02),
        'w_s': nrm((DEPTH, A_GROUPS, CHUNK, CHUNK), CHUNK ** -0.5),
        'b_s': gain((DEPTH, A_GROUPS, CHUNK)),
        'q_norm_g': gain((DEPTH, Q_RANK)),
        'w_uq': nrm((DEPTH, Q_RANK, MLA_HEADS * (QK_NOPE + QK_ROPE)), Q_RANK ** -0.5),
        'kv_norm_g': gain((DEPTH, KV_RANK)),
        'w_ukv': nrm((DEPTH, KV_RANK, MLA_HEADS * (QK_NOPE + V_DIM)), KV_RANK ** -0.5),
        'conv_w': nrm((DEPTH, CONV_W, C_WIDTH), CONV_W ** -0.5),
        'conv_b': nrm((DEPTH, C_WIDTH), 0.02),
        'w_r': nrm((DEPTH, 2, C_BLOCKS, C_BLOCK_W, C_BLOCK_W), C_BLOCK_W ** -0.5),
        'b_r': nrm((DEPTH, 2, C_WIDTH), 0.02),
        'w_i': nrm((DEPTH, 2, C_BLOCKS, C_BLOCK_W, C_BLOCK_W), C_BLOCK_W ** -0.5),
        'b_i': nrm((DEPTH, 2, C_WIDTH), 0.02),
        'lru_lambda': lru_lambda,
        'w_br': nrm((DEPTH, N_BRANCH, BRANCH_W, D_MODEL), BRANCH_W ** -0.5),
        'w_o': nrm((DEPTH, D_MODEL, D_MODEL), beta * D_MODEL ** -0.5),
        'ln1_g': gain((DEPTH, D_MODEL)),
        'ln1_b': nrm((DEPTH, D_MODEL), 0.02),
        'w_router': nrm((D_MODEL, N_EXPERTS), D_MODEL ** -0.5),
        'router_bias': nrm((N_EXPERTS,), 0.01),
        'w_gate': nrm((DEPTH, N_EXPERTS, D_MODEL, D_EXPERT), D_MODEL ** -0.5),
        'w_up': nrm((DEPTH, N_EXPERTS, D_MODEL, D_EXPERT), D_MODEL ** -0.5),
        'w_down': nrm((DEPTH, N_EXPERTS, D_EXPERT, D_MODEL), beta * D_EXPERT ** -0.5),
        'ln2_g': gain((DEPTH, D_MODEL)),
        'ln2_b': nrm((DEPTH, D_MODEL), 0.02),
    }


def reference(x, c, ctx, c_ctx, w_ada, b_ada, w_in, a_ln_g, a_ln_b, w_s, b_s, q_norm_g, w_uq,
              kv_norm_g, w_ukv, conv_w, conv_b, w_r, b_r, w_i, b_i, lru_lambda, w_br, w_o,
              ln1_g, ln1_b, w_router, router_bias, w_gate, w_up, w_down, ln2_g, ln2_b):
    alpha = (2.0 * DEPTH) ** 0.25
    bsz, n_lat, d = x.shape
    rows = n_lat // GRID_W
    rope = axial_rope(rows)
    cond_lat = jax.nn.silu(c)
    cond_ctx = jax.nn.silu(c_ctx)
    x_lat, x_ctx = x, ctx
    for l in range(DEPTH):
        ctx_out = l < DEPTH - 1
        p = {
            'w_in': w_in[l], 'a_ln_g': a_ln_g[l], 'a_ln_b': a_ln_b[l], 'w_s': w_s[l], 'b_s': b_s[l],
            'q_norm_g': q_norm_g[l], 'w_uq': w_uq[l], 'kv_norm_g': kv_norm_g[l], 'w_ukv': w_ukv[l],
            'conv_w': conv_w[l], 'conv_b': conv_b[l], 'w_r': w_r[l], 'b_r': b_r[l], 'w_i': w_i[l],
            'b_i': b_i[l], 'lru_lambda': lru_lambda[l], 'w_br': w_br[l], 'w_o': w_o[l],
        }
        mod_lat = (cond_lat @ w_ada[l] + b_ada[l])[:, None, :]
        mod_ctx = (cond_ctx @ w_ada[l] + b_ada[l])[None, None, :]
        sh1, sc1, g1, sh2, sc2, g2 = jnp.split(mod_lat, 6, axis=-1)
        csh1, csc1, cg1, csh2, csc2, cg2 = jnp.split(mod_ctx, 6, axis=-1)
        h_lat = x_lat * (1.0 + sc1) + sh1
        h_ctx = x_ctx * (1.0 + csc1) + csh1
        y_lat, y_ctx = token_mixer(h_lat, h_ctx, p, rope, ctx_out)
        x_lat = layer_norm(alpha * x_lat + g1 * y_lat, ln1_g[l], ln1_b[l])
        f_in_lat = (x_lat * (1.0 + sc2) + sh2).reshape(-1, d)
        if ctx_out:
            x_ctx = layer_norm(alpha * x_ctx + cg1 * y_ctx, ln1_g[l], ln1_b[l])
            f_in_ctx = (x_ctx * (1.0 + csc2) + csh2).reshape(-1, d)
            f_out = moe(jnp.concatenate([f_in_lat, f_in_ctx], axis=0), w_router, router_bias,
                        w_gate[l], w_up[l], w_down[l])
            f_lat = f_out[:f_in_lat.shape[0]].reshape(x_lat.shape)
            f_ctx = f_out[f_in_lat.shape[0]:].reshape(x_ctx.shape)
            x_ctx = layer_norm(alpha * x_ctx + cg2 * f_ctx, ln2_g[l], ln2_b[l])
        else:
            f_lat = moe(f_in_lat, w_router, router_bias, w_gate[l], w_up[l], w_down[l]).reshape(x_lat.shape)
        x_lat = layer_norm(alpha * x_lat + g2 * f_lat, ln2_g[l], ln2_b[l])
    return x_lat
```

```python
import numpy as np
from contextlib import ExitStack
import concourse.bass as bass
import concourse.mybir as mybir
from concourse.bass_utils import run_bass_kernel_spmd

F32 = mybir.dt.float32
BF16 = mybir.dt.bfloat16
AF = mybir.ActivationFunctionType
ALU = mybir.AluOpType
AX = mybir.AxisListType

D = 1024
DEPTH = 4
NCTX = 256
NLAT = 2048
T = NCTX + NLAT
NT = T // 128
ALPHA = (2.0 * DEPTH) ** 0.25
EPS = 1e-6
NEXP = 16
KB = 1024
CHUNKS = [(0, 256), (256, 512), (768, 512), (1280, 512), (1792, 512)]
ATT_SCALE = 96 ** -0.5
BIG = 1.0e4

OFF_CONST = 0
OFF_GB = 12 * KB
OFF_HT = 20 * KB
OFF_RING = 56 * KB
NSLOT = 6
SLOT = 8 * KB
OFF_X = 104 * KB
OFF_PH = 176 * KB
ARENA = 204 * KB


class Buf:
    __slots__ = ("name", "w", "r", "x")

    def __init__(self, name=""):
        self.name = name
        self.w = None
        self.r = {}
        self.x = False


class Em:
    SEM_LIMIT = 30000
    NDMA = 8

    def __init__(self, nc, stack):
        self.nc = nc
        self.stack = stack
        self.eng = {"pe": nc.tensor, "act": nc.scalar, "dve": nc.vector,
                    "pool": nc.gpsimd, "sp": nc.sync}
        self.cur = {}
        self.nsem = 0
        self.waited = {e: {} for e in self.eng}
        self.last = {}
        for e in self.eng:
            self._new_sem(e)
        self.dq = {}
        self.dqi = {}
        self.dlast = {}
        for q in ("sp", "pool"):
            self.dq[q] = [self._alloc("d%s%d" % (q, i)) for i in range(self.NDMA)]
            self.dqi[q] = 0
        self.bufs = {}
        self.ninst = {e: 0 for e in self.eng}
        self.nwait = 0

    def _alloc(self, name):
        self.nsem += 1
        key = "%s_%d" % (name, self.nsem)
        sem = self.stack.enter_context(self.nc.semaphore(key))
        return [key, sem, 0]

    def _new_sem(self, e):
        self.cur[e] = self._alloc("s" + e)

    def B(self, *key):
        b = self.bufs.get(key)
        if b is None:
            b = Buf(str(key))
            self.bufs[key] = b
        return b

    def _deps(self, reads, writes):
        deps = {}

        def add(t):
            if t is None:
                return
            k = t[0]
            if k not in deps or deps[k][2] < t[2]:
                deps[k] = t
        for b in reads:
            add(b.w)
            if b.x:
                for t in b.r.values():
                    add(t)
        for b in writes:
            add(b.w)
            for t in b.r.values():
                add(t)
        return deps

    def _wait(self, e, deps, skip_self=False):
        w = self.waited[e]
        for k, (_, sem, v) in deps.items():
            if w.get(k, 0) >= v:
                continue
            if skip_self and k == self.cur[e][0]:
                continue
            self.eng[e].wait_ge(sem, v)
            self.nwait += 1
            w[k] = v

    def _commit(self, ticket, reads, writes):
        for b in writes:
            b.w = ticket
            b.r = {}
        for b in reads:
            b.r[ticket[0]] = ticket

    def op(self, e, fn, reads=(), writes=()):
        deps = self._deps(reads, writes)
        self._wait(e, deps, skip_self=(e == "pe"))
        ins = fn()
        c = self.cur[e]
        c[2] += 1
        ins.then_inc(c[1], 1)
        ticket = (c[0], c[1], c[2])
        self.last[e] = ticket
        self._commit(ticket, reads, writes)
        self.ninst[e] += 1
        if c[2] >= self.SEM_LIMIT:
            self._new_sem(e)
        return ticket

    def dma(self, q, out, in_, reads=(), writes=()):
        deps = self._deps(reads, writes)
        i = self.dqi[q]
        self.dqi[q] = i + 1
        si = i % self.NDMA
        slot = self.dq[q][si]
        if slot[2] > 0:
            t = (slot[0], slot[1], slot[2])
            if slot[0] not in deps or deps[slot[0]][2] < slot[2]:
                deps[slot[0]] = t
        self._wait(q, deps)
        ins = self.eng[q].dma_start(out=out, in_=in_)
        slot[2] += 16
        ins.then_inc(slot[1], 16)
        ticket = (slot[0], slot[1], slot[2])
        self.dlast[(q, si)] = ticket
        self._commit(ticket, reads, writes)
        if slot[2] >= self.SEM_LIMIT:
            self.dq[q][si] = self._alloc("d%s" % q)
        return ticket

    def barrier(self):
        tickets = list(self.last.values()) + list(self.dlast.values())
        for e in self.eng:
            deps = {}
            for t in tickets:
                if t[0] not in deps or deps[t[0]][2] < t[2]:
                    deps[t[0]] = t
            self._wait(e, deps)

    def finish(self, e="sp"):
        tickets = list(self.last.values()) + list(self.dlast.values())
        deps = {}
        for t in tickets:
            if t[0] not in deps or deps[t[0]][2] < t[2]:
                deps[t[0]] = t
        self._wait(e, deps)


class Prog:
    def __init__(self, nl=DEPTH, dbg=()):
        self.nl = nl
        self.dbg = set(dbg)
        self.nc = bass.Bass("TRN2", target_bir_lowering=False)
        self.din = {}
        self.dout = {}

    def inp(self, name, shape):
        self.din[name] = self.nc.dram_tensor(name, list(shape), F32, kind="ExternalInput").ap()
        return self.din[name]

    def outp(self, name, shape, dt=F32):
        self.dout[name] = self.nc.dram_tensor(name, list(shape), dt, kind="ExternalOutput").ap()
        return self.dout[name]

    def v(self, off, shape, dt=F32):
        n = 1
        for s in shape:
            n *= s
        esz = 4 if dt == F32 else 2
        nb = n * esz
        assert off % 4 == 0 and nb % 4 == 0, (off, shape)
        assert off + nb <= ARENA, (off, shape)
        ap = self.AR[:, off // 4:(off + nb) // 4]
        if dt != F32:
            ap = ap.bitcast(dt)
        if len(shape) == 2:
            ap = ap.rearrange("p (a b) -> p a b", b=shape[1])
        elif len(shape) == 3:
            ap = ap.rearrange("p (a b c) -> p a b c", b=shape[1], c=shape[2])
        return ap

    def ring_take(self):
        s = self.ring_i % NSLOT
        self.ring_i += 1
        return OFF_RING + s * SLOT, self.em.B("ring", s)

    def ring_load(self, src, shape):
        off, b = self.ring_take()
        view = self.v(off, shape, BF16)
        self.em.dma("pool", view, src, writes=[b])
        return view, b

    def psb(self, i):
        return self.PS[i], self.em.B("ps", i)

    def mm(self, out, lhsT, rhs, start, stop, reads, writes):
        nc = self.nc
        self.em.op("pe", lambda: nc.tensor.matmul(out=out, lhsT=lhsT, rhs=rhs, start=start, stop=stop),
                   reads, writes)

    def act(self, out, in_, func, reads, writes, bias=None, scale=None):
        nc = self.nc
        kw = {}
        if bias is not None:
            kw["bias"] = bias
        if scale is not None:
            kw["scale"] = scale
        self.em.op("act", lambda: nc.scalar.activation(out=out, in_=in_, func=func, **kw), reads, writes)

    def tt(self, e, out, in0, in1, op, reads, writes):
        eng = self.em.eng[e]
        self.em.op(e, lambda: eng.tensor_tensor(out=out, in0=in0, in1=in1, op=op), reads, writes)

    def ts(self, e, out, in0, s1, s2, op0, op1, reads, writes):
        eng = self.em.eng[e]
        self.em.op(e, lambda: eng.tensor_scalar(out=out, in0=in0, scalar1=s1, scalar2=s2, op0=op0, op1=op1),
                   reads, writes)

    def stt(self, out, in0, scalar, in1, op0, op1, reads, writes):
        nc = self.nc
        self.em.op("dve", lambda: nc.vector.scalar_tensor_tensor(out=out, in0=in0, scalar=scalar, in1=in1,
                                                                  op0=op0, op1=op1), reads, writes)

    def cp(self, e, out, in_, reads, writes):
        if e == "act":
            nc = self.nc
            self.em.op("act", lambda: nc.scalar.copy(out=out, in_=in_), reads, writes)
        else:
            eng = self.em.eng[e]
            self.em.op(e, lambda: eng.tensor_copy(out=out, in_=in_), reads, writes)

    def memset(self, e, ap, val, writes):
        eng = self.em.eng[e]
        self.em.op(e, lambda: eng.memset(ap, val), (), writes)

    def dump(self, name, ap_sb, shape, reads, dt=F32):
        if name not in self.dbg:
            return
        o = self.outp("dbg_" + name, shape, dt)
        self.em.dma("sp", o, ap_sb, reads=reads)

    def build(self):
        nc = self.nc
        L = self.nl
        inp = self.inp
        xin = inp("xin", [T, D])
        condT_d = inp("condT", [128, 16])
        ident_d = inp("ident", [128, 128])
        cossin_d = inp("cossin", [2, 128, T])
        rbias_d = inp("rbias", [128, NT * 16])
        wrouter_d = inp("wrouter", [128, 8, 16])
        w_ada_d = inp("w_ada_h", [DEPTH, 12, 128, 8, 512])
        b_ada_d = inp("b_ada_col", [DEPTH, 128, 48])
        wA_d = inp("wA", [DEPTH, 2, 128, 8, 512])
        wB_d = inp("wB", [DEPTH, 128, 8, 416])
        wC_d = inp("wC", [DEPTH, 2, 128, 8, 512])
        wG_d = inp("wG", [DEPTH, 6, 128, 8, 512])
        aln_d = inp("aln", [DEPTH, 2, 128, 512])
        wsT_d = inp("wsT", [DEPTH, 128, 4, 128])
        bs_d = inp("bs_row", [DEPTH, 1, 512])
        ncol_d = inp("ncol", [DEPTH, 128, 48])
        wuq_d = inp("wuq", [DEPTH, 128, 2, 768])
        wukv_d = inp("wukv", [DEPTH, 128, 1024])
        wri_d = inp("wri", [DEPTH, 128, 16, 128])
        wbr_d = inp("wbr", [DEPTH, 3, 128, 4, 1024])
        wo_d = inp("wo", [DEPTH, 2, 128, 8, 512])
        lnp_d = inp("lnp", [DEPTH, 4, 128, D])
        wg_d = inp("wg", [DEPTH, NEXP, 128, 8, 512])
        wu_d = inp("wu", [DEPTH, NEXP, 128, 8, 512])
        wd_d = inp("wd", [DEPTH, NEXP, 128, 4, 1024])
        out_d = self.outp("out", [NLAT, D])
        XD = nc.dram_tensor("xscr", [NT, 128, D], F32).ap()

        with ExitStack() as st:
            self.em = em = Em(nc, st)
            self.AR = st.enter_context(nc.sbuf_tensor("arena", [128, ARENA // 4], F32))
            self.PS = [st.enter_context(nc.psum_tensor("ps%d" % i, [128, 512], F32)) for i in range(8)]
            self.ring_i = 0
            B = em.B
            v = self.v
            PS = self.PS
            PB = [B("ps", i) for i in range(8)]
            for b_ in PB:
                b_.x = True

            ident = v(OFF_CONST + 0, [128])
            ident_bf = v(OFF_CONST + 512, [128], BF16)
            ones_bf = v(OFF_CONST + 768, [128], BF16)
            ones_f = v(OFF_CONST + 1024, [128])
            CM = [v(OFF_CONST + 1536, [48, 2]), v(OFF_CONST + 10752, [48, 2])]
            condT = v(OFF_CONST + 1920, [16])
            conds_bf = v(OFF_CONST + 1984, [8, 2], BF16)
            ncol = v(OFF_CONST + 2048, [48])
            ccol = v(OFF_CONST + 2240, [8])
            etmp = v(OFF_CONST + 2272, [8])
            wrouter = v(OFF_CONST + 2560, [8, 16])
            rbias = v(OFF_CONST + 3072, [NT * 16])
            bs_row = v(OFF_CONST + 4224, [512], BF16)
            alnG = v(OFF_CONST + 5248, [512])
            alnB = v(OFF_CONST + 7296, [512])
            stat = v(OFF_CONST + 9344, [4, 16])
            CWt = v(OFF_CONST + 9600, [NT, 16])
            GBv = [v(OFF_GB, [D]), v(OFF_GB + 4 * KB, [D])]
            HT = v(OFF_HT, [8, T], BF16)
            Xs = v(OFF_X, [NT, D])
            bConst = B("const")
            bHT = [B("HT", i) for i in range(NT)]
            bX = [B("X", i) for i in range(NT)]
            bXD = [B("XD", i) for i in range(NT)]
            bYT = [B("YT", i) for i in range(NT)]
            bGB = B("GB")
            bCM = [B("colmods", 0), B("colmods", 1)]
            bNcol = B("ncol")

            def tiles_of(c0, n):
                return list(range(c0 // 128, (c0 + n) // 128))

            def which(i):
                return 1 if i < 2 else 0

            em.dma("sp", ident, ident_d, writes=[bConst])
            em.dma("sp", condT, condT_d, writes=[bConst])
            em.dma("sp", wrouter, wrouter_d, writes=[bConst])
            em.dma("sp", rbias, rbias_d, writes=[bConst])
            for i in range(NT):
                em.dma("sp", Xs[:, i, :], xin[i * 128:(i + 1) * 128, :], writes=[bX[i]])
            self.memset("dve", ones_f, 1.0, [bConst])
            self.memset("dve", ones_bf, 1.0, [bConst])
            self.cp("dve", ident_bf, ident, [bConst], [bConst])
            sil = v(OFF_PH + 25 * KB, [16])
            self.act(sil, condT, AF.Silu, [bConst], [B("sil")])
            for w in range(2):
                self.cp("dve", conds_bf[:, :, w], sil[:, w * 8:(w + 1) * 8], [B("sil")], [bConst])

            psrr = [0]

            def next_ps(lo, hi):
                i = lo + psrr[0] % (hi - lo)
                psrr[0] += 1
                return i

            def s1_steps(l2):
                cm = CM[l2 % 2]
                bcm = bCM[l2 % 2]
                badac = v(OFF_PH + 27 * KB, [48])
                em.dma("sp", badac, b_ada_d[l2], writes=[B("badac")])
                def s1_load(s_):
                    W_ = v(OFF_PH + (s_ % 2) * SLOT, [8, 512], BF16)
                    bW_ = B("s1ring", s_ % 2)
                    em.dma("pool", W_, w_ada_d[l2, s_], writes=[bW_])
                    return W_, bW_
                q0 = []
                if l2 == 0:
                    for s_ in range(5):
                        q0.append(self.ring_load(w_ada_d[0, s_], [8, 512]))
                for s_ in range(12):
                    if l2 == 0:
                        if s_ + 5 < 12:
                            q0.append(self.ring_load(w_ada_d[0, s_ + 5], [8, 512]))
                        W, bW = q0.pop(0)
                    else:
                        if s_ % 4 == 0:
                            nxt = s1_load(s_)
                            yield
                        W, bW = nxt
                        if (s_ + 1) % 4 != 0:
                            nxt = s1_load(s_ + 1)
                    pm = next_ps(4, 8)
                    for cc in range(4):
                        for kc in range(8):
                            self.mm(PS[pm][:, 2 * cc:2 * cc + 2], W[:, kc, cc * 128:(cc + 1) * 128],
                                    conds_bf[:, kc, :], kc == 0, kc == 7, [bW, bConst], [PB[pm]])
                    psv = PS[pm][:, 0:8].rearrange("p (a b) -> p a b", b=2)
                    for w in range(2):
                        self.tt("dve", cm[:, 4 * s_:4 * s_ + 4, w], psv[:, :, w], badac[:, 4 * s_:4 * s_ + 4], ALU.add,
                                [PB[pm], B("badac")], [bcm])
                    yield
                for m in (1, 4):
                    self.ts("dve", cm[:, m * 8:(m + 1) * 8, :], cm[:, m * 8:(m + 1) * 8, :], 1.0, None,
                            ALU.add, ALU.bypass, [bcm], [bcm])
                yield

            def gate_bcast(colmods, bCol, m):
                diag = v(OFF_PH + 26 * KB, [2, 128])
                for w in range(2):
                    for hf in range(2):
                        pi = next_ps(6, 8)
                        for q in range(4):
                            cc = hf * 4 + q
                            dd = diag[:, (cc + w) % 2, :]
                            bd = B("diag", (cc + w) % 2)
                            self.ts("dve", dd, ident, colmods[:, m * 8 + cc, w:w + 1], None, ALU.mult, ALU.bypass,
                                    [bConst, bCol], [bd])
                            self.mm(PS[pi][:, q * 128:(q + 1) * 128], ones_f, dd, True, True,
                                    [bConst, bd], [PB[pi]])
                        self.cp("act", GBv[w][:, hf * 512:(hf + 1) * 512], PS[pi][:, :], [PB[pi]], [bGB])

            def layer(l):
                ctx_out = l < DEPTH - 1
                last = (l == L - 1)
                act_chunks = CHUNKS if ctx_out else CHUNKS[1:]
                act_tiles = list(range(NT)) if ctx_out else list(range(2, NT))

                colmods = CM[l % 2]
                bCol = bCM[l % 2]
                em.dma("sp", ncol, ncol_d[l], writes=[bNcol])
                if l == 0:
                    for _ in s1_steps(0):
                        pass
                s1gen = s1_steps(l + 1) if l + 1 < L else None
                self.dump("colmods%d" % l, colmods, [128, 48, 2], [bCol])
                gate_bcast(colmods, bCol, 2)

                def build_T(dst_is_ft):
                    mS, mB = (4, 3) if dst_is_ft else (1, 0)
                    for i in (act_tiles if dst_is_ft else range(NT)):
                        w = which(i)
                        for hf in range(2):
                            pi = next_ps(4, 6) if not dst_is_ft else next_ps(4, 6)
                            for q in range(4):
                                kc = hf * 4 + q
                                self.em.op("pe", lambda: nc.tensor.transpose(
                                    out=PS[pi][:, q * 128:(q + 1) * 128],
                                    in_=Xs[:, i, kc * 128:(kc + 1) * 128], identity=ident),
                                    [bX[i], bConst], [PB[pi]])
                            for q in range(4):
                                kc = hf * 4 + q
                                sc = colmods[:, mS * 8 + kc, w:w + 1]
                                bi = colmods[:, mB * 8 + kc, w:w + 1]
                                src = PS[pi][:, q * 128:(q + 1) * 128]
                                if dst_is_ft:
                                    f32t = self.F32T[:, (i % 2) * 8 + kc, :]
                                    bf = B("f32t", i % 2, kc)
                                    self.act(f32t, src, AF.Identity, [PB[pi], bCol], [bf], bias=bi, scale=sc)
                                    self.cp("dve", HT[:, kc, i * 128:(i + 1) * 128], f32t, [bf], [bHT[i]])
                                else:
                                    dst = HT[:, kc, i * 128:(i + 1) * 128]
                                    if hf == 0:
                                        self.act(dst, src, AF.Identity, [PB[pi], bCol], [bHT[i]], bias=bi, scale=sc)
                                    else:
                                        self.ts("dve", dst, src, sc, bi, ALU.mult, ALU.add, [PB[pi], bCol], [bHT[i]])
                        if dst_is_ft:
                            for kc in range(8):
                                self.mm(PS[6][:, i * 16:(i + 1) * 16], self.F32T[:, (i % 2) * 8 + kc, :],
                                        wrouter[:, kc, :], kc == 0, kc == 7,
                                        [B("f32t", i % 2, kc), bConst], [PB[6]])
                        if dst_is_ft:
                            self.em.op("act", lambda: nc.scalar.mul(out=Xs[:, i, :], in_=Xs[:, i, :], mul=ALPHA),
                                       [bX[i]], [bX[i]])
                        else:
                            self.ts("pool", Xs[:, i, :], Xs[:, i, :], ALPHA, 0.0, ALU.mult, ALU.add, [bX[i]], [bX[i]])
                        if not dst_is_ft:
                            em.dma("sp", XD[i], Xs[:, i, :], reads=[bX[i]], writes=[bXD[i]])

                def load_a():
                    AU = self.ring_load(wA_d[l, 0], [8, 512])
                    AV = self.ring_load(wA_d[l, 1], [8, 512])
                    WS = self.ring_load(wsT_d[l], [4, 128])
                    em.dma("pool", bs_row[0:1, :], bs_d[l], writes=[B("bsrow")])
                    em.dma("sp", alnG, aln_d[l, 0], writes=[B("aln")])
                    em.dma("sp", alnB, aln_d[l, 1], writes=[B("aln")])
                    return AU, AV, WS

                hold0 = load_a()
                build_T(False)
                self.dump("HT%d" % l, HT, [128, 8, T], bHT, BF16)
                em.barrier()

                def load_merge(k):
                    G = [self.ring_load(wG_d[l, 2 * k + hf], [8, 512]) for hf in range(2)]
                    BR = self.ring_load(wbr_d[l, k], [4, D])
                    WO = [self.ring_load(wo_d[l, hf], [8, 512]) for hf in range(2)]
                    return G, BR, WO

                def merge(k, Wm, pre_barrier=None):
                    YT = self.YT
                    G, BR, WO = Wm
                    base = OFF_X + 18 * KB
                    Mb = [v(base + r * 8 * KB, [8, 512], BF16) for r in range(2)]
                    Xt = [v(base + 16 * KB + r * 4 * KB, [D]) for r in range(3)]
                    sgb = [v(base + 28 * KB + r * 2 * KB, [512]) for r in range(2)]
                    tmpb = [v(base + 32 * KB + r * 2 * KB, [512]) for r in range(2)]
                    cnt = 0
                    xcnt = 0
                    xl = [0]

                    def xload():
                        if xl[0] < len(act_tiles):
                            ii = act_tiles[xl[0]]
                            em.dma("sp", Xt[xl[0] % 3], XD[ii], reads=[bXD[ii]], writes=[B("xt", xl[0] % 3)])
                            xl[0] += 1
                    xload()
                    xload()
                    for ci, (c0, n) in enumerate(act_chunks):
                        tl = tiles_of(c0, n)
                        M = Mb[ci % 2]
                        bM = B("M", ci % 2)
                        rdh = [bHT[i] for i in tl]
                        rdy = [bYT[i] for i in tl]
                        for dc in range(8):
                            hf, q = dc // 4, dc % 4
                            pg = next_ps(0, 2)
                            pb = next_ps(2, 4)
                            for kc in range(8):
                                self.mm(PS[pg][:, 0:n], G[hf][0][:, kc, q * 128:(q + 1) * 128], HT[:, kc, c0:c0 + n],
                                        kc == 0, kc == 7, [G[hf][1]] + rdh, [PB[pg]])
                            for jj in range(4):
                                self.mm(PS[pb][:, 0:n], BR[0][:, jj, dc * 128:(dc + 1) * 128], YT[:, jj, c0:c0 + n],
                                        jj == 0, jj == 3, [BR[1]] + rdy, [PB[pb]])
                            sg = sgb[cnt % 2]
                            bsg = B("sg", cnt % 2)
                            cnt += 1
                            self.act(sg[:, 0:n], PS[pg][:, 0:n], AF.Sigmoid, [PB[pg]], [bsg])
                            self.tt("dve", M[:, dc, 0:n], sg[:, 0:n], PS[pb][:, 0:n], ALU.mult, [bsg, PB[pb]], [bM])
                        for t, i in enumerate(tl):
                            w = which(i)
                            xt = Xt[xcnt % 3]
                            bxt = B("xt", xcnt % 3)
                            xcnt += 1
                            for hf in range(2):
                                py = next_ps(4, 8)
                                for kc in range(8):
                                    self.mm(PS[py][:, :], M[:, kc, t * 128:(t + 1) * 128], WO[hf][0][:, kc, :],
                                            kc == 0, kc == 7, [bM, WO[hf][1]], [PB[py]])
                                tmp = tmpb[(cnt) % 2]
                                btmp = B("mtmp", cnt % 2)
                                cnt += 1
                                self.tt("dve", tmp, PS[py][:, :], GBv[w][:, hf * 512:(hf + 1) * 512], ALU.mult,
                                        [PB[py], bGB], [btmp])
                                self.tt("pool", xt[:, hf * 512:(hf + 1) * 512], xt[:, hf * 512:(hf + 1) * 512], tmp,
                                        ALU.add, [btmp, bxt], [bxt])
                            xload()
                            em.dma("sp", XD[i], xt, reads=[bxt], writes=[bXD[i]])
                        if s1gen is not None:
                            next(s1gen, None)
                    if s1gen is not None and k == 2:
                        for _ in s1gen:
                            pass
                    if pre_barrier is not None:
                        pre_barrier()
                    em.barrier()

                self.YT = v(OFF_X, [4, T], BF16)
                YT = self.YT
                IB = OFF_X + 18 * KB

                def mixer_a(Wa, pre_barrier=None):
                    AU, AV, WS = Wa
                    UTb = [v(IB + r * 4 * KB, [4, 512], BF16) for r in range(2)]
                    vgb = [v(IB + 8 * KB + r * 2 * KB, [512]) for r in range(2)]
                    vnb = [v(IB + 12 * KB + r * 2 * KB, [512]) for r in range(2)]
                    vbb = [v(IB + 16 * KB + r * KB, [512], BF16) for r in range(2)]
                    tcnt = 0
                    for ci, (c0, n) in enumerate(act_chunks):
                        tl = tiles_of(c0, n)
                        UT = UTb[ci % 2]
                        bUT = B("UT", ci % 2)
                        rdh = [bHT[i] for i in tl]
                        for g in range(4):
                            pi = next_ps(0, 2)
                            for kc in range(8):
                                self.mm(PS[pi][:, 0:n], AU[0][:, kc, g * 128:(g + 1) * 128], HT[:, kc, c0:c0 + n],
                                        kc == 0, kc == 7, [AU[1]] + rdh, [PB[pi]])
                            self.act(UT[:, g, 0:n], PS[pi][:, 0:n], AF.Gelu_apprx_tanh, [PB[pi]], [bUT])
                        for t, i in enumerate(tl):
                            r = tcnt % 2
                            tcnt += 1
                            pv = next_ps(2, 4)
                            for kc in range(8):
                                self.mm(PS[pv][:, :], HT[:, kc, i * 128:(i + 1) * 128], AV[0][:, kc, :],
                                        kc == 0, kc == 7, [AV[1], bHT[i]], [PB[pv]])
                            vg, vn, vb = vgb[r], vnb[r], vbb[r]
                            bvg, bvn, bvb, bst = B("vg", r), B("vn", r), B("vb", r), B("stA", r)
                            sa = stat[:, r, :]
                            self.act(vg, PS[pv][:, :], AF.Gelu_apprx_tanh, [PB[pv]], [bvg])
                            em.op("dve", lambda: nc.vector.bn_stats(out=sa[:, 0:6], in_=vg), [bvg], [bst])
                            em.op("dve", lambda: nc.vector.bn_aggr(out=sa[:, 6:8], in_=sa[:, 0:6]), [bst], [bst])
                            self.act(sa[:, 8:9], sa[:, 7:8], AF.Sqrt, [bst], [bst], bias=epsc, scale=1.0)
                            em.op("dve", lambda: nc.vector.reciprocal(out=sa[:, 9:10], in_=sa[:, 8:9]), [bst], [bst])
                            self.ts("dve", vn, vg, sa[:, 6:7], sa[:, 9:10], ALU.subtract, ALU.mult, [bvg, bst], [bvn])
                            self.tt("pool", vn, vn, alnG, ALU.mult, [bvn, B("aln")], [bvn])
                            self.tt("pool", vb, vn, alnB, ALU.add, [bvn, B("aln")], [bvb])
                            pm_ = next_ps(4, 8)
                            for g in range(4):
                                self.mm(PS[pm_][:, g * 128:(g + 1) * 128], vb[:, g * 128:(g + 1) * 128], WS[0][:, g, :],
                                        True, False, [bvb, WS[1]], [PB[pm_]])
                                self.mm(PS[pm_][:, g * 128:(g + 1) * 128], ones_bf[0:1, :], bs_row[0:1, g * 128:(g + 1) * 128],
                                        False, True, [bConst, B("bsrow")], [PB[pm_]])
                            self.tt("dve", YT[:, :, i * 128:(i + 1) * 128],
                                    PS[pm_][:, :].rearrange("p (a b) -> p a b", b=128),
                                    UT[:, :, t * 128:(t + 1) * 128], ALU.mult, [PB[pm_], bUT], [bYT[i]])
                    self.dump("ya%d" % l, YT, [128, 4, T], bYT, BF16)
                    if pre_barrier is not None:
                        pre_barrier()
                    em.barrier()

                def load_b():
                    WB = self.ring_load(wB_d[l], [8, 416])
                    offq, bWQ = self.ring_take()
                    WUQ = v(offq, [2, 768], BF16)
                    WUKV = v(offq + 6 * KB, [D], BF16)
                    em.dma("pool", WUQ, wuq_d[l], writes=[bWQ])
                    em.dma("pool", WUKV, wukv_d[l], writes=[bWQ])
                    return WB, offq, bWQ

                def mixer_b(Wb, pre_barrier=None):
                    WB, offq, bWQ = Wb
                    WUQ = v(offq, [2, 768], BF16)
                    WUQS = v(offq + 3 * KB, [2, 768], BF16)
                    WUKV = v(offq + 6 * KB, [D], BF16)
                    COS = v(IB, [T])
                    SIN = v(IB + 9 * KB, [T])
                    bCS = B("cossin")
                    em.dma("sp", COS, cossin_d[0], writes=[bCS])
                    em.dma("sp", SIN, cossin_d[1], writes=[bCS])
                    CQN = v(IB + 18 * KB, [2, T], BF16)
                    CKN = v(IB + 27 * KB, [T], BF16)
                    KROPE = v(IB + 31 * KB + 512, [T], BF16)
                    KRW = v(IB + 36 * KB, [8, 96], BF16)
                    KRS = v(IB + 37 * KB + 512, [8, 96], BF16)
                    cqf = v(IB + 39 * KB, [2, 512])
                    sq = v(IB + 43 * KB, [2, 512])
                    rstd = v(IB + 47 * KB, [512])
                    t1 = v(IB + 49 * KB, [512])
                    t2 = v(IB + 51 * KB, [512])
                    QTh = v(IB + 53 * KB, [T], BF16)
                    KTh = v(IB + 57 * KB + 512, [T], BF16)
                    VAb = [v(IB + 62 * KB + r * 2560, [NT, 65], BF16) for r in range(2)]
                    PTb = [v(IB + 67 * KB + r * KB, [512], BF16) for r in range(4)]
                    YBp = v(IB + 71 * KB, [NT, 128], BF16)
                    rden = v(IB + 76 * KB, [8])
                    bKR, bWQS = B("KRW"), B("WUQS")
                    self.memset("pool", KRW, 0.0, [bKR])
                    self.memset("pool", KRS, 0.0, [bKR])
                    self.memset("pool", WUQS, 0.0, [bWQS])
                    self.cp("pool", KRW[:, :, 64:96], WB[0][:, :, 384:416], [WB[1], bKR], [bKR])
                    for a in range(2):
                        o = 64 + a * 16
                        s_ = 384 + a * 16
                        self.ts("pool", KRS[:, :, o:o + 8], WB[0][:, :, s_ + 8:s_ + 16], -1.0, 0.0, ALU.mult, ALU.add,
                                [WB[1], bKR], [bKR])
                        self.cp("pool", KRS[:, :, o + 8:o + 16], WB[0][:, :, s_:s_ + 8], [WB[1], bKR], [bKR])
                    wq4 = WUQ.rearrange("p j (h e) -> p j h e", e=96)
                    ws4 = WUQS.rearrange("p j (h e) -> p j h e", e=96)
                    for j in range(2):
                        for a in range(2):
                            o = 64 + a * 16
                            self.ts("pool", ws4[:, j, :, o:o + 8], wq4[:, j, :, o + 8:o + 16], -1.0, 0.0,
                                    ALU.mult, ALU.add, [bWQ, bWQS], [bWQS])
                            self.cp("pool", ws4[:, j, :, o + 8:o + 16], wq4[:, j, :, o:o + 8], [bWQ, bWQS], [bWQS])
                    for r in range(2):
                        self.memset("pool", VAb[r][:, :, 64:65], 1.0, [B("VA", r)])
                    qg = ncol[:, 0:2]
                    kvg = ncol[:, 2:3]
                    bCQN = [B("CQN", c) for c in range(5)]
                    bCKN = [B("CKN", c) for c in range(5)]
                    bKRP = [B("KROPE", c) for c in range(5)]
                    for ci, (c0, n) in enumerate(CHUNKS):
                        tl = tiles_of(c0, n)
                        rdh = [bHT[i] for i in tl]
                        bcq, bsq, brs = B("cqf"), B("sq"), B("rstd")

                        def rms(nchunks, colbase, inv_n, gcol, dst_fn, bdst):
                            pss = []
                            for j in range(nchunks):
                                pi = next_ps(0, 4)
                                pss.append(pi)
                                for kc in range(8):
                                    self.mm(PS[pi][:, 0:n], WB[0][:, kc, colbase + j * 128:colbase + (j + 1) * 128],
                                            HT[:, kc, c0:c0 + n], kc == 0, kc == 7, [WB[1]] + rdh, [PB[pi]])
                                self.cp("act", cqf[:, j, 0:n], PS[pi][:, 0:n], [PB[pi]], [bcq])
                                self.act(sq[:, j, 0:n], PS[pi][:, 0:n], AF.Square, [PB[pi]], [bsq])
                            pq = next_ps(4, 6)
                            for j in range(nchunks):
                                self.mm(PS[pq][:, 0:n], ones_f, sq[:, j, 0:n], j == 0, j == nchunks - 1,
                                        [bConst, bsq], [PB[pq]])
                            self.act(rstd[:, 0:n], PS[pq][:, 0:n], AF.Sqrt, [PB[pq]], [brs], bias=epsc, scale=inv_n)
                            em.op("dve", lambda: nc.vector.reciprocal(out=rstd[:, 0:n], in_=rstd[:, 0:n]), [brs], [brs])
                            for j in range(nchunks):
                                self.stt(dst_fn(j), cqf[:, j, 0:n], gcol[:, j:j + 1], rstd[:, 0:n], ALU.mult, ALU.mult,
                                         [bcq, brs, bNcol], [bdst])

                        rms(2, 0, 1.0 / 256, qg, lambda j: CQN[:, j, c0:c0 + n], bCQN[ci])
                        rms(1, 256, 1.0 / 128, kvg, lambda j: CKN[:, c0:c0 + n], bCKN[ci])
                        pk = next_ps(0, 4)
                        pks = next_ps(0, 4)
                        for kc in range(8):
                            self.mm(PS[pk][0:96, 0:n], KRW[:, kc, :], HT[:, kc, c0:c0 + n], kc == 0, kc == 7,
                                    [bKR] + rdh, [PB[pk]])
                        for kc in range(8):
                            self.mm(PS[pks][0:96, 0:n], KRS[:, kc, :], HT[:, kc, c0:c0 + n], kc == 0, kc == 7,
                                    [bKR] + rdh, [PB[pks]])
                        bt1, bt2 = B("t1"), B("t2")
                        self.tt("dve", t1[64:96, 0:n], PS[pk][64:96, 0:n], COS[64:96, c0:c0 + n], ALU.mult,
                                [PB[pk], bCS], [bt1])
                        self.tt("dve", t2[64:96, 0:n], PS[pks][64:96, 0:n], SIN[64:96, c0:c0 + n], ALU.mult,
                                [PB[pks], bCS], [bt2])
                        self.tt("pool", KROPE[64:96, c0:c0 + n], t1[64:96, 0:n], t2[64:96, 0:n], ALU.add,
                                [bt1, bt2], [bKRP[ci]])
                    q_chunks = act_chunks
                    QT2 = v(IB + 39 * KB, [T], BF16)
                    KT2 = v(IB + 43 * KB + 512, [T], BF16)
                    alias_b = [B("cqf"), B("sq"), B("rstd")]
                    QTs = [QTh, QT2]
                    KTs = [KTh, KT2]
                    bQTs = [[B("QTh")], alias_b]
                    bKTs = [[B("KTh")], alias_b]
                    ptc = [0]

                    SBANKS = (0, 1, 6)
                    sbc = [0]

                    def prologue(h):
                        VA = VAb[h % 2]
                        bVA = B("VA", h % 2)
                        QT, KT = QTs[h % 2], KTs[h % 2]
                        bQT, bKT = bQTs[h % 2], bKTs[h % 2]
                        self.cp("pool", KT[64:96, :], KROPE[64:96, :], bKRP, bKT)
                        yield
                        for ci, (c0, n) in enumerate(CHUNKS):
                            pi = next_ps(7, 8)
                            self.mm(PS[pi][0:64, 0:n], WUKV[:, h * 128:h * 128 + 64], CKN[:, c0:c0 + n], True, True,
                                    [bWQ, bCKN[ci]], [PB[pi]])
                            self.cp("dve", KT[0:64, c0:c0 + n], PS[pi][0:64, 0:n], [PB[pi]], bKT)
                            yield
                        for g0 in range(0, NT, 8):
                            g1 = min(NT, g0 + 8)
                            pi = next_ps(7, 8)
                            for i in range(g0, g1):
                                self.mm(PS[pi][:, (i - g0) * 64:(i - g0 + 1) * 64], CKN[:, i * 128:(i + 1) * 128],
                                        WUKV[:, h * 128 + 64:h * 128 + 128], True, True,
                                        [bWQ, bCKN[min(4, (i + 2) // 4)]], [PB[pi]])
                            self.cp("dve", VA[:, g0:g1, 0:64],
                                    PS[pi][:, 0:(g1 - g0) * 64].rearrange("p (a b) -> p a b", b=64), [PB[pi]], [bVA])
                            yield
                        for ci, (c0, n) in enumerate(q_chunks):
                            cidx = CHUNKS.index((c0, n))
                            pq_ = 7
                            pqs = 7
                            for j in range(2):
                                self.mm(PS[pq_][0:96, 0:n], WUQ[:, j, h * 96:(h + 1) * 96], CQN[:, j, c0:c0 + n],
                                        j == 0, j == 1, [bWQ, bCQN[cidx]], [PB[pq_]])
                            bt1, bt2 = B("t1"), B("t2")
                            self.cp("dve", QT[0:64, c0:c0 + n], PS[pq_][0:64, 0:n], [PB[pq_]], bQT)
                            self.tt("dve", t1[64:96, 0:n], PS[pq_][64:96, 0:n], COS[64:96, c0:c0 + n], ALU.mult,
                                    [PB[pq_], bCS], [bt1])
                            yield
                            for j in range(2):
                                self.mm(PS[pqs][0:96, 0:n], WUQS[:, j, h * 96:(h + 1) * 96], CQN[:, j, c0:c0 + n],
                                        j == 0, j == 1, [bWQS, bWQ, bCQN[cidx]], [PB[pqs]])
                            self.tt("dve", t2[64:96, 0:n], PS[pqs][64:96, 0:n], SIN[64:96, c0:c0 + n], ALU.mult,
                                    [PB[pqs], bCS], [bt2])
                            self.tt("pool", QT[64:96, c0:c0 + n], t1[64:96, 0:n], t2[64:96, 0:n], ALU.add,
                                    [bt1, bt2], bQT)
                            yield

                    pgen = [None]
                    pit = [0]

                    def attend(h, ci, c0, n):
                        hsub = h % 2
                        VA = VAb[h % 2]
                        bVA = B("VA", h % 2)
                        QT, KT = QTs[h % 2], KTs[h % 2]
                        bQT, bKT = bQTs[h % 2], bKTs[h % 2]
                        keys = list(range(NT)) if c0 >= NCTX else [0, 1]
                        nq = n // 128
                        pend = []

                        def pv(pd):
                            ki, i, PT, bPT = pd
                            for qs in range(nq):
                                self.mm(PS[2 + qs][:, 0:65], PT[:, qs * 128:(qs + 1) * 128], VA[:, i, 0:65],
                                        ki == 0, ki == len(keys) - 1, [bPT, bVA], [PB[2 + qs]])
                        for ki, i in enumerate(keys):
                            ps_ = SBANKS[sbc[0] % 3]
                            sbc[0] += 1
                            self.mm(PS[ps_][:, 0:n], KT[0:96, i * 128:(i + 1) * 128], QT[0:96, c0:c0 + n],
                                    True, True, bKT + bQT, [PB[ps_]])
                            if len(pend) >= 2:
                                pv(pend.pop(0))
                            PT = PTb[ptc[0] % 4]
                            bPT = B("PT", ptc[0] % 4)
                            ptc[0] += 1
                            self.act(PT[:, 0:n], PS[ps_][:, 0:n], AF.Exp, [PB[ps_]], [bPT], scale=ATT_SCALE)
                            pend.append((ki, i, PT, bPT))
                            pit[0] += 1
                            if pgen[0] is not None and pit[0] % 3 == 0:
                                next(pgen[0], None)
                        while pend:
                            pv(pend.pop(0))
                        for qs in range(nq):
                            iq = c0 // 128 + qs
                            rslot = (qs % 4) + 4 * (ci % 2)
                            rd = rden[:, rslot:rslot + 1]
                            brd = B("rden", rslot)
                            em.op("dve", lambda: nc.vector.reciprocal(out=rd, in_=PS[2 + qs][:, 64:65]),
                                  [PB[2 + qs]], [brd])
                            self.ts("dve", YBp[:, iq, hsub * 64:(hsub + 1) * 64], PS[2 + qs][:, 0:64], rd, None,
                                    ALU.mult, ALU.bypass, [PB[2 + qs], brd], [B("YBp", iq)])

                    for _ in prologue(0):
                        pass
                    for h in range(8):
                        jpair, hsub = h // 2, h % 2
                        pgen[0] = prologue(h + 1) if h + 1 < 8 else None
                        for ci, (c0, n) in enumerate(q_chunks):
                            attend(h, ci, c0, n)
                        if pgen[0] is not None:
                            for _ in pgen[0]:
                                pass
                        if hsub == 1:
                            for g0 in range(0, NT, 4):
                                pi = next_ps(7, 8)
                                psb16 = PS[pi][:, 0:256].bitcast(BF16)
                                tls = [i for i in range(g0, min(NT, g0 + 4)) if i in act_tiles]
                                for i in tls:
                                    em.op("pe", lambda: nc.tensor.transpose(
                                        out=psb16[:, (i - g0) * 128:(i - g0 + 1) * 128], in_=YBp[:, i, :],
                                        identity=ident_bf), [B("YBp", i), bConst], [PB[pi]])
                                for i in tls:
                                    self.cp("act", YT[:, jpair, i * 128:(i + 1) * 128],
                                            psb16[:, (i - g0) * 128:(i - g0 + 1) * 128], [PB[pi]], [bYT[i]])
                    self.dump("yb%d" % l, YT, [128, 4, T], bYT, BF16)
                    if pre_barrier is not None:
                        pre_barrier()
                    em.barrier()

                def load_c():
                    CX = self.ring_load(wC_d[l, 0], [8, 512])
                    CG = self.ring_load(wC_d[l, 1], [8, 512])
                    WRI = self.ring_load(wri_d[l], [16, 128])
                    return CX, CG, WRI

                def mixer_c(Wc, pre_barrier=None):
                    CX, CG, WRI = Wc
                    XR = v(IB, [T])
                    XC = v(IB + 9 * KB, [T])
                    XCB = v(IB + 18 * KB, [T], BF16)
                    Ad = [v(IB + 23 * KB, [T]), v(IB + 41 * KB, [T])]
                    Bd = [v(IB + 32 * KB, [T]), v(IB + 50 * KB, [T])]
                    Hf = v(IB + 59 * KB, [T])
                    Hb = v(IB + 68 * KB, [T])
                    tb = [v(IB + 77 * KB + r * 2 * KB, [512]) for r in range(2)]
                    lam = ncol[:, 40:48]
                    bcc = B("ccol")
                    self.act(etmp, lam, AF.Exp, [bNcol], [bcc], scale=-1.0)
                    self.act(etmp, etmp, AF.Ln, [bcc], [bcc], bias=onec, scale=1.0)
                    self.ts("dve", ccol, etmp, -8.0, None, ALU.mult, ALU.bypass, [bcc], [bcc])
                    convw = ncol[:, 4:20].rearrange("p (j k) -> p j k", k=4)
                    convb = ncol[:, 20:24]
                    bri = ncol[:, 24:40]
                    bXR, bXC, bXCB = B("XR"), B("XC"), B("XCB")
                    bAd = [B("Ad", 0), B("Ad", 1)]
                    bBd = [B("Bd", 0), B("Bd", 1)]
                    bHf, bHb = B("Hf"), B("Hb")
                    tmps = [Hf, Hb]
                    btmps = [bHf, bHb]
                    tcn = [0]

                    def st1a(j):
                        for ci, (c0, n) in enumerate(CHUNKS):
                            tl = tiles_of(c0, n)
                            pi = next_ps(0, 4)
                            for kc in range(8):
                                self.mm(PS[pi][:, 0:n], CX[0][:, kc, j * 128:(j + 1) * 128], HT[:, kc, c0:c0 + n],
                                        kc == 0, kc == 7, [CX[1]] + [bHT[i] for i in tl], [PB[pi]])
                            self.cp("dve", XR[:, c0:c0 + n], PS[pi][:, 0:n], [PB[pi]], [bXR])
                        for (s_, e_) in ((0, NCTX), (NCTX, T)):
                            self.ts("dve", XC[:, s_:e_], XR[:, s_:e_], convw[:, j, 2:3], convb[:, j:j + 1], ALU.mult, ALU.add,
                                    [bXR, bNcol], [bXC])
                            self.stt(XC[:, s_ + 1:e_], XR[:, s_:e_ - 1], convw[:, j, 1:2], XC[:, s_ + 1:e_], ALU.mult, ALU.add,
                                     [bXR, bXC, bNcol], [bXC])
                            self.stt(XC[:, s_ + 2:e_], XR[:, s_:e_ - 2], convw[:, j, 0:1], XC[:, s_ + 2:e_], ALU.mult, ALU.add,
                                     [bXR, bXC, bNcol], [bXC])
                            self.stt(XC[:, s_:e_ - 1], XR[:, s_ + 1:e_], convw[:, j, 3:4], XC[:, s_:e_ - 1], ALU.mult, ALU.add,
                                     [bXR, bXC, bNcol], [bXC])
                        self.cp("pool", XCB, XC, [bXC], [bXCB])

                    def st1b(j):
                        for d in range(2):
                            ir = (d * 2 + 0) * 4 + j
                            ii = (d * 2 + 1) * 4 + j
                            for ci, (c0, n) in enumerate(CHUNKS):
                                pr = next_ps(4, 8)
                                pi_ = next_ps(4, 8)
                                self.mm(PS[pr][:, 0:n], WRI[0][:, ir, :], XCB[:, c0:c0 + n], True, True,
                                        [WRI[1], bXCB], [PB[pr]])
                                self.mm(PS[pi_][:, 0:n], WRI[0][:, ii, :], XCB[:, c0:c0 + n], True, True,
                                        [WRI[1], bXCB], [PB[pi_]])
                                self.act(Ad[d][:, c0:c0 + n], PS[pr][:, 0:n], AF.Sigmoid, [PB[pr], bNcol], [bAd[d]],
                                         bias=bri[:, ir:ir + 1], scale=1.0)
                                self.act(Bd[d][:, c0:c0 + n], PS[pi_][:, 0:n], AF.Sigmoid, [PB[pi_], bNcol], [bBd[d]],
                                         bias=bri[:, ii:ii + 1], scale=1.0)
                        for d in range(2):
                            self.tt("pool", Bd[d], Bd[d], XC, ALU.mult, [bBd[d], bXC], [bBd[d]])

                    def st2(j, mid=None):
                        for d in range(2):
                            self.act(Ad[d], Ad[d], AF.Exp, [bAd[d], bcc], [bAd[d]],
                                     scale=ccol[:, d * 4 + j:d * 4 + j + 1])
                        for d in range(2):
                            self.tt("dve", tmps[d], Ad[d], Ad[d], ALU.mult, [bAd[d]], [btmps[d]])
                        if mid is not None:
                            mid()
                        for d in range(2):
                            self.act(tmps[d], tmps[d], AF.Sqrt, [btmps[d]], [btmps[d]], bias=onec, scale=-1.0)
                        for d in range(2):
                            self.tt("pool", Bd[d], Bd[d], tmps[d], ALU.mult, [bBd[d], btmps[d]], [bBd[d]])
                        em.op("dve", lambda: nc.vector.tensor_tensor_scan(
                            out=Hf[:, 0:T], data0=Ad[0][:, 0:T], data1=Bd[0][:, 0:T], initial=0.0,
                            op0=ALU.mult, op1=ALU.add), [bAd[0], bBd[0]], [bHf])
                        em.op("dve", lambda: nc.vector.tensor_tensor_scan(
                            out=Hb[:, 0:NCTX][:, ::-1], data0=Ad[1][:, 0:NCTX][:, ::-1],
                            data1=Bd[1][:, 0:NCTX][:, ::-1], initial=0.0,
                            op0=ALU.mult, op1=ALU.add), [bAd[1], bBd[1]], [bHb])
                        em.op("dve", lambda: nc.vector.tensor_tensor_scan(
                            out=Hb[:, NCTX:T][:, ::-1], data0=Ad[1][:, NCTX:T][:, ::-1],
                            data1=Bd[1][:, NCTX:T][:, ::-1], initial=Hb[:, 0:1],
                            op0=ALU.mult, op1=ALU.add), [bAd[1], bBd[1], bHb], [bHb])
                        self.tt("pool", Hf, Hf, Hb, ALU.add, [bHf, bHb], [bHf])
                        for ci, (c0, n) in enumerate(act_chunks):
                            tl = tiles_of(c0, n)
                            pi = next_ps(0, 4)
                            for kc in range(8):
                                self.mm(PS[pi][:, 0:n], CG[0][:, kc, j * 128:(j + 1) * 128], HT[:, kc, c0:c0 + n],
                                        kc == 0, kc == 7, [CG[1]] + [bHT[i] for i in tl], [PB[pi]])
                            tg = tb[tcn[0] % 2]
                            btg = B("tb", tcn[0] % 2)
                            tcn[0] += 1
                            self.act(tg[:, 0:n], PS[pi][:, 0:n], AF.Gelu_apprx_tanh, [PB[pi]], [btg])
                            self.tt("dve", YT[:, j, c0:c0 + n], tg[:, 0:n], Hf[:, c0:c0 + n], ALU.mult, [btg, bHf],
                                    [bYT[i] for i in tl])

                    st1a(0)
                    st1b(0)
                    for j in range(4):
                        st2(j, (lambda jj=j: st1a(jj + 1)) if j + 1 < 4 else None)
                        if j + 1 < 4:
                            st1b(j + 1)
                    self.dump("yc%d" % l, YT, [128, 4, T], bYT, BF16)
                    if pre_barrier is not None:
                        pre_barrier()
                    em.barrier()

                def load_expert(e_):
                    return (self.ring_load(wg_d[l, e_], [8, 512]), self.ring_load(wu_d[l, e_], [8, 512]),
                            self.ring_load(wd_d[l, e_], [4, D]))
                stop = getattr(self, "stop", None)
                hold = {"A": hold0}

                def pf(name, fn):
                    def go():
                        hold[name] = fn()
                    return go
                mixer_a(hold.pop("A"), pf("m0", lambda: load_merge(0)))
                if stop == "A":
                    merge(0, hold.pop("m0"))
                else:
                    merge(0, hold.pop("m0"), pf("B", load_b))
                    mixer_b(hold.pop("B"), pf("m1", lambda: load_merge(1)))
                    if stop == "B":
                        merge(1, hold.pop("m1"))
                    else:
                        merge(1, hold.pop("m1"), pf("C", load_c))
                        mixer_c(hold.pop("C"), pf("m2", lambda: load_merge(2)))
                        merge(2, hold.pop("m2"), pf("E0", lambda: load_expert(0)) if stop is None else None)

                def layer_norm(pidx, tiles):
                    Lg = v(OFF_PH, [D])
                    Lb = v(OFF_PH + 4 * KB, [D])
                    bL = B("lnp")
                    em.dma("sp", Lg, lnp_d[l, pidx], writes=[bL])
                    em.dma("sp", Lb, lnp_d[l, pidx + 1], writes=[bL])
                    for n_, i in enumerate(tiles):
                        r = n_ % 4
                        sa = stat[:, r, :]
                        bst = B("stL", r)
                        for hf in range(2):
                            em.op("dve", lambda: nc.vector.bn_stats(out=sa[:, hf * 6:(hf + 1) * 6],
                                                                    in_=Xs[:, i, hf * 512:(hf + 1) * 512]),
                                  [bX[i]], [bst])
                        em.op("dve", lambda: nc.vector.bn_aggr(out=sa[:, 12:14], in_=sa[:, 0:12]), [bst], [bst])
                        self.act(sa[:, 14:15], sa[:, 13:14], AF.Sqrt, [bst], [bst], bias=epsc, scale=1.0)
                        em.op("dve", lambda: nc.vector.reciprocal(out=sa[:, 15:16], in_=sa[:, 14:15]), [bst], [bst])
                        self.ts("dve", sa[:, 14:15], sa[:, 12:13], sa[:, 15:16], -1.0, ALU.mult, ALU.mult, [bst], [bst])
                        self.act(Xs[:, i, :], Xs[:, i, :], AF.Identity, [bX[i], bst], [bX[i]],
                                 bias=sa[:, 14:15], scale=sa[:, 15:16])
                        self.tt("dve", Xs[:, i, :], Xs[:, i, :], Lg, ALU.mult, [bX[i], bL], [bX[i]])
                        self.tt("pool", Xs[:, i, :], Xs[:, i, :], Lb, ALU.add, [bX[i], bL], [bX[i]])

                for i in act_tiles:
                    em.dma("sp", Xs[:, i, :], XD[i], reads=[bXD[i]], writes=[bX[i]])
                layer_norm(0, act_tiles)
                self.dump("x1_%d" % l, Xs, [128, NT, D], bX)
                if stop in ("A", "B", "C"):
                    return act_tiles

                self.F32T = v(OFF_PH + 8 * KB, [16, 128])
                gate_bcast(colmods, bCol, 5)
                build_T(True)
                RB = OFF_PH + 16 * KB
                SC = v(RB, [NT * 16])
                SEL = v(RB + 1152, [NT * 16])
                PR = v(RB + 2304, [6, NT * 4])
                GS = v(RB + 4096, [NT * 4])
                GM = v(RB + 4416, [NT])
                OG = v(RB + 4512, [NT * 4])
                PEN = v(RB + 4800, [NT * 4])
                SM = v(RB + 5120, [NT * 16])
                CNT = v(RB + 6272, [NT * 16])
                CMP = v(RB + 7424, [NT * 4])
                WM = v(RB + 7712, [NT * 16])
                DEN = v(RB + 8864, [NT])
                bR = B("route")
                bCW = B("CW")
                self.act(SC, PS[6][:, 0:NT * 16], AF.Sigmoid, [PB[6]], [bR])
                self.tt("dve", SEL, SC, rbias, ALU.add, [bR, bConst], [bR])
                sel4 = SEL.rearrange("p (a b) -> p a b", b=4)
                pairs = [(0, 1), (0, 2), (0, 3), (1, 2), (1, 3), (2, 3)]
                for pi_, (a, b_) in enumerate(pairs):
                    self.tt("dve", PR[:, pi_, :], sel4[:, :, a], sel4[:, :, b_], ALU.add, [bR], [bR])
                self.tt("dve", GS, PR[:, 0, :], PR[:, 1, :], ALU.max, [bR], [bR])
                for pi_ in range(2, 6):
                    self.tt("dve", GS, GS, PR[:, pi_, :], ALU.max, [bR], [bR])
                gs3 = GS.rearrange("p (a b) -> p a b", b=4)
                self.tt("dve", GM, gs3[:, :, 0], gs3[:, :, 1], ALU.max, [bR], [bR])
                for g in range(2, 4):
                    self.tt("dve", GM, GM, gs3[:, :, g], ALU.max, [bR], [bR])
                og3 = OG.rearrange("p (a b) -> p a b", b=4)
                for g in range(4):
                    self.tt("dve", og3[:, :, g], gs3[:, :, g], GM, ALU.is_ge, [bR], [bR])
                self.ts("dve", PEN, OG, BIG, -BIG, ALU.mult, ALU.add, [bR], [bR])
                sm4 = SM.rearrange("p (a b) -> p a b", b=4)
                cnt4 = CNT.rearrange("p (a b) -> p a b", b=4)
                for e_ in range(4):
                    self.tt("dve", sm4[:, :, e_], sel4[:, :, e_], PEN, ALU.add, [bR], [bR])
                for e_ in range(4):
                    first = True
                    for e2 in range(4):
                        if e2 == e_:
                            continue
                        if first:
                            self.tt("dve", cnt4[:, :, e_], sm4[:, :, e2], sm4[:, :, e_], ALU.is_gt, [bR], [bR])
                            first = False
                        else:
                            self.tt("dve", CMP, sm4[:, :, e2], sm4[:, :, e_], ALU.is_gt, [bR], [bR])
                            self.tt("dve", cnt4[:, :, e_], cnt4[:, :, e_], CMP, ALU.add, [bR], [bR])
                wm4 = WM.rearrange("p (a b) -> p a b", b=4)
                sc4 = SC.rearrange("p (a b) -> p a b", b=4)
                for e_ in range(4):
                    self.ts("dve", CMP, cnt4[:, :, e_], 1.5, None, ALU.is_lt, ALU.bypass, [bR], [bR])
                    self.tt("dve", CMP, CMP, OG, ALU.mult, [bR], [bR])
                    self.tt("dve", wm4[:, :, e_], sc4[:, :, e_], CMP, ALU.mult, [bR], [bR])
                wm16 = WM.rearrange("p (a b) -> p a b", b=16)
                em.op("dve", lambda: nc.vector.tensor_reduce(out=DEN, in_=wm16, axis=AX.X, op=ALU.add), [bR], [bR])
                em.op("dve", lambda: nc.vector.reciprocal(out=DEN, in_=DEN), [bR], [bR])
                for e_ in range(16):
                    self.stt(CWt[:, :, e_], wm16[:, :, e_], 2.5, DEN, ALU.mult, ALU.mult, [bR], [bCW])
                self.dump("cw%d" % l, CWt, [128, NT, 16], [bCW])

                MB = OFF_PH + 8 * KB
                ATb = [v(MB + r * 4 * KB, [4, 512], BF16) for r in range(2)]
                bATl = [[B("f32t", r, kc) for kc in range(8)] for r in range(2)]
                sgm = [v(RB + r * 2 * KB, [512]) for r in range(2)]
                tmm = [v(RB + 4 * KB + r * 2 * KB, [512]) for r in range(2)]
                claimed = set()

                def claim(key):
                    if key in claimed:
                        return []
                    claimed.add(key)
                    return [bR]

                cnt = 0
                Wn = hold.pop("E0")
                for e_ in range(NEXP):
                    WG, WU, WD = Wn
                    if e_ + 1 < NEXP:
                        Wn = load_expert(e_ + 1)
                    for ci, (c0, n) in enumerate(act_chunks):
                        tl = tiles_of(c0, n)
                        rdh = [bHT[i] for i in tl]
                        AT = ATb[(e_ * 5 + ci) % 2]
                        bATs = bATl[(e_ * 5 + ci) % 2]
                        for j in range(4):
                            pg = next_ps(0, 2)
                            pu = next_ps(2, 4)
                            for kc in range(8):
                                self.mm(PS[pg][:, 0:n], WG[0][:, kc, j * 128:(j + 1) * 128], HT[:, kc, c0:c0 + n],
                                        kc == 0, kc == 7, [WG[1]] + rdh, [PB[pg]])
                            for kc in range(8):
                                self.mm(PS[pu][:, 0:n], WU[0][:, kc, j * 128:(j + 1) * 128], HT[:, kc, c0:c0 + n],
                                        kc == 0, kc == 7, [WU[1]] + rdh, [PB[pu]])
                            sg = sgm[cnt % 2]
                            bsg = B("sgm", cnt % 2)
                            ck = claim(("sgm", cnt % 2))
                            cnt += 1
                            self.act(sg[:, 0:n], PS[pg][:, 0:n], AF.Silu, [PB[pg]], [bsg] + ck)
                            self.tt("dve", AT[:, j, 0:n], sg[:, 0:n], PS[pu][:, 0:n], ALU.mult, [bsg, PB[pu]], bATs)
                        for t, i in enumerate(tl):
                            w = which(i)
                            for hf in range(2):
                                py = next_ps(4, 8)
                                for j in range(4):
                                    self.mm(PS[py][:, :], AT[:, j, t * 128:(t + 1) * 128],
                                            WD[0][:, j, hf * 512:(hf + 1) * 512], j == 0, j == 3,
                                            bATs + [WD[1]], [PB[py]])
                                tmp = tmm[cnt % 2]
                                btmp = B("tmm", cnt % 2)
                                ck = claim(("tmm", cnt % 2))
                                cnt += 1
                                self.stt(tmp, PS[py][:, :], CWt[:, i, e_:e_ + 1], GBv[w][:, hf * 512:(hf + 1) * 512],
                                         ALU.mult, ALU.mult, [PB[py], bCW, bGB], [btmp] + ck)
                                self.tt("pool", Xs[:, i, hf * 512:(hf + 1) * 512], Xs[:, i, hf * 512:(hf + 1) * 512],
                                        tmp, ALU.add, [btmp, bX[i]], [bX[i]])
                layer_norm(2, act_tiles)
                self.dump("x2_%d" % l, Xs, [128, NT, D], bX)
                return act_tiles

            epsc = v(OFF_CONST + 2304, [1])
            onec = v(OFF_CONST + 2308, [1])
            self.memset("dve", epsc, EPS, [bConst])
            self.memset("dve", onec, 1.0, [bConst])

            for l in range(L):
                layer(l)

            for i in range(2, NT):
                em.dma("sp", out_d[(i - 2) * 128:(i - 1) * 128, :], Xs[:, i, :], reads=[bX[i]])
            em.finish("sp")
            self.stats = dict(ninst=dict(em.ninst), nwait=em.nwait, nsem=em.nsem)
        return nc


def _rope_tables():
    rows = NLAT // 64
    row = np.repeat(np.arange(rows, dtype=np.float32), 64)
    col = np.tile(np.arange(64, dtype=np.float32), rows)
    n_freq = 8
    inv = (np.float32(10000.0) ** (-np.arange(n_freq, dtype=np.float32) / np.float32(n_freq))).astype(np.float32)
    ang = np.stack([row[:, None] * inv, col[:, None] * inv], axis=1).astype(np.float32)
    cos = np.cos(ang).astype(np.float32)
    sin = np.sin(ang).astype(np.float32)
    tab = np.zeros((2, 128, T), np.float32)
    tab[0] = 1.0
    for a in range(2):
        for hf in range(2):
            r0 = 64 + a * 16 + hf * 8
            tab[0, r0:r0 + 8, NCTX:] = cos[:, a, :].T
            tab[1, r0:r0 + 8, NCTX:] = sin[:, a, :].T
    return tab


def _kc(w):
    *lead, K, N = w.shape
    w = w.reshape(*lead, K // 128, 128, N)
    nd = w.ndim
    perm = list(range(nd - 3)) + [nd - 2, nd - 3, nd - 1]
    return np.ascontiguousarray(w.transpose(perm))


def _col(vv):
    *lead, n = vv.shape
    return np.ascontiguousarray(np.swapaxes(vv.reshape(*lead, n // 128, 128), -1, -2))


def prep_shared(inp):
    f = lambda a: np.ascontiguousarray(np.asarray(a, dtype=np.float32))
    sh = {}
    sh["ident"] = np.eye(128, dtype=np.float32)
    sh["cossin"] = _rope_tables()
    sh["rbias"] = np.ascontiguousarray(np.broadcast_to(np.tile(f(inp["router_bias"]), NT)[None, :], (128, NT * 16)))
    sh["wrouter"] = _kc(f(inp["w_router"]))
    w_ada = f(inp["w_ada"])
    wa = _kc(w_ada)
    sh["w_ada_h"] = np.ascontiguousarray(wa.reshape(DEPTH, 128, 8, 12, 512).transpose(0, 3, 1, 2, 4))
    sh["b_ada_col"] = _col(f(inp["b_ada"]))
    w_in = _kc(f(inp["w_in"]))
    sh["wA"] = np.ascontiguousarray(np.stack([w_in[..., 0:512], w_in[..., 512:1024]], axis=1))
    sh["wB"] = np.ascontiguousarray(w_in[..., 1024:1440])
    sh["wC"] = np.ascontiguousarray(np.stack([w_in[..., 1440:1952], w_in[..., 1952:2464]], axis=1))
    sh["wG"] = np.ascontiguousarray(np.stack([w_in[..., 2464 + s * 512:2464 + (s + 1) * 512] for s in range(6)], axis=1))
    aln = np.stack([f(inp["a_ln_g"]), f(inp["a_ln_b"])], axis=1)
    sh["aln"] = np.ascontiguousarray(np.broadcast_to(aln[:, :, None, :], (DEPTH, 2, 128, 512)))
    sh["wsT"] = np.ascontiguousarray(f(inp["w_s"]).transpose(0, 3, 1, 2))
    sh["bs_row"] = np.ascontiguousarray(f(inp["b_s"]).reshape(DEPTH, 1, 512))
    ncol = np.zeros((DEPTH, 128, 48), np.float32)
    ncol[:, :, 0:2] = _col(f(inp["q_norm_g"]))
    ncol[:, :, 2:3] = _col(f(inp["kv_norm_g"]))
    cw = f(inp["conv_w"])
    ncol[:, :, 4:20] = np.ascontiguousarray(cw.reshape(DEPTH, 4, 4, 128).transpose(0, 3, 2, 1)).reshape(DEPTH, 128, 16)
    ncol[:, :, 20:24] = _col(f(inp["conv_b"]))
    br = _col(f(inp["b_r"]))
    bi = _col(f(inp["b_i"]))
    for d in range(2):
        ncol[:, :, 24 + (d * 2 + 0) * 4:24 + (d * 2 + 0) * 4 + 4] = br[:, d]
        ncol[:, :, 24 + (d * 2 + 1) * 4:24 + (d * 2 + 1) * 4 + 4] = bi[:, d]
    lam = _col(f(inp["lru_lambda"]))
    for d in range(2):
        ncol[:, :, 40 + d * 4:44 + d * 4] = lam[:, d]
    sh["ncol"] = ncol
    sh["wuq"] = _kc(f(inp["w_uq"]))
    sh["wukv"] = f(inp["w_ukv"])
    wri = np.zeros((DEPTH, 128, 16, 128), np.float32)
    for d in range(2):
        for ri, name in enumerate(("w_r", "w_i")):
            w = f(inp[name])[:, d]
            for j in range(4):
                for bb in range(2):
                    wri[:, bb * 64:(bb + 1) * 64, (d * 2 + ri) * 4 + j, bb * 64:(bb + 1) * 64] = w[:, 2 * j + bb]
    sh["wri"] = wri
    sh["wbr"] = _kc(f(inp["w_br"]))
    wo = _kc(f(inp["w_o"]))
    sh["wo"] = np.ascontiguousarray(np.stack([wo[..., 0:512], wo[..., 512:1024]], axis=1))
    lnp = np.stack([f(inp["ln1_g"]), f(inp["ln1_b"]), f(inp["ln2_g"]), f(inp["ln2_b"])], axis=1)
    sh["lnp"] = np.ascontiguousarray(np.broadcast_to(lnp[:, :, None, :], (DEPTH, 4, 128, D)))
    sh["wg"] = _kc(f(inp["w_gate"]))
    sh["wu"] = _kc(f(inp["w_up"]))
    sh["wd"] = _kc(f(inp["w_down"]))
    return sh


def prep_core(inp, b):
    f = lambda a: np.asarray(a, dtype=np.float32)
    xin = np.ascontiguousarray(np.concatenate([f(inp["ctx"])[b], f(inp["x"])[b]], axis=0))
    condT = np.concatenate([_col(f(inp["c"])[b]), _col(f(inp["c_ctx"]))], axis=1)
    return {"xin": xin, "condT": np.ascontiguousarray(condT)}


_CACHE = {}


def kernel(**inputs):
    nb = inputs["x"].shape[0]
    if "prog" not in _CACHE:
        p = Prog()
        p.build()
        _CACHE["prog"] = p
    p = _CACHE["prog"]
    sh = prep_shared(inputs)
    in_maps = []
    for b in range(nb):
        m = dict(sh)
        m.update(prep_core(inputs, b))
        in_maps.append(m)
    res = run_bass_kernel_spmd(p.nc, in_maps, core_ids=list(range(nb)))
    out = np.stack([np.asarray(r["out"], dtype=np.float32) for r in res.results], axis=0)
    return out
```

```python
import numpy as np
from contextlib import ExitStack
import concourse.bass as bass
import concourse.mybir as mybir
from concourse.bass_utils import run_bass_kernel_spmd

F32 = mybir.dt.float32
BF16 = mybir.dt.bfloat16
AF = mybir.ActivationFunctionType
ALU = mybir.AluOpType
AX = mybir.AxisListType

D = 1024
DEPTH = 4
NCTX = 256
NLAT = 2048
T = NCTX + NLAT
NT = T // 128
ALPHA = (2.0 * DEPTH) ** 0.25
EPS = 1e-6
NEXP = 16
KB = 1024
CHUNKS = [(0, 256), (256, 512), (768, 512), (1280, 512), (1792, 512)]
ATT_SCALE = 96 ** -0.5
BIG = 1.0e4

OFF_CONST = 0
OFF_GB = 12 * KB
OFF_HT = 20 * KB
OFF_RING = 56 * KB
NSLOT = 6
SLOT = 8 * KB
OFF_X = 104 * KB
OFF_PH = 176 * KB
ARENA = 204 * KB


class Buf:
    __slots__ = ("name", "w", "r", "x")

    def __init__(self, name=""):
        self.name = name
        self.w = None
        self.r = {}
        self.x = False


class Em:
    SEM_LIMIT = 30000
    NDMA = 8

    def __init__(self, nc, stack):
        self.nc = nc
        self.stack = stack
        self.eng = {"pe": nc.tensor, "act": nc.scalar, "dve": nc.vector,
                    "pool": nc.gpsimd, "sp": nc.sync}
        self.cur = {}
        self.nsem = 0
        self.waited = {e: {} for e in self.eng}
        self.last = {}
        for e in self.eng:
            self._new_sem(e)
        self.dq = {}
        self.dqi = {}
        self.dlast = {}
        for q in ("sp", "pool"):
            self.dq[q] = [self._alloc("d%s%d" % (q, i)) for i in range(self.NDMA)]
            self.dqi[q] = 0
        self.bufs = {}
        self.ninst = {e: 0 for e in self.eng}
        self.nwait = 0

    def _alloc(self, name):
        self.nsem += 1
        key = "%s_%d" % (name, self.nsem)
        sem = self.stack.enter_context(self.nc.semaphore(key))
        return [key, sem, 0]

    def _new_sem(self, e):
        self.cur[e] = self._alloc("s" + e)

    def B(self, *key):
        b = self.bufs.get(key)
        if b is None:
            b = Buf(str(key))
            self.bufs[key] = b
        return b

    def _deps(self, reads, writes):
        deps = {}

        def add(t):
            if t is None:
                return
            k = t[0]
            if k not in deps or deps[k][2] < t[2]:
                deps[k] = t
        for b in reads:
            add(b.w)
            if b.x:
                for t in b.r.values():
                    add(t)
        for b in writes:
            add(b.w)
            for t in b.r.values():
                add(t)
        return deps

    def _wait(self, e, deps, skip_self=False):
        w = self.waited[e]
        for k, (_, sem, v) in deps.items():
            if w.get(k, 0) >= v:
                continue
            if skip_self and k == self.cur[e][0]:
                continue
            self.eng[e].wait_ge(sem, v)
            self.nwait += 1
            w[k] = v

    def _commit(self, ticket, reads, writes):
        for b in writes:
            b.w = ticket
            b.r = {}
        for b in reads:
            b.r[ticket[0]] = ticket

    def op(self, e, fn, reads=(), writes=()):
        deps = self._deps(reads, writes)
        self._wait(e, deps, skip_self=(e == "pe"))
        ins = fn()
        c = self.cur[e]
        c[2] += 1
        ins.then_inc(c[1], 1)
        ticket = (c[0], c[1], c[2])
        self.last[e] = ticket
        self._commit(ticket, reads, writes)
        self.ninst[e] += 1
        if c[2] >= self.SEM_LIMIT:
            self._new_sem(e)
        return ticket

    def dma(self, q, out, in_, reads=(), writes=()):
        deps = self._deps(reads, writes)
        i = self.dqi[q]
        self.dqi[q] = i + 1
        si = i % self.NDMA
        slot = self.dq[q][si]
        if slot[2] > 0:
            t = (slot[0], slot[1], slot[2])
            if slot[0] not in deps or deps[slot[0]][2] < slot[2]:
                deps[slot[0]] = t
        self._wait(q, deps)
        ins = self.eng[q].dma_start(out=out, in_=in_)
        slot[2] += 16
        ins.then_inc(slot[1], 16)
        ticket = (slot[0], slot[1], slot[2])
        self.dlast[(q, si)] = ticket
        self._commit(ticket, reads, writes)
        if slot[2] >= self.SEM_LIMIT:
            self.dq[q][si] = self._alloc("d%s" % q)
        return ticket

    def barrier(self):
        tickets = list(self.last.values()) + list(self.dlast.values())
        for e in self.eng:
            deps = {}
            for t in tickets:
                if t[0] not in deps or deps[t[0]][2] < t[2]:
                    deps[t[0]] = t
            self._wait(e, deps)

    def finish(self, e="sp"):
        tickets = list(self.last.values()) + list(self.dlast.values())
        deps = {}
        for t in tickets:
            if t[0] not in deps or deps[t[0]][2] < t[2]:
                deps[t[0]] = t
        self._wait(e, deps)


class Prog:
    def __init__(self, nl=DEPTH, dbg=()):
        self.nl = nl
        self.dbg = set(dbg)
        self.nc = bass.Bass("TRN2", target_bir_lowering=False)
        self.din = {}
        self.dout = {}

    def inp(self, name, shape):
        self.din[name] = self.nc.dram_tensor(name, list(shape), F32, kind="ExternalInput").ap()
        return self.din[name]

    def outp(self, name, shape, dt=F32):
        self.dout[name] = self.nc.dram_tensor(name, list(shape), dt, kind="ExternalOutput").ap()
        return self.dout[name]

    def v(self, off, shape, dt=F32):
        n = 1
        for s in shape:
            n *= s
        esz = 4 if dt == F32 else 2
        nb = n * esz
        assert off % 4 == 0 and nb % 4 == 0, (off, shape)
        assert off + nb <= ARENA, (off, shape)
        ap = self.AR[:, off // 4:(off + nb) // 4]
        if dt != F32:
            ap = ap.bitcast(dt)
        if len(shape) == 2:
            ap = ap.rearrange("p (a b) -> p a b", b=shape[1])
        elif len(shape) == 3:
            ap = ap.rearrange("p (a b c) -> p a b c", b=shape[1], c=shape[2])
        return ap

    def ring_take(self):
        s = self.ring_i % NSLOT
        self.ring_i += 1
        return OFF_RING + s * SLOT, self.em.B("ring", s)

    def ring_load(self, src, shape):
        off, b = self.ring_take()
        view = self.v(off, shape, BF16)
        self.em.dma("pool", view, src, writes=[b])
        return view, b

    def psb(self, i):
        return self.PS[i], self.em.B("ps", i)

    def mm(self, out, lhsT, rhs, start, stop, reads, writes):
        nc = self.nc
        self.em.op("pe", lambda: nc.tensor.matmul(out=out, lhsT=lhsT, rhs=rhs, start=start, stop=stop),
                   reads, writes)

    def act(self, out, in_, func, reads, writes, bias=None, scale=None):
        nc = self.nc
        kw = {}
        if bias is not None:
            kw["bias"] = bias
        if scale is not None:
            kw["scale"] = scale
        self.em.op("act", lambda: nc.scalar.activation(out=out, in_=in_, func=func, **kw), reads, writes)

    def tt(self, e, out, in0, in1, op, reads, writes):
        eng = self.em.eng[e]
        self.em.op(e, lambda: eng.tensor_tensor(out=out, in0=in0, in1=in1, op=op), reads, writes)

    def ts(self, e, out, in0, s1, s2, op0, op1, reads, writes):
        eng = self.em.eng[e]
        self.em.op(e, lambda: eng.tensor_scalar(out=out, in0=in0, scalar1=s1, scalar2=s2, op0=op0, op1=op1),
                   reads, writes)

    def stt(self, out, in0, scalar, in1, op0, op1, reads, writes):
        nc = self.nc
        self.em.op("dve", lambda: nc.vector.scalar_tensor_tensor(out=out, in0=in0, scalar=scalar, in1=in1,
                                                                  op0=op0, op1=op1), reads, writes)

    def cp(self, e, out, in_, reads, writes):
        if e == "act":
            nc = self.nc
            self.em.op("act", lambda: nc.scalar.copy(out=out, in_=in_), reads, writes)
        else:
            eng = self.em.eng[e]
            self.em.op(e, lambda: eng.tensor_copy(out=out, in_=in_), reads, writes)

    def memset(self, e, ap, val, writes):
        eng = self.em.eng[e]
        self.em.op(e, lambda: eng.memset(ap, val), (), writes)

    def dump(self, name, ap_sb, shape, reads, dt=F32):
        if name not in self.dbg:
            return
        o = self.outp("dbg_" + name, shape, dt)
        self.em.dma("sp", o, ap_sb, reads=reads)

    def build(self):
        nc = self.nc
        L = self.nl
        inp = self.inp
        xin = inp("xin", [T, D])
        condT_d = inp("condT", [128, 16])
        ident_d = inp("ident", [128, 128])
        cossin_d = inp("cossin", [2, 128, T])
        rbias_d = inp("rbias", [128, NT * 16])
        wrouter_d = inp("wrouter", [128, 8, 16])
        w_ada_d = inp("w_ada_h", [DEPTH, 12, 128, 8, 512])
        b_ada_d = inp("b_ada_col", [DEPTH, 128, 48])
        wA_d = inp("wA", [DEPTH, 2, 128, 8, 512])
        wB_d = inp("wB", [DEPTH, 128, 8, 416])
        wC_d = inp("wC", [DEPTH, 2, 128, 8, 512])
        wG_d = inp("wG", [DEPTH, 6, 128, 8, 512])
        aln_d = inp("aln", [DEPTH, 2, 128, 512])
        wsT_d = inp("wsT", [DEPTH, 128, 4, 128])
        bs_d = inp("bs_row", [DEPTH, 1, 512])
        ncol_d = inp("ncol", [DEPTH, 128, 48])
        wuq_d = inp("wuq", [DEPTH, 128, 2, 768])
        wukv_d = inp("wukv", [DEPTH, 128, 1024])
        wri_d = inp("wri", [DEPTH, 128, 16, 128])
        wbr_d = inp("wbr", [DEPTH, 3, 128, 4, 1024])
        wo_d = inp("wo", [DEPTH, 2, 128, 8, 512])
        lnp_d = inp("lnp", [DEPTH, 4, 128, D])
        wg_d = inp("wg", [DEPTH, NEXP, 128, 8, 512])
        wu_d = inp("wu", [DEPTH, NEXP, 128, 8, 512])
        wd_d = inp("wd", [DEPTH, NEXP, 128, 4, 1024])
        out_d = self.outp("out", [NLAT, D])
        XD = nc.dram_tensor("xscr", [NT, 128, D], F32).ap()

        with ExitStack() as st:
            self.em = em = Em(nc, st)
            self.AR = st.enter_context(nc.sbuf_tensor("arena", [128, ARENA // 4], F32))
            self.PS = [st.enter_context(nc.psum_tensor("ps%d" % i, [128, 512], F32)) for i in range(8)]
            self.ring_i = 0
            B = em.B
            v = self.v
            PS = self.PS
            PB = [B("ps", i) for i in range(8)]
            for b_ in PB:
                b_.x = True

            ident = v(OFF_CONST + 0, [128])
            ident_bf = v(OFF_CONST + 512, [128], BF16)
            ones_bf = v(OFF_CONST + 768, [128], BF16)
            ones_f = v(OFF_CONST + 1024, [128])
            CM = [v(OFF_CONST + 1536, [48, 2]), v(OFF_CONST + 10752, [48, 2])]
            condT = v(OFF_CONST + 1920, [16])
            conds_bf = v(OFF_CONST + 1984, [8, 2], BF16)
            ncol = v(OFF_CONST + 2048, [48])
            ccol = v(OFF_CONST + 2240, [8])
            etmp = v(OFF_CONST + 2272, [8])
            wrouter = v(OFF_CONST + 2560, [8, 16])
            rbias = v(OFF_CONST + 3072, [NT * 16])
            bs_row = v(OFF_CONST + 4224, [512], BF16)
            alnG = v(OFF_CONST + 5248, [512])
            alnB = v(OFF_CONST + 7296, [512])
            stat = v(OFF_CONST + 9344, [4, 16])
            CWt = v(OFF_CONST + 9600, [NT, 16])
            GBv = [v(OFF_GB, [D]), v(OFF_GB + 4 * KB, [D])]
            HT = v(OFF_HT, [8, T], BF16)
            Xs = v(OFF_X, [NT, D])
            bConst = B("const")
            bHT = [B("HT", i) for i in range(NT)]
            bX = [B("X", i) for i in range(NT)]
            bXD = [B("XD", i) for i in range(NT)]
            bYT = [B("YT", i) for i in range(NT)]
            bGB = B("GB")
            bCM = [B("colmods", 0), B("colmods", 1)]
            bNcol = B("ncol")

            def tiles_of(c0, n):
                return list(range(c0 // 128, (c0 + n) // 128))

            def which(i):
                return 1 if i < 2 else 0

            em.dma("sp", ident, ident_d, writes=[bConst])
            em.dma("sp", condT, condT_d, writes=[bConst])
            em.dma("sp", wrouter, wrouter_d, writes=[bConst])
            em.dma("sp", rbias, rbias_d, writes=[bConst])
            for i in range(NT):
                em.dma("sp", Xs[:, i, :], xin[i * 128:(i + 1) * 128, :], writes=[bX[i]])
            self.memset("dve", ones_f, 1.0, [bConst])
            self.memset("dve", ones_bf, 1.0, [bConst])
            self.cp("dve", ident_bf, ident, [bConst], [bConst])
            sil = v(OFF_PH + 25 * KB, [16])
            self.act(sil, condT, AF.Silu, [bConst], [B("sil")])
            for w in range(2):
                self.cp("dve", conds_bf[:, :, w], sil[:, w * 8:(w + 1) * 8], [B("sil")], [bConst])

            psrr = [0]

            def next_ps(lo, hi):
                i = lo + psrr[0] % (hi - lo)
                psrr[0] += 1
                return i

            def s1_steps(l2):
                cm = CM[l2 % 2]
                bcm = bCM[l2 % 2]
                badac = v(OFF_PH + 27 * KB, [48])
                em.dma("sp", badac, b_ada_d[l2], writes=[B("badac")])
                def s1_load(s_):
                    W_ = v(OFF_PH + (s_ % 2) * SLOT, [8, 512], BF16)
                    bW_ = B("s1ring", s_ % 2)
                    em.dma("pool", W_, w_ada_d[l2, s_], writes=[bW_])
                    return W_, bW_
                q0 = []
                if l2 == 0:
                    for s_ in range(5):
                        q0.append(self.ring_load(w_ada_d[0, s_], [8, 512]))
                for s_ in range(12):
                    if l2 == 0:
                        if s_ + 5 < 12:
                            q0.append(self.ring_load(w_ada_d[0, s_ + 5], [8, 512]))
                        W, bW = q0.pop(0)
                    else:
                        if s_ % 4 == 0:
                            nxt = s1_load(s_)
                            yield
                        W, bW = nxt
                        if (s_ + 1) % 4 != 0:
                            nxt = s1_load(s_ + 1)
                    pm = next_ps(4, 8)
                    for cc in range(4):
                        for kc in range(8):
                            self.mm(PS[pm][:, 2 * cc:2 * cc + 2], W[:, kc, cc * 128:(cc + 1) * 128],
                                    conds_bf[:, kc, :], kc == 0, kc == 7, [bW, bConst], [PB[pm]])
                    psv = PS[pm][:, 0:8].rearrange("p (a b) -> p a b", b=2)
                    for w in range(2):
                        self.tt("dve", cm[:, 4 * s_:4 * s_ + 4, w], psv[:, :, w], badac[:, 4 * s_:4 * s_ + 4], ALU.add,
                                [PB[pm], B("badac")], [bcm])
                    yield
                for m in (1, 4):
                    self.ts("dve", cm[:, m * 8:(m + 1) * 8, :], cm[:, m * 8:(m + 1) * 8, :], 1.0, None,
                            ALU.add, ALU.bypass, [bcm], [bcm])
                yield

            def gate_bcast(colmods, bCol, m):
                diag = v(OFF_PH + 26 * KB, [2, 128])
                for w in range(2):
                    for hf in range(2):
                        pi = next_ps(6, 8)
                        for q in range(4):
                            cc = hf * 4 + q
                            dd = diag[:, (cc + w) % 2, :]
                            bd = B("diag", (cc + w) % 2)
                            self.ts("dve", dd, ident, colmods[:, m * 8 + cc, w:w + 1], None, ALU.mult, ALU.bypass,
                                    [bConst, bCol], [bd])
                            self.mm(PS[pi][:, q * 128:(q + 1) * 128], ones_f, dd, True, True,
                                    [bConst, bd], [PB[pi]])
                        self.cp("act", GBv[w][:, hf * 512:(hf + 1) * 512], PS[pi][:, :], [PB[pi]], [bGB])

            def layer(l):
                ctx_out = l < DEPTH - 1
                last = (l == L - 1)
                act_chunks = CHUNKS if ctx_out else CHUNKS[1:]
                act_tiles = list(range(NT)) if ctx_out else list(range(2, NT))

                colmods = CM[l % 2]
                bCol = bCM[l % 2]
                em.dma("sp", ncol, ncol_d[l], writes=[bNcol])
                if l == 0:
                    for _ in s1_steps(0):
                        pass
                s1gen = s1_steps(l + 1) if l + 1 < L else None
                self.dump("colmods%d" % l, colmods, [128, 48, 2], [bCol])
                gate_bcast(colmods, bCol, 2)

                def build_T(dst_is_ft):
                    mS, mB = (4, 3) if dst_is_ft else (1, 0)
                    for i in (act_tiles if dst_is_ft else range(NT)):
                        w = which(i)
                        for hf in range(2):
                            pi = next_ps(4, 6) if not dst_is_ft else next_ps(4, 6)
                            for q in range(4):
                                kc = hf * 4 + q
                                self.em.op("pe", lambda: nc.tensor.transpose(
                                    out=PS[pi][:, q * 128:(q + 1) * 128],
                                    in_=Xs[:, i, kc * 128:(kc + 1) * 128], identity=ident),
                                    [bX[i], bConst], [PB[pi]])
                            for q in range(4):
                                kc = hf * 4 + q
                                sc = colmods[:, mS * 8 + kc, w:w + 1]
                                bi = colmods[:, mB * 8 + kc, w:w + 1]
                                src = PS[pi][:, q * 128:(q + 1) * 128]
                                if dst_is_ft:
                                    f32t = self.F32T[:, (i % 2) * 8 + kc, :]
                                    bf = B("f32t", i % 2, kc)
                                    self.act(f32t, src, AF.Identity, [PB[pi], bCol], [bf], bias=bi, scale=sc)
                                    self.cp("dve", HT[:, kc, i * 128:(i + 1) * 128], f32t, [bf], [bHT[i]])
                                else:
                                    dst = HT[:, kc, i * 128:(i + 1) * 128]
                                    if hf == 0:
                                        self.act(dst, src, AF.Identity, [PB[pi], bCol], [bHT[i]], bias=bi, scale=sc)
                                    else:
                                        self.ts("dve", dst, src, sc, bi, ALU.mult, ALU.add, [PB[pi], bCol], [bHT[i]])
                        if dst_is_ft:
                            for kc in range(8):
                                self.mm(PS[6][:, i * 16:(i + 1) * 16], self.F32T[:, (i % 2) * 8 + kc, :],
                                        wrouter[:, kc, :], kc == 0, kc == 7,
                                        [B("f32t", i % 2, kc), bConst], [PB[6]])
                        if dst_is_ft:
                            self.em.op("act", lambda: nc.scalar.mul(out=Xs[:, i, :], in_=Xs[:, i, :], mul=ALPHA),
                                       [bX[i]], [bX[i]])
                        else:
                            self.ts("pool", Xs[:, i, :], Xs[:, i, :], ALPHA, 0.0, ALU.mult, ALU.add, [bX[i]], [bX[i]])
                        if not dst_is_ft:
                            em.dma("sp", XD[i], Xs[:, i, :], reads=[bX[i]], writes=[bXD[i]])

                def load_a():
                    AU = self.ring_load(wA_d[l, 0], [8, 512])
                    AV = self.ring_load(wA_d[l, 1], [8, 512])
                    WS = self.ring_load(wsT_d[l], [4, 128])
                    em.dma("pool", bs_row[0:1, :], bs_d[l], writes=[B("bsrow")])
                    em.dma("sp", alnG, aln_d[l, 0], writes=[B("aln")])
                    em.dma("sp", alnB, aln_d[l, 1], writes=[B("aln")])
                    return AU, AV, WS

                hold0 = load_a()
                build_T(False)
                self.dump("HT%d" % l, HT, [128, 8, T], bHT, BF16)
                em.barrier()

                def load_merge(k):
                    G = [self.ring_load(wG_d[l, 2 * k + hf], [8, 512]) for hf in range(2)]
                    BR = self.ring_load(wbr_d[l, k], [4, D])
                    WO = [self.ring_load(wo_d[l, hf], [8, 512]) for hf in range(2)]
                    return G, BR, WO

                def merge(k, Wm, pre_barrier=None):
                    YT = self.YT
                    G, BR, WO = Wm
                    base = OFF_X + 18 * KB
                    Mb = [v(base + r * 8 * KB, [8, 512], BF16) for r in range(2)]
                    Xt = [v(base + 16 * KB + r * 4 * KB, [D]) for r in range(3)]
                    sgb = [v(base + 28 * KB + r * 2 * KB, [512]) for r in range(2)]
                    tmpb = [v(base + 32 * KB + r * 2 * KB, [512]) for r in range(2)]
                    cnt = 0
                    xcnt = 0
                    xl = [0]

                    def xload():
                        if xl[0] < len(act_tiles):
                            ii = act_tiles[xl[0]]
                            em.dma("sp", Xt[xl[0] % 3], XD[ii], reads=[bXD[ii]], writes=[B("xt", xl[0] % 3)])
                            xl[0] += 1
                    xload()
                    xload()
                    for ci, (c0, n) in enumerate(act_chunks):
                        tl = tiles_of(c0, n)
                        M = Mb[ci % 2]
                        bM = B("M", ci % 2)
                        rdh = [bHT[i] for i in tl]
                        rdy = [bYT[i] for i in tl]
                        for dc in range(8):
                            hf, q = dc // 4, dc % 4
                            pg = next_ps(0, 2)
                            pb = next_ps(2, 4)
                            for kc in range(8):
                                self.mm(PS[pg][:, 0:n], G[hf][0][:, kc, q * 128:(q + 1) * 128], HT[:, kc, c0:c0 + n],
                                        kc == 0, kc == 7, [G[hf][1]] + rdh, [PB[pg]])
                            for jj in range(4):
                                self.mm(PS[pb][:, 0:n], BR[0][:, jj, dc * 128:(dc + 1) * 128], YT[:, jj, c0:c0 + n],
                                        jj == 0, jj == 3, [BR[1]] + rdy, [PB[pb]])
                            sg = sgb[cnt % 2]
                            bsg = B("sg", cnt % 2)
                            cnt += 1
                            self.act(sg[:, 0:n], PS[pg][:, 0:n], AF.Sigmoid, [PB[pg]], [bsg])
                            self.tt("dve", M[:, dc, 0:n], sg[:, 0:n], PS[pb][:, 0:n], ALU.mult, [bsg, PB[pb]], [bM])
                        for t, i in enumerate(tl):
                            w = which(i)
                            xt = Xt[xcnt % 3]
                            bxt = B("xt", xcnt % 3)
                            xcnt += 1
                            for hf in range(2):
                                py = next_ps(4, 8)
                                for kc in range(8):
                                    self.mm(PS[py][:, :], M[:, kc, t * 128:(t + 1) * 128], WO[hf][0][:, kc, :],
                                            kc == 0, kc == 7, [bM, WO[hf][1]], [PB[py]])
                                tmp = tmpb[(cnt) % 2]
                                btmp = B("mtmp", cnt % 2)
                                cnt += 1
                                self.tt("dve", tmp, PS[py][:, :], GBv[w][:, hf * 512:(hf + 1) * 512], ALU.mult,
                                        [PB[py], bGB], [btmp])
                                self.tt("pool", xt[:, hf * 512:(hf + 1) * 512], xt[:, hf * 512:(hf + 1) * 512], tmp,
                                        ALU.add, [btmp, bxt], [bxt])
                            xload()
                            em.dma("sp", XD[i], xt, reads=[bxt], writes=[bXD[i]])
                        if s1gen is not None:
                            next(s1gen, None)
                    if s1gen is not None and k == 2:
                        for _ in s1gen:
                            pass
                    if pre_barrier is not None:
                        pre_barrier()
                    em.barrier()

                self.YT = v(OFF_X, [4, T], BF16)
                YT = self.YT
                IB = OFF_X + 18 * KB

                def mixer_a(Wa, pre_barrier=None):
                    AU, AV, WS = Wa
                    UTb = [v(IB + r * 4 * KB, [4, 512], BF16) for r in range(2)]
                    vgb = [v(IB + 8 * KB + r * 2 * KB, [512]) for r in range(4)]
                    vnb = [v(IB + 16 * KB + r * 2 * KB, [512]) for r in range(4)]
                    vbb = [v(IB + 24 * KB + r * KB, [512], BF16) for r in range(4)]
                    tcnt = 0
                    pendq = []

                    def spatial(pd):
                        i, t, r, UT, bUT = pd
                        vb, bvb = vbb[r], B("vb", r)
                        pm_ = next_ps(4, 8)
                        for g in range(4):
                            self.mm(PS[pm_][:, g * 128:(g + 1) * 128], vb[:, g * 128:(g + 1) * 128], WS[0][:, g, :],
                                    True, False, [bvb, WS[1]], [PB[pm_]])
                            self.mm(PS[pm_][:, g * 128:(g + 1) * 128], ones_bf[0:1, :], bs_row[0:1, g * 128:(g + 1) * 128],
                                    False, True, [bConst, B("bsrow")], [PB[pm_]])
                        self.tt("dve", YT[:, :, i * 128:(i + 1) * 128],
                                PS[pm_][:, :].rearrange("p (a b) -> p a b", b=128),
                                UT[:, :, t * 128:(t + 1) * 128], ALU.mult, [PB[pm_], bUT], [bYT[i]])

                    for ci, (c0, n) in enumerate(act_chunks):
                        tl = tiles_of(c0, n)
                        UT = UTb[ci % 2]
                        bUT = B("UT", ci % 2)
                        rdh = [bHT[i] for i in tl]
                        for g in range(4):
                            pi = next_ps(0, 2)
                            for kc in range(8):
                                self.mm(PS[pi][:, 0:n], AU[0][:, kc, g * 128:(g + 1) * 128], HT[:, kc, c0:c0 + n],
                                        kc == 0, kc == 7, [AU[1]] + rdh, [PB[pi]])
                            self.act(UT[:, g, 0:n], PS[pi][:, 0:n], AF.Gelu_apprx_tanh, [PB[pi]], [bUT])
                        for t, i in enumerate(tl):
                            r = tcnt % 4
                            tcnt += 1
                            pv = next_ps(2, 4)
                            for kc in range(8):
                                self.mm(PS[pv][:, :], HT[:, kc, i * 128:(i + 1) * 128], AV[0][:, kc, :],
                                        kc == 0, kc == 7, [AV[1], bHT[i]], [PB[pv]])
                            vg, vn, vb = vgb[r], vnb[r], vbb[r]
                            bvg, bvn, bvb, bst = B("vg", r), B("vn", r), B("vb", r), B("stA", r)
                            sa = stat[:, r, :]
                            self.act(vg, PS[pv][:, :], AF.Gelu_apprx_tanh, [PB[pv]], [bvg])
                            em.op("dve", lambda: nc.vector.bn_stats(out=sa[:, 0:6], in_=vg), [bvg], [bst])
                            em.op("dve", lambda: nc.vector.bn_aggr(out=sa[:, 6:8], in_=sa[:, 0:6]), [bst], [bst])
                            self.act(sa[:, 8:9], sa[:, 7:8], AF.Sqrt, [bst], [bst], bias=epsc, scale=1.0)
                            em.op("dve", lambda: nc.vector.reciprocal(out=sa[:, 9:10], in_=sa[:, 8:9]), [bst], [bst])
                            self.ts("dve", vn, vg, sa[:, 6:7], sa[:, 9:10], ALU.subtract, ALU.mult, [bvg, bst], [bvn])
                            self.tt("pool", vn, vn, alnG, ALU.mult, [bvn, B("aln")], [bvn])
                            self.tt("pool", vb, vn, alnB, ALU.add, [bvn, B("aln")], [bvb])
                            pendq.append((i, t, r, UT, bUT))
                            if len(pendq) > 2:
                                spatial(pendq.pop(0))
                    while pendq:
                        spatial(pendq.pop(0))
                    self.dump("ya%d" % l, YT, [128, 4, T], bYT, BF16)
                    if pre_barrier is not None:
                        pre_barrier()
                    em.barrier()

                def load_b():
                    WB = self.ring_load(wB_d[l], [8, 416])
                    offq, bWQ = self.ring_take()
                    WUQ = v(offq, [2, 768], BF16)
                    WUKV = v(offq + 6 * KB, [D], BF16)
                    em.dma("pool", WUQ, wuq_d[l], writes=[bWQ])
                    em.dma("pool", WUKV, wukv_d[l], writes=[bWQ])
                    return WB, offq, bWQ

                def mixer_b(Wb, pre_barrier=None):
                    WB, offq, bWQ = Wb
                    WUQ = v(offq, [2, 768], BF16)
                    WUQS = v(offq + 3 * KB, [2, 768], BF16)
                    WUKV = v(offq + 6 * KB, [D], BF16)
                    COS = v(IB, [T])
                    SIN = v(IB + 9 * KB, [T])
                    bCS = B("cossin")
                    em.dma("sp", COS, cossin_d[0], writes=[bCS])
                    em.dma("sp", SIN, cossin_d[1], writes=[bCS])
                    CQN = v(IB + 18 * KB, [2, T], BF16)
                    CKN = v(IB + 27 * KB, [T], BF16)
                    KROPE = v(IB + 31 * KB + 512, [T], BF16)
                    KRW = v(IB + 36 * KB, [8, 96], BF16)
                    KRS = v(IB + 37 * KB + 512, [8, 96], BF16)
                    cqf = v(IB + 39 * KB, [2, 512])
                    sq = v(IB + 43 * KB, [2, 512])
                    rstd = v(IB + 47 * KB, [512])
                    t1 = v(IB + 49 * KB, [512])
                    t2 = v(IB + 51 * KB, [512])
                    QTh = v(IB + 53 * KB, [T], BF16)
                    KTh = v(IB + 57 * KB + 512, [T], BF16)
                    VAb = [v(IB + 62 * KB + r * 2560, [NT, 65], BF16) for r in range(2)]
                    PTb = [v(IB + 67 * KB + r * KB, [512], BF16) for r in range(4)]
                    YBp = v(IB + 71 * KB, [NT, 128], BF16)
                    rden = v(IB + 76 * KB, [8])
                    bKR, bWQS = B("KRW"), B("WUQS")
                    self.memset("pool", KRW, 0.0, [bKR])
                    self.memset("pool", KRS, 0.0, [bKR])
                    self.memset("pool", WUQS, 0.0, [bWQS])
                    self.cp("pool", KRW[:, :, 64:96], WB[0][:, :, 384:416], [WB[1], bKR], [bKR])
                    for a in range(2):
                        o = 64 + a * 16
                        s_ = 384 + a * 16
                        self.ts("pool", KRS[:, :, o:o + 8], WB[0][:, :, s_ + 8:s_ + 16], -1.0, 0.0, ALU.mult, ALU.add,
                                [WB[1], bKR], [bKR])
                        self.cp("pool", KRS[:, :, o + 8:o + 16], WB[0][:, :, s_:s_ + 8], [WB[1], bKR], [bKR])
                    wq4 = WUQ.rearrange("p j (h e) -> p j h e", e=96)
                    ws4 = WUQS.rearrange("p j (h e) -> p j h e", e=96)
                    for j in range(2):
                        for a in range(2):
                            o = 64 + a * 16
                            self.ts("pool", ws4[:, j, :, o:o + 8], wq4[:, j, :, o + 8:o + 16], -1.0, 0.0,
                                    ALU.mult, ALU.add, [bWQ, bWQS], [bWQS])
                            self.cp("pool", ws4[:, j, :, o + 8:o + 16], wq4[:, j, :, o:o + 8], [bWQ, bWQS], [bWQS])
                    for r in range(2):
                        self.memset("pool", VAb[r][:, :, 64:65], 1.0, [B("VA", r)])
                    qg = ncol[:, 0:2]
                    kvg = ncol[:, 2:3]
                    bCQN = [B("CQN", c) for c in range(5)]
                    bCKN = [B("CKN", c) for c in range(5)]
                    bKRP = [B("KROPE", c) for c in range(5)]
                    for ci, (c0, n) in enumerate(CHUNKS):
                        tl = tiles_of(c0, n)
                        rdh = [bHT[i] for i in tl]
                        bcq, bsq, brs = B("cqf"), B("sq"), B("rstd")

                        def rms(nchunks, colbase, inv_n, gcol, dst_fn, bdst):
                            pss = []
                            for j in range(nchunks):
                                pi = next_ps(0, 4)
                                pss.append(pi)
                                for kc in range(8):
                                    self.mm(PS[pi][:, 0:n], WB[0][:, kc, colbase + j * 128:colbase + (j + 1) * 128],
                                            HT[:, kc, c0:c0 + n], kc == 0, kc == 7, [WB[1]] + rdh, [PB[pi]])
                                self.cp("act", cqf[:, j, 0:n], PS[pi][:, 0:n], [PB[pi]], [bcq])
                                self.act(sq[:, j, 0:n], PS[pi][:, 0:n], AF.Square, [PB[pi]], [bsq])
                            pq = next_ps(4, 6)
                            for j in range(nchunks):
                                self.mm(PS[pq][:, 0:n], ones_f, sq[:, j, 0:n], j == 0, j == nchunks - 1,
                                        [bConst, bsq], [PB[pq]])
                            self.act(rstd[:, 0:n], PS[pq][:, 0:n], AF.Sqrt, [PB[pq]], [brs], bias=epsc, scale=inv_n)
                            em.op("dve", lambda: nc.vector.reciprocal(out=rstd[:, 0:n], in_=rstd[:, 0:n]), [brs], [brs])
                            for j in range(nchunks):
                                self.stt(dst_fn(j), cqf[:, j, 0:n], gcol[:, j:j + 1], rstd[:, 0:n], ALU.mult, ALU.mult,
                                         [bcq, brs, bNcol], [bdst])

                        rms(2, 0, 1.0 / 256, qg, lambda j: CQN[:, j, c0:c0 + n], bCQN[ci])
                        rms(1, 256, 1.0 / 128, kvg, lambda j: CKN[:, c0:c0 + n], bCKN[ci])
                        pk = next_ps(0, 4)
                        pks = next_ps(0, 4)
                        for kc in range(8):
                            self.mm(PS[pk][0:96, 0:n], KRW[:, kc, :], HT[:, kc, c0:c0 + n], kc == 0, kc == 7,
                                    [bKR] + rdh, [PB[pk]])
                        for kc in range(8):
                            self.mm(PS[pks][0:96, 0:n], KRS[:, kc, :], HT[:, kc, c0:c0 + n], kc == 0, kc == 7,
                                    [bKR] + rdh, [PB[pks]])
                        bt1, bt2 = B("t1"), B("t2")
                        self.tt("dve", t1[64:96, 0:n], PS[pk][64:96, 0:n], COS[64:96, c0:c0 + n], ALU.mult,
                                [PB[pk], bCS], [bt1])
                        self.tt("dve", t2[64:96, 0:n], PS[pks][64:96, 0:n], SIN[64:96, c0:c0 + n], ALU.mult,
                                [PB[pks], bCS], [bt2])
                        self.tt("pool", KROPE[64:96, c0:c0 + n], t1[64:96, 0:n], t2[64:96, 0:n], ALU.add,
                                [bt1, bt2], [bKRP[ci]])
                    q_chunks = act_chunks
                    QT2 = v(IB + 39 * KB, [T], BF16)
                    KT2 = v(IB + 43 * KB + 512, [T], BF16)
                    alias_b = [B("cqf"), B("sq"), B("rstd")]
                    QTs = [QTh, QT2]
                    KTs = [KTh, KT2]
                    bQTs = [[B("QTh")], alias_b]
                    bKTs = [[B("KTh")], alias_b]
                    ptc = [0]

                    SBANKS = (0, 1, 6)
                    sbc = [0]

                    def prologue(h):
                        VA = VAb[h % 2]
                        bVA = B("VA", h % 2)
                        QT, KT = QTs[h % 2], KTs[h % 2]
                        bQT, bKT = bQTs[h % 2], bKTs[h % 2]
                        self.cp("pool", KT[64:96, :], KROPE[64:96, :], bKRP, bKT)
                        yield
                        for ci, (c0, n) in enumerate(CHUNKS):
                            pi = next_ps(7, 8)
                            self.mm(PS[pi][0:64, 0:n], WUKV[:, h * 128:h * 128 + 64], CKN[:, c0:c0 + n], True, True,
                                    [bWQ, bCKN[ci]], [PB[pi]])
                            self.cp("dve", KT[0:64, c0:c0 + n], PS[pi][0:64, 0:n], [PB[pi]], bKT)
                            yield
                        for g0 in range(0, NT, 8):
                            g1 = min(NT, g0 + 8)
                            pi = next_ps(7, 8)
                            for i in range(g0, g1):
                                self.mm(PS[pi][:, (i - g0) * 64:(i - g0 + 1) * 64], CKN[:, i * 128:(i + 1) * 128],
                                        WUKV[:, h * 128 + 64:h * 128 + 128], True, True,
                                        [bWQ, bCKN[min(4, (i + 2) // 4)]], [PB[pi]])
                            self.cp("dve", VA[:, g0:g1, 0:64],
                                    PS[pi][:, 0:(g1 - g0) * 64].rearrange("p (a b) -> p a b", b=64), [PB[pi]], [bVA])
                            yield
                        for ci, (c0, n) in enumerate(q_chunks):
                            cidx = CHUNKS.index((c0, n))
                            pq_ = 7
                            pqs = 7
                            for j in range(2):
                                self.mm(PS[pq_][0:96, 0:n], WUQ[:, j, h * 96:(h + 1) * 96], CQN[:, j, c0:c0 + n],
                                        j == 0, j == 1, [bWQ, bCQN[cidx]], [PB[pq_]])
                            bt1, bt2 = B("t1"), B("t2")
                            self.cp("dve", QT[0:64, c0:c0 + n], PS[pq_][0:64, 0:n], [PB[pq_]], bQT)
                            self.tt("dve", t1[64:96, 0:n], PS[pq_][64:96, 0:n], COS[64:96, c0:c0 + n], ALU.mult,
                                    [PB[pq_], bCS], [bt1])
                            yield
                            for j in range(2):
                                self.mm(PS[pqs][0:96, 0:n], WUQS[:, j, h * 96:(h + 1) * 96], CQN[:, j, c0:c0 + n],
                                        j == 0, j == 1, [bWQS, bWQ, bCQN[cidx]], [PB[pqs]])
                            self.tt("dve", t2[64:96, 0:n], PS[pqs][64:96, 0:n], SIN[64:96, c0:c0 + n], ALU.mult,
                                    [PB[pqs], bCS], [bt2])
                            self.tt("pool", QT[64:96, c0:c0 + n], t1[64:96, 0:n], t2[64:96, 0:n], ALU.add,
                                    [bt1, bt2], bQT)
                            yield

                    pgen = [None]
                    pit = [0]

                    def attend(h, ci, c0, n):
                        hsub = h % 2
                        VA = VAb[h % 2]
                        bVA = B("VA", h % 2)
                        QT, KT = QTs[h % 2], KTs[h % 2]
                        bQT, bKT = bQTs[h % 2], bKTs[h % 2]
                        keys = list(range(NT)) if c0 >= NCTX else [0, 1]
                        nq = n // 128
                        pend = []

                        def pv(pd):
                            ki, i, PT, bPT = pd
                            for qs in range(nq):
                                self.mm(PS[2 + qs][:, 0:65], PT[:, qs * 128:(qs + 1) * 128], VA[:, i, 0:65],
                                        ki == 0, ki == len(keys) - 1, [bPT, bVA], [PB[2 + qs]])
                        for ki, i in enumerate(keys):
                            ps_ = SBANKS[sbc[0] % 3]
                            sbc[0] += 1
                            self.mm(PS[ps_][:, 0:n], KT[0:96, i * 128:(i + 1) * 128], QT[0:96, c0:c0 + n],
                                    True, True, bKT + bQT, [PB[ps_]])
                            if len(pend) >= 2:
                                pv(pend.pop(0))
                            PT = PTb[ptc[0] % 4]
                            bPT = B("PT", ptc[0] % 4)
                            ptc[0] += 1
                            self.act(PT[:, 0:n], PS[ps_][:, 0:n], AF.Exp, [PB[ps_]], [bPT], scale=ATT_SCALE)
                            pend.append((ki, i, PT, bPT))
                            pit[0] += 1
                            if pgen[0] is not None and pit[0] % 3 == 0:
                                next(pgen[0], None)
                        while pend:
                            pv(pend.pop(0))
                        for qs in range(nq):
                            iq = c0 // 128 + qs
                            rslot = (qs % 4) + 4 * (ci % 2)
                            rd = rden[:, rslot:rslot + 1]
                            brd = B("rden", rslot)
                            em.op("dve", lambda: nc.vector.reciprocal(out=rd, in_=PS[2 + qs][:, 64:65]),
                                  [PB[2 + qs]], [brd])
                            self.ts("dve", YBp[:, iq, hsub * 64:(hsub + 1) * 64], PS[2 + qs][:, 0:64], rd, None,
                                    ALU.mult, ALU.bypass, [PB[2 + qs], brd], [B("YBp", iq)])

                    for _ in prologue(0):
                        pass
                    for h in range(8):
                        jpair, hsub = h // 2, h % 2
                        pgen[0] = prologue(h + 1) if h + 1 < 8 else None
                        for ci, (c0, n) in enumerate(q_chunks):
                            attend(h, ci, c0, n)
                        if pgen[0] is not None:
                            for _ in pgen[0]:
                                pass
                        if hsub == 1:
                            for g0 in range(0, NT, 4):
                                pi = next_ps(7, 8)
                                psb16 = PS[pi][:, 0:256].bitcast(BF16)
                                tls = [i for i in range(g0, min(NT, g0 + 4)) if i in act_tiles]
                                for i in tls:
                                    em.op("pe", lambda: nc.tensor.transpose(
                                        out=psb16[:, (i - g0) * 128:(i - g0 + 1) * 128], in_=YBp[:, i, :],
                                        identity=ident_bf), [B("YBp", i), bConst], [PB[pi]])
                                for i in tls:
                                    self.cp("act", YT[:, jpair, i * 128:(i + 1) * 128],
                                            psb16[:, (i - g0) * 128:(i - g0 + 1) * 128], [PB[pi]], [bYT[i]])
                    self.dump("yb%d" % l, YT, [128, 4, T], bYT, BF16)
                    if pre_barrier is not None:
                        pre_barrier()
                    em.barrier()

                def load_c():
                    CX = self.ring_load(wC_d[l, 0], [8, 512])
                    CG = self.ring_load(wC_d[l, 1], [8, 512])
                    WRI = self.ring_load(wri_d[l], [16, 128])
                    return CX, CG, WRI

                def mixer_c(Wc, pre_barrier=None):
                    CX, CG, WRI = Wc
                    XR = v(IB, [T])
                    XC = v(IB + 9 * KB, [T])
                    XCB = v(IB + 18 * KB, [T], BF16)
                    Ad = [v(IB + 23 * KB, [T]), v(IB + 41 * KB, [T])]
                    Bd = [v(IB + 32 * KB, [T]), v(IB + 50 * KB, [T])]
                    Hf = v(IB + 59 * KB, [T])
                    Hb = v(IB + 68 * KB, [T])
                    tb = [v(IB + 77 * KB + r * 2 * KB, [512]) for r in range(2)]
                    lam = ncol[:, 40:48]
                    bcc = B("ccol")
                    self.act(etmp, lam, AF.Exp, [bNcol], [bcc], scale=-1.0)
                    self.act(etmp, etmp, AF.Ln, [bcc], [bcc], bias=onec, scale=1.0)
                    self.ts("dve", ccol, etmp, -8.0, None, ALU.mult, ALU.bypass, [bcc], [bcc])
                    convw = ncol[:, 4:20].rearrange("p (j k) -> p j k", k=4)
                    convb = ncol[:, 20:24]
                    bri = ncol[:, 24:40]
                    bXR, bXC, bXCB = B("XR"), B("XC"), B("XCB")
                    bAd = [B("Ad", 0), B("Ad", 1)]
                    bBd = [B("Bd", 0), B("Bd", 1)]
                    bHf, bHb = B("Hf"), B("Hb")
                    tmps = [Hf, Hb]
                    btmps = [bHf, bHb]
                    tcn = [0]

                    def st1a(j):
                        for ci, (c0, n) in enumerate(CHUNKS):
                            tl = tiles_of(c0, n)
                            pi = next_ps(0, 4)
                            for kc in range(8):
                                self.mm(PS[pi][:, 0:n], CX[0][:, kc, j * 128:(j + 1) * 128], HT[:, kc, c0:c0 + n],
                                        kc == 0, kc == 7, [CX[1]] + [bHT[i] for i in tl], [PB[pi]])
                            self.cp("dve", XR[:, c0:c0 + n], PS[pi][:, 0:n], [PB[pi]], [bXR])
                        for (s_, e_) in ((0, NCTX), (NCTX, T)):
                            self.ts("dve", XC[:, s_:e_], XR[:, s_:e_], convw[:, j, 2:3], convb[:, j:j + 1], ALU.mult, ALU.add,
                                    [bXR, bNcol], [bXC])
                            self.stt(XC[:, s_ + 1:e_], XR[:, s_:e_ - 1], convw[:, j, 1:2], XC[:, s_ + 1:e_], ALU.mult, ALU.add,
                                     [bXR, bXC, bNcol], [bXC])
                            self.stt(XC[:, s_ + 2:e_], XR[:, s_:e_ - 2], convw[:, j, 0:1], XC[:, s_ + 2:e_], ALU.mult, ALU.add,
                                     [bXR, bXC, bNcol], [bXC])
                            self.stt(XC[:, s_:e_ - 1], XR[:, s_ + 1:e_], convw[:, j, 3:4], XC[:, s_:e_ - 1], ALU.mult, ALU.add,
                                     [bXR, bXC, bNcol], [bXC])
                        self.cp("pool", XCB, XC, [bXC], [bXCB])

                    def st1b(j):
                        for d in range(2):
                            ir = (d * 2 + 0) * 4 + j
                            ii = (d * 2 + 1) * 4 + j
                            for ci, (c0, n) in enumerate(CHUNKS):
                                pr = next_ps(4, 8)
                                pi_ = next_ps(4, 8)
                                self.mm(PS[pr][:, 0:n], WRI[0][:, ir, :], XCB[:, c0:c0 + n], True, True,
                                        [WRI[1], bXCB], [PB[pr]])
                                self.mm(PS[pi_][:, 0:n], WRI[0][:, ii, :], XCB[:, c0:c0 + n], True, True,
                                        [WRI[1], bXCB], [PB[pi_]])
                                self.act(Ad[d][:, c0:c0 + n], PS[pr][:, 0:n], AF.Sigmoid, [PB[pr], bNcol], [bAd[d]],
                                         bias=bri[:, ir:ir + 1], scale=1.0)
                                self.act(Bd[d][:, c0:c0 + n], PS[pi_][:, 0:n], AF.Sigmoid, [PB[pi_], bNcol], [bBd[d]],
                                         bias=bri[:, ii:ii + 1], scale=1.0)
                        for d in range(2):
                            self.tt("pool", Bd[d], Bd[d], XC, ALU.mult, [bBd[d], bXC], [bBd[d]])

                    def st2(j, mid=None):
                        for d in range(2):
                            self.act(Ad[d], Ad[d], AF.Exp, [bAd[d], bcc], [bAd[d]],
                                     scale=ccol[:, d * 4 + j:d * 4 + j + 1])
                        for d in range(2):
                            self.tt("dve", tmps[d], Ad[d], Ad[d], ALU.mult, [bAd[d]], [btmps[d]])
                        if mid is not None:
                            mid()
                        for d in range(2):
                            self.act(tmps[d], tmps[d], AF.Sqrt, [btmps[d]], [btmps[d]], bias=onec, scale=-1.0)
                        for d in range(2):
                            self.tt("pool", Bd[d], Bd[d], tmps[d], ALU.mult, [bBd[d], btmps[d]], [bBd[d]])
                        em.op("dve", lambda: nc.vector.tensor_tensor_scan(
                            out=Hf[:, 0:T], data0=Ad[0][:, 0:T], data1=Bd[0][:, 0:T], initial=0.0,
                            op0=ALU.mult, op1=ALU.add), [bAd[0], bBd[0]], [bHf])
                        em.op("dve", lambda: nc.vector.tensor_tensor_scan(
                            out=Hb[:, 0:NCTX][:, ::-1], data0=Ad[1][:, 0:NCTX][:, ::-1],
                            data1=Bd[1][:, 0:NCTX][:, ::-1], initial=0.0,
                            op0=ALU.mult, op1=ALU.add), [bAd[1], bBd[1]], [bHb])
                        em.op("dve", lambda: nc.vector.tensor_tensor_scan(
                            out=Hb[:, NCTX:T][:, ::-1], data0=Ad[1][:, NCTX:T][:, ::-1],
                            data1=Bd[1][:, NCTX:T][:, ::-1], initial=Hb[:, 0:1],
                            op0=ALU.mult, op1=ALU.add), [bAd[1], bBd[1], bHb], [bHb])
                        self.tt("pool", Hf, Hf, Hb, ALU.add, [bHf, bHb], [bHf])
                        for ci, (c0, n) in enumerate(act_chunks):
                            tl = tiles_of(c0, n)
                            pi = next_ps(0, 4)
                            for kc in range(8):
                                self.mm(PS[pi][:, 0:n], CG[0][:, kc, j * 128:(j + 1) * 128], HT[:, kc, c0:c0 + n],
                                        kc == 0, kc == 7, [CG[1]] + [bHT[i] for i in tl], [PB[pi]])
                            tg = tb[tcn[0] % 2]
                            btg = B("tb", tcn[0] % 2)
                            tcn[0] += 1
                            self.act(tg[:, 0:n], PS[pi][:, 0:n], AF.Gelu_apprx_tanh, [PB[pi]], [btg])
                            self.tt("dve", YT[:, j, c0:c0 + n], tg[:, 0:n], Hf[:, c0:c0 + n], ALU.mult, [btg, bHf],
                                    [bYT[i] for i in tl])

                    st1a(0)
                    st1b(0)
                    for j in range(4):
                        st2(j, (lambda jj=j: st1a(jj + 1)) if j + 1 < 4 else None)
                        if j + 1 < 4:
                            st1b(j + 1)
                    self.dump("yc%d" % l, YT, [128, 4, T], bYT, BF16)
                    if pre_barrier is not None:
                        pre_barrier()
                    em.barrier()

                def load_expert(e_):
                    return (self.ring_load(wg_d[l, e_], [8, 512]), self.ring_load(wu_d[l, e_], [8, 512]),
                            self.ring_load(wd_d[l, e_], [4, D]))
                stop = getattr(self, "stop", None)
                hold = {"A": hold0}

                def pf(name, fn):
                    def go():
                        hold[name] = fn()
                    return go
                mixer_a(hold.pop("A"), pf("m0", lambda: load_merge(0)))
                if stop == "A":
                    merge(0, hold.pop("m0"))
                else:
                    merge(0, hold.pop("m0"), pf("B", load_b))
                    mixer_b(hold.pop("B"), pf("m1", lambda: load_merge(1)))
                    if stop == "B":
                        merge(1, hold.pop("m1"))
                    else:
                        merge(1, hold.pop("m1"), pf("C", load_c))
                        mixer_c(hold.pop("C"), pf("m2", lambda: load_merge(2)))
                        merge(2, hold.pop("m2"), pf("E0", lambda: load_expert(0)) if stop is None else None)

                def layer_norm(pidx, tiles):
                    Lg = v(OFF_PH, [D])
                    Lb = v(OFF_PH + 4 * KB, [D])
                    bL = B("lnp")
                    em.dma("sp", Lg, lnp_d[l, pidx], writes=[bL])
                    em.dma("sp", Lb, lnp_d[l, pidx + 1], writes=[bL])
                    for n_, i in enumerate(tiles):
                        r = n_ % 4
                        sa = stat[:, r, :]
                        bst = B("stL", r)
                        for hf in range(2):
                            em.op("dve", lambda: nc.vector.bn_stats(out=sa[:, hf * 6:(hf + 1) * 6],
                                                                    in_=Xs[:, i, hf * 512:(hf + 1) * 512]),
                                  [bX[i]], [bst])
                        em.op("dve", lambda: nc.vector.bn_aggr(out=sa[:, 12:14], in_=sa[:, 0:12]), [bst], [bst])
                        self.act(sa[:, 14:15], sa[:, 13:14], AF.Sqrt, [bst], [bst], bias=epsc, scale=1.0)
                        em.op("dve", lambda: nc.vector.reciprocal(out=sa[:, 15:16], in_=sa[:, 14:15]), [bst], [bst])
                        self.ts("dve", sa[:, 14:15], sa[:, 12:13], sa[:, 15:16], -1.0, ALU.mult, ALU.mult, [bst], [bst])
                        self.act(Xs[:, i, :], Xs[:, i, :], AF.Identity, [bX[i], bst], [bX[i]],
                                 bias=sa[:, 14:15], scale=sa[:, 15:16])
                        self.tt("dve", Xs[:, i, :], Xs[:, i, :], Lg, ALU.mult, [bX[i], bL], [bX[i]])
                        self.tt("pool", Xs[:, i, :], Xs[:, i, :], Lb, ALU.add, [bX[i], bL], [bX[i]])

                for i in act_tiles:
                    em.dma("sp", Xs[:, i, :], XD[i], reads=[bXD[i]], writes=[bX[i]])
                layer_norm(0, act_tiles)
                self.dump("x1_%d" % l, Xs, [128, NT, D], bX)
                if stop in ("A", "B", "C"):
                    return act_tiles

                self.F32T = v(OFF_PH + 8 * KB, [16, 128])
                gate_bcast(colmods, bCol, 5)
                build_T(True)
                RB = OFF_PH + 16 * KB
                SC = v(RB, [NT * 16])
                SEL = v(RB + 1152, [NT * 16])
                PR = v(RB + 2304, [6, NT * 4])
                GS = v(RB + 4096, [NT * 4])
                GM = v(RB + 4416, [NT])
                OG = v(RB + 4512, [NT * 4])
                PEN = v(RB + 4800, [NT * 4])
                SM = v(RB + 5120, [NT * 16])
                CNT = v(RB + 6272, [NT * 16])
                CMP = v(RB + 7424, [NT * 4])
                WM = v(RB + 7712, [NT * 16])
                DEN = v(RB + 8864, [NT])
                bR = B("route")
                bCW = B("CW")
                self.act(SC, PS[6][:, 0:NT * 16], AF.Sigmoid, [PB[6]], [bR])
                self.tt("dve", SEL, SC, rbias, ALU.add, [bR, bConst], [bR])
                sel4 = SEL.rearrange("p (a b) -> p a b", b=4)
                pairs = [(0, 1), (0, 2), (0, 3), (1, 2), (1, 3), (2, 3)]
                for pi_, (a, b_) in enumerate(pairs):
                    self.tt("dve", PR[:, pi_, :], sel4[:, :, a], sel4[:, :, b_], ALU.add, [bR], [bR])
                self.tt("dve", GS, PR[:, 0, :], PR[:, 1, :], ALU.max, [bR], [bR])
                for pi_ in range(2, 6):
                    self.tt("dve", GS, GS, PR[:, pi_, :], ALU.max, [bR], [bR])
                gs3 = GS.rearrange("p (a b) -> p a b", b=4)
                self.tt("dve", GM, gs3[:, :, 0], gs3[:, :, 1], ALU.max, [bR], [bR])
                for g in range(2, 4):
                    self.tt("dve", GM, GM, gs3[:, :, g], ALU.max, [bR], [bR])
                og3 = OG.rearrange("p (a b) -> p a b", b=4)
                for g in range(4):
                    self.tt("dve", og3[:, :, g], gs3[:, :, g], GM, ALU.is_ge, [bR], [bR])
                self.ts("dve", PEN, OG, BIG, -BIG, ALU.mult, ALU.add, [bR], [bR])
                sm4 = SM.rearrange("p (a b) -> p a b", b=4)
                cnt4 = CNT.rearrange("p (a b) -> p a b", b=4)
                for e_ in range(4):
                    self.tt("dve", sm4[:, :, e_], sel4[:, :, e_], PEN, ALU.add, [bR], [bR])
                for e_ in range(4):
                    first = True
                    for e2 in range(4):
                        if e2 == e_:
                            continue
                        if first:
                            self.tt("dve", cnt4[:, :, e_], sm4[:, :, e2], sm4[:, :, e_], ALU.is_gt, [bR], [bR])
                            first = False
                        else:
                            self.tt("dve", CMP, sm4[:, :, e2], sm4[:, :, e_], ALU.is_gt, [bR], [bR])
                            self.tt("dve", cnt4[:, :, e_], cnt4[:, :, e_], CMP, ALU.add, [bR], [bR])
                wm4 = WM.rearrange("p (a b) -> p a b", b=4)
                sc4 = SC.rearrange("p (a b) -> p a b", b=4)
                for e_ in range(4):
                    self.ts("dve", CMP, cnt4[:, :, e_], 1.5, None, ALU.is_lt, ALU.bypass, [bR], [bR])
                    self.tt("dve", CMP, CMP, OG, ALU.mult, [bR], [bR])
                    self.tt("dve", wm4[:, :, e_], sc4[:, :, e_], CMP, ALU.mult, [bR], [bR])
                wm16 = WM.rearrange("p (a b) -> p a b", b=16)
                em.op("dve", lambda: nc.vector.tensor_reduce(out=DEN, in_=wm16, axis=AX.X, op=ALU.add), [bR], [bR])
                em.op("dve", lambda: nc.vector.reciprocal(out=DEN, in_=DEN), [bR], [bR])
                for e_ in range(16):
                    self.stt(CWt[:, :, e_], wm16[:, :, e_], 2.5, DEN, ALU.mult, ALU.mult, [bR], [bCW])
                self.dump("cw%d" % l, CWt, [128, NT, 16], [bCW])

                MB = OFF_PH + 8 * KB
                ATb = [v(MB + r * 4 * KB, [4, 512], BF16) for r in range(2)]
                bATl = [[B("f32t", r, kc) for kc in range(8)] for r in range(2)]
                sgm = [v(RB + r * 2 * KB, [512]) for r in range(2)]
                tmm = [v(RB + 4 * KB + r * 2 * KB, [512]) for r in range(2)]
                claimed = set()

                def claim(key):
                    if key in claimed:
                        return []
                    claimed.add(key)
                    return [bR]

                cnt = 0
                Wn = hold.pop("E0")
                for e_ in range(NEXP):
                    WG, WU, WD = Wn
                    if e_ + 1 < NEXP:
                        Wn = load_expert(e_ + 1)
                    for ci, (c0, n) in enumerate(act_chunks):
                        tl = tiles_of(c0, n)
                        rdh = [bHT[i] for i in tl]
                        AT = ATb[(e_ * 5 + ci) % 2]
                        bATs = bATl[(e_ * 5 + ci) % 2]
                        for j in range(4):
                            pg = next_ps(0, 2)
                            pu = next_ps(2, 4)
                            for kc in range(8):
                                self.mm(PS[pg][:, 0:n], WG[0][:, kc, j * 128:(j + 1) * 128], HT[:, kc, c0:c0 + n],
                                        kc == 0, kc == 7, [WG[1]] + rdh, [PB[pg]])
                            for kc in range(8):
                                self.mm(PS[pu][:, 0:n], WU[0][:, kc, j * 128:(j + 1) * 128], HT[:, kc, c0:c0 + n],
                                        kc == 0, kc == 7, [WU[1]] + rdh, [PB[pu]])
                            sg = sgm[cnt % 2]
                            bsg = B("sgm", cnt % 2)
                            ck = claim(("sgm", cnt % 2))
                            cnt += 1
                            self.act(sg[:, 0:n], PS[pg][:, 0:n], AF.Silu, [PB[pg]], [bsg] + ck)
                            self.tt("dve", AT[:, j, 0:n], sg[:, 0:n], PS[pu][:, 0:n], ALU.mult, [bsg, PB[pu]], bATs)
                        for t, i in enumerate(tl):
                            w = which(i)
                            for hf in range(2):
                                py = next_ps(4, 8)
                                for j in range(4):
                                    self.mm(PS[py][:, :], AT[:, j, t * 128:(t + 1) * 128],
                                            WD[0][:, j, hf * 512:(hf + 1) * 512], j == 0, j == 3,
                                            bATs + [WD[1]], [PB[py]])
                                tmp = tmm[cnt % 2]
                                btmp = B("tmm", cnt % 2)
                                ck = claim(("tmm", cnt % 2))
                                cnt += 1
                                self.stt(tmp, PS[py][:, :], CWt[:, i, e_:e_ + 1], GBv[w][:, hf * 512:(hf + 1) * 512],
                                         ALU.mult, ALU.mult, [PB[py], bCW, bGB], [btmp] + ck)
                                self.tt("pool", Xs[:, i, hf * 512:(hf + 1) * 512], Xs[:, i, hf * 512:(hf + 1) * 512],
                                        tmp, ALU.add, [btmp, bX[i]], [bX[i]])
                layer_norm(2, act_tiles)
                self.dump("x2_%d" % l, Xs, [128, NT, D], bX)
                return act_tiles

            epsc = v(OFF_CONST + 2304, [1])
            onec = v(OFF_CONST + 2308, [1])
            self.memset("dve", epsc, EPS, [bConst])
            self.memset("dve", onec, 1.0, [bConst])

            for l in range(L):
                layer(l)

            for i in range(2, NT):
                em.dma("sp", out_d[(i - 2) * 128:(i - 1) * 128, :], Xs[:, i, :], reads=[bX[i]])
            em.finish("sp")
            self.stats = dict(ninst=dict(em.ninst), nwait=em.nwait, nsem=em.nsem)
        return nc


def _rope_tables():
    rows = NLAT // 64
    row = np.repeat(np.arange(rows, dtype=np.float32), 64)
    col = np.tile(np.arange(64, dtype=np.float32), rows)
    n_freq = 8
    inv = (np.float32(10000.0) ** (-np.arange(n_freq, dtype=np.float32) / np.float32(n_freq))).astype(np.float32)
    ang = np.stack([row[:, None] * inv, col[:, None] * inv], axis=1).astype(np.float32)
    cos = np.cos(ang).astype(np.float32)
    sin = np.sin(ang).astype(np.float32)
    tab = np.zeros((2, 128, T), np.float32)
    tab[0] = 1.0
    for a in range(2):
        for hf in range(2):
            r0 = 64 + a * 16 + hf * 8
            tab[0, r0:r0 + 8, NCTX:] = cos[:, a, :].T
            tab[1, r0:r0 + 8, NCTX:] = sin[:, a, :].T
    return tab


def _kc(w):
    *lead, K, N = w.shape
    w = w.reshape(*lead, K // 128, 128, N)
    nd = w.ndim
    perm = list(range(nd - 3)) + [nd - 2, nd - 3, nd - 1]
    return np.ascontiguousarray(w.transpose(perm))


def _col(vv):
    *lead, n = vv.shape
    return np.ascontiguousarray(np.swapaxes(vv.reshape(*lead, n // 128, 128), -1, -2))


def prep_shared(inp):
    f = lambda a: np.ascontiguousarray(np.asarray(a, dtype=np.float32))
    sh = {}
    sh["ident"] = np.eye(128, dtype=np.float32)
    sh["cossin"] = _rope_tables()
    sh["rbias"] = np.ascontiguousarray(np.broadcast_to(np.tile(f(inp["router_bias"]), NT)[None, :], (128, NT * 16)))
    sh["wrouter"] = _kc(f(inp["w_router"]))
    w_ada = f(inp["w_ada"])
    wa = _kc(w_ada)
    sh["w_ada_h"] = np.ascontiguousarray(wa.reshape(DEPTH, 128, 8, 12, 512).transpose(0, 3, 1, 2, 4))
    sh["b_ada_col"] = _col(f(inp["b_ada"]))
    w_in = _kc(f(inp["w_in"]))
    sh["wA"] = np.ascontiguousarray(np.stack([w_in[..., 0:512], w_in[..., 512:1024]], axis=1))
    sh["wB"] = np.ascontiguousarray(w_in[..., 1024:1440])
    sh["wC"] = np.ascontiguousarray(np.stack([w_in[..., 1440:1952], w_in[..., 1952:2464]], axis=1))
    sh["wG"] = np.ascontiguousarray(np.stack([w_in[..., 2464 + s * 512:2464 + (s + 1) * 512] for s in range(6)], axis=1))
    aln = np.stack([f(inp["a_ln_g"]), f(inp["a_ln_b"])], axis=1)
    sh["aln"] = np.ascontiguousarray(np.broadcast_to(aln[:, :, None, :], (DEPTH, 2, 128, 512)))
    sh["wsT"] = np.ascontiguousarray(f(inp["w_s"]).transpose(0, 3, 1, 2))
    sh["bs_row"] = np.ascontiguousarray(f(inp["b_s"]).reshape(DEPTH, 1, 512))
    ncol = np.zeros((DEPTH, 128, 48), np.float32)
    ncol[:, :, 0:2] = _col(f(inp["q_norm_g"]))
    ncol[:, :, 2:3] = _col(f(inp["kv_norm_g"]))
    cw = f(inp["conv_w"])
    ncol[:, :, 4:20] = np.ascontiguousarray(cw.reshape(DEPTH, 4, 4, 128).transpose(0, 3, 2, 1)).reshape(DEPTH, 128, 16)
    ncol[:, :, 20:24] = _col(f(inp["conv_b"]))
    br = _col(f(inp["b_r"]))
    bi = _col(f(inp["b_i"]))
    for d in range(2):
        ncol[:, :, 24 + (d * 2 + 0) * 4:24 + (d * 2 + 0) * 4 + 4] = br[:, d]
        ncol[:, :, 24 + (d * 2 + 1) * 4:24 + (d * 2 + 1) * 4 + 4] = bi[:, d]
    lam = _col(f(inp["lru_lambda"]))
    for d in range(2):
        ncol[:, :, 40 + d * 4:44 + d * 4] = lam[:, d]
    sh["ncol"] = ncol
    sh["wuq"] = _kc(f(inp["w_uq"]))
    sh["wukv"] = f(inp["w_ukv"])
    wri = np.zeros((DEPTH, 128, 16, 128), np.float32)
    for d in range(2):
        for ri, name in enumerate(("w_r", "w_i")):
            w = f(inp[name])[:, d]
            for j in range(4):
                for bb in range(2):
                    wri[:, bb * 64:(bb + 1) * 64, (d * 2 + ri) * 4 + j, bb * 64:(bb + 1) * 64] = w[:, 2 * j + bb]
    sh["wri"] = wri
    sh["wbr"] = _kc(f(inp["w_br"]))
    wo = _kc(f(inp["w_o"]))
    sh["wo"] = np.ascontiguousarray(np.stack([wo[..., 0:512], wo[..., 512:1024]], axis=1))
    lnp = np.stack([f(inp["ln1_g"]), f(inp["ln1_b"]), f(inp["ln2_g"]), f(inp["ln2_b"])], axis=1)
    sh["lnp"] = np.ascontiguousarray(np.broadcast_to(lnp[:, :, None, :], (DEPTH, 4, 128, D)))
    sh["wg"] = _kc(f(inp["w_gate"]))
    sh["wu"] = _kc(f(inp["w_up"]))
    sh["wd"] = _kc(f(inp["w_down"]))
    return sh


def prep_core(inp, b):
    f = lambda a: np.asarray(a, dtype=np.float32)
    xin = np.ascontiguousarray(np.concatenate([f(inp["ctx"])[b], f(inp["x"])[b]], axis=0))
    condT = np.concatenate([_col(f(inp["c"])[b]), _col(f(inp["c_ctx"]))], axis=1)
    return {"xin": xin, "condT": np.ascontiguousarray(condT)}


_CACHE = {}


def kernel(**inputs):
    nb = inputs["x"].shape[0]
    if "prog" not in _CACHE:
        p = Prog()
        p.build()
        _CACHE["prog"] = p
    p = _CACHE["prog"]
    sh = prep_shared(inputs)
    in_maps = []
    for b in range(nb):
        m = dict(sh)
        m.update(prep_core(inputs, b))
        in_maps.append(m)
    res = run_bass_kernel_spmd(p.nc, in_maps, core_ids=list(range(nb)))
    out = np.stack([np.asarray(r["out"], dtype=np.float32) for r in res.results], axis=0)
    return out
```

```python
import numpy as np
from contextlib import ExitStack
import concourse.bass as bass
import concourse.mybir as mybir
from concourse.bass_utils import run_bass_kernel_spmd

F32 = mybir.dt.float32
BF16 = mybir.dt.bfloat16
AF = mybir.ActivationFunctionType
ALU = mybir.AluOpType
AX = mybir.AxisListType

D = 1024
DEPTH = 4
NCTX = 256
NLAT = 2048
T = NCTX + NLAT
NT = T // 128
ALPHA = (2.0 * DEPTH) ** 0.25
EPS = 1e-6
NEXP = 16
KB = 1024
CHUNKS = [(0, 256), (256, 512), (768, 512), (1280, 512), (1792, 512)]
ATT_SCALE = 96 ** -0.5
BIG = 1.0e4

OFF_CONST = 0
OFF_GB = 12 * KB
OFF_HT = 20 * KB
OFF_RING = 56 * KB
NSLOT = 6
SLOT = 8 * KB
OFF_X = 104 * KB
OFF_PH = 176 * KB
ARENA = 204 * KB


class Buf:
    __slots__ = ("name", "w", "r", "x")

    def __init__(self, name=""):
        self.name = name
        self.w = None
        self.r = {}
        self.x = False


class Em:
    SEM_LIMIT = 30000
    NDMA = 8

    def __init__(self, nc, stack):
        self.nc = nc
        self.stack = stack
        self.eng = {"pe": nc.tensor, "act": nc.scalar, "dve": nc.vector,
                    "pool": nc.gpsimd, "sp": nc.sync}
        self.cur = {}
        self.nsem = 0
        self.waited = {e: {} for e in self.eng}
        self.last = {}
        for e in self.eng:
            self._new_sem(e)
        self.dq = {}
        self.dqi = {}
        self.dlast = {}
        for q in ("sp", "pool"):
            self.dq[q] = [self._alloc("d%s%d" % (q, i)) for i in range(self.NDMA)]
            self.dqi[q] = 0
        self.bufs = {}
        self.ninst = {e: 0 for e in self.eng}
        self.nwait = 0

    def _alloc(self, name):
        self.nsem += 1
        key = "%s_%d" % (name, self.nsem)
        sem = self.stack.enter_context(self.nc.semaphore(key))
        return [key, sem, 0]

    def _new_sem(self, e):
        self.cur[e] = self._alloc("s" + e)

    def B(self, *key):
        b = self.bufs.get(key)
        if b is None:
            b = Buf(str(key))
            self.bufs[key] = b
        return b

    def _deps(self, reads, writes):
        deps = {}

        def add(t):
            if t is None:
                return
            k = t[0]
            if k not in deps or deps[k][2] < t[2]:
                deps[k] = t
        for b in reads:
            add(b.w)
            if b.x:
                for t in b.r.values():
                    add(t)
        for b in writes:
            add(b.w)
            for t in b.r.values():
                add(t)
        return deps

    def _wait(self, e, deps, skip_self=False):
        w = self.waited[e]
        for k, (_, sem, v) in deps.items():
            if w.get(k, 0) >= v:
                continue
            if skip_self and k == self.cur[e][0]:
                continue
            self.eng[e].wait_ge(sem, v)
            self.nwait += 1
            w[k] = v

    def _commit(self, ticket, reads, writes):
        for b in writes:
            b.w = ticket
            b.r = {}
        for b in reads:
            b.r[ticket[0]] = ticket

    def op(self, e, fn, reads=(), writes=()):
        deps = self._deps(reads, writes)
        self._wait(e, deps, skip_self=(e == "pe"))
        ins = fn()
        c = self.cur[e]
        c[2] += 1
        ins.then_inc(c[1], 1)
        ticket = (c[0], c[1], c[2])
        self.last[e] = ticket
        self._commit(ticket, reads, writes)
        self.ninst[e] += 1
        if c[2] >= self.SEM_LIMIT:
            self._new_sem(e)
        return ticket

    def dma(self, q, out, in_, reads=(), writes=()):
        deps = self._deps(reads, writes)
        i = self.dqi[q]
        self.dqi[q] = i + 1
        si = i % self.NDMA
        slot = self.dq[q][si]
        if slot[2] > 0:
            t = (slot[0], slot[1], slot[2])
            if slot[0] not in deps or deps[slot[0]][2] < slot[2]:
                deps[slot[0]] = t
        self._wait(q, deps)
        ins = self.eng[q].dma_start(out=out, in_=in_)
        slot[2] += 16
        ins.then_inc(slot[1], 16)
        ticket = (slot[0], slot[1], slot[2])
        self.dlast[(q, si)] = ticket
        self._commit(ticket, reads, writes)
        if slot[2] >= self.SEM_LIMIT:
            self.dq[q][si] = self._alloc("d%s" % q)
        return ticket

    def barrier(self):
        tickets = list(self.last.values()) + list(self.dlast.values())
        for e in self.eng:
            deps = {}
            for t in tickets:
                if t[0] not in deps or deps[t[0]][2] < t[2]:
                    deps[t[0]] = t
            self._wait(e, deps)

    def finish(self, e="sp"):
        tickets = list(self.last.values()) + list(self.dlast.values())
        deps = {}
        for t in tickets:
            if t[0] not in deps or deps[t[0]][2] < t[2]:
                deps[t[0]] = t
        self._wait(e, deps)


class Prog:
    def __init__(self, nl=DEPTH, dbg=()):
        self.nl = nl
        self.dbg = set(dbg)
        self.nc = bass.Bass("TRN2", target_bir_lowering=False)
        self.din = {}
        self.dout = {}

    def inp(self, name, shape):
        self.din[name] = self.nc.dram_tensor(name, list(shape), F32, kind="ExternalInput").ap()
        return self.din[name]

    def outp(self, name, shape, dt=F32):
        self.dout[name] = self.nc.dram_tensor(name, list(shape), dt, kind="ExternalOutput").ap()
        return self.dout[name]

    def v(self, off, shape, dt=F32):
        n = 1
        for s in shape:
            n *= s
        esz = 4 if dt == F32 else 2
        nb = n * esz
        assert off % 4 == 0 and nb % 4 == 0, (off, shape)
        assert off + nb <= ARENA, (off, shape)
        ap = self.AR[:, off // 4:(off + nb) // 4]
        if dt != F32:
            ap = ap.bitcast(dt)
        if len(shape) == 2:
            ap = ap.rearrange("p (a b) -> p a b", b=shape[1])
        elif len(shape) == 3:
            ap = ap.rearrange("p (a b c) -> p a b c", b=shape[1], c=shape[2])
        return ap

    def ring_take(self):
        s = self.ring_i % NSLOT
        self.ring_i += 1
        return OFF_RING + s * SLOT, self.em.B("ring", s)

    def ring_load(self, src, shape):
        off, b = self.ring_take()
        view = self.v(off, shape, BF16)
        self.em.dma("pool", view, src, writes=[b])
        return view, b

    def psb(self, i):
        return self.PS[i], self.em.B("ps", i)

    def mm(self, out, lhsT, rhs, start, stop, reads, writes):
        nc = self.nc
        self.em.op("pe", lambda: nc.tensor.matmul(out=out, lhsT=lhsT, rhs=rhs, start=start, stop=stop),
                   reads, writes)

    def act(self, out, in_, func, reads, writes, bias=None, scale=None):
        nc = self.nc
        kw = {}
        if bias is not None:
            kw["bias"] = bias
        if scale is not None:
            kw["scale"] = scale
        self.em.op("act", lambda: nc.scalar.activation(out=out, in_=in_, func=func, **kw), reads, writes)

    def tt(self, e, out, in0, in1, op, reads, writes):
        eng = self.em.eng[e]
        self.em.op(e, lambda: eng.tensor_tensor(out=out, in0=in0, in1=in1, op=op), reads, writes)

    def ts(self, e, out, in0, s1, s2, op0, op1, reads, writes):
        eng = self.em.eng[e]
        self.em.op(e, lambda: eng.tensor_scalar(out=out, in0=in0, scalar1=s1, scalar2=s2, op0=op0, op1=op1),
                   reads, writes)

    def stt(self, out, in0, scalar, in1, op0, op1, reads, writes):
        nc = self.nc
        self.em.op("dve", lambda: nc.vector.scalar_tensor_tensor(out=out, in0=in0, scalar=scalar, in1=in1,
                                                                  op0=op0, op1=op1), reads, writes)

    def cp(self, e, out, in_, reads, writes):
        if e == "act":
            nc = self.nc
            self.em.op("act", lambda: nc.scalar.copy(out=out, in_=in_), reads, writes)
        else:
            eng = self.em.eng[e]
            self.em.op(e, lambda: eng.tensor_copy(out=out, in_=in_), reads, writes)

    def memset(self, e, ap, val, writes):
        eng = self.em.eng[e]
        self.em.op(e, lambda: eng.memset(ap, val), (), writes)

    def dump(self, name, ap_sb, shape, reads, dt=F32):
        if name not in self.dbg:
            return
        o = self.outp("dbg_" + name, shape, dt)
        self.em.dma("sp", o, ap_sb, reads=reads)

    def build(self):
        nc = self.nc
        L = self.nl
        inp = self.inp
        xin = inp("xin", [T, D])
        condT_d = inp("condT", [128, 16])
        ident_d = inp("ident", [128, 128])
        cossin_d = inp("cossin", [2, 128, T])
        rbias_d = inp("rbias", [128, NT * 16])
        wrouter_d = inp("wrouter", [128, 8, 16])
        w_ada_d = inp("w_ada_h", [DEPTH, 12, 128, 8, 512])
        b_ada_d = inp("b_ada_col", [DEPTH, 128, 48])
        wA_d = inp("wA", [DEPTH, 2, 128, 8, 512])
        wB_d = inp("wB", [DEPTH, 128, 8, 416])
        wC_d = inp("wC", [DEPTH, 2, 128, 8, 512])
        wG_d = inp("wG", [DEPTH, 6, 128, 8, 512])
        aln_d = inp("aln", [DEPTH, 2, 128, 512])
        wsT_d = inp("wsT", [DEPTH, 128, 4, 128])
        bs_d = inp("bs_row", [DEPTH, 1, 512])
        ncol_d = inp("ncol", [DEPTH, 128, 48])
        wuq_d = inp("wuq", [DEPTH, 128, 2, 768])
        wukv_d = inp("wukv", [DEPTH, 128, 1024])
        wri_d = inp("wri", [DEPTH, 128, 16, 128])
        wbr_d = inp("wbr", [DEPTH, 3, 128, 4, 1024])
        wo_d = inp("wo", [DEPTH, 2, 128, 8, 512])
        lnp_d = inp("lnp", [DEPTH, 4, 128, D])
        wg_d = inp("wg", [DEPTH, NEXP, 128, 8, 512])
        wu_d = inp("wu", [DEPTH, NEXP, 128, 8, 512])
        wd_d = inp("wd", [DEPTH, NEXP, 128, 4, 1024])
        out_d = self.outp("out", [NLAT, D])
        XD = nc.dram_tensor("xscr", [NT, 128, D], F32).ap()

        with ExitStack() as st:
            self.em = em = Em(nc, st)
            self.AR = st.enter_context(nc.sbuf_tensor("arena", [128, ARENA // 4], F32))
            self.PS = [st.enter_context(nc.psum_tensor("ps%d" % i, [128, 512], F32)) for i in range(8)]
            self.ring_i = 0
            B = em.B
            v = self.v
            PS = self.PS
            PB = [B("ps", i) for i in range(8)]
            for b_ in PB:
                b_.x = True

            ident = v(OFF_CONST + 0, [128])
            ident_bf = v(OFF_CONST + 512, [128], BF16)
            ones_bf = v(OFF_CONST + 768, [128], BF16)
            ones_f = v(OFF_CONST + 1024, [128])
            CM = [v(OFF_CONST + 1536, [48, 2]), v(OFF_CONST + 10752, [48, 2])]
            condT = v(OFF_CONST + 1920, [16])
            conds_bf = v(OFF_CONST + 1984, [8, 2], BF16)
            ncol = v(OFF_CONST + 2048, [48])
            ccol = v(OFF_CONST + 2240, [8])
            etmp = v(OFF_CONST + 2272, [8])
            wrouter = v(OFF_CONST + 2560, [8, 16])
            rbias = v(OFF_CONST + 3072, [NT * 16])
            bs_row = v(OFF_CONST + 4224, [512], BF16)
            alnG = v(OFF_CONST + 5248, [512])
            alnB = v(OFF_CONST + 7296, [512])
            stat = v(OFF_CONST + 9344, [4, 16])
            CWt = v(OFF_CONST + 9600, [NT, 16])
            GBv = [v(OFF_GB, [D]), v(OFF_GB + 4 * KB, [D])]
            HT = v(OFF_HT, [8, T], BF16)
            Xs = v(OFF_X, [NT, D])
            bConst = B("const")
            bHT = [B("HT", i) for i in range(NT)]
            bX = [B("X", i) for i in range(NT)]
            bXD = [B("XD", i) for i in range(NT)]
            bYT = [B("YT", i) for i in range(NT)]
            bGB = B("GB")
            bCM = [B("colmods", 0), B("colmods", 1)]
            bNcol = B("ncol")

            def tiles_of(c0, n):
                return list(range(c0 // 128, (c0 + n) // 128))

            def which(i):
                return 1 if i < 2 else 0

            em.dma("sp", ident, ident_d, writes=[bConst])
            em.dma("sp", condT, condT_d, writes=[bConst])
            em.dma("sp", wrouter, wrouter_d, writes=[bConst])
            em.dma("sp", rbias, rbias_d, writes=[bConst])
            for i in range(NT):
                em.dma("sp", Xs[:, i, :], xin[i * 128:(i + 1) * 128, :], writes=[bX[i]])
            self.memset("dve", ones_f, 1.0, [bConst])
            self.memset("dve", ones_bf, 1.0, [bConst])
            self.cp("dve", ident_bf, ident, [bConst], [bConst])
            sil = v(OFF_PH + 25 * KB, [16])
            self.act(sil, condT, AF.Silu, [bConst], [B("sil")])
            for w in range(2):
                self.cp("dve", conds_bf[:, :, w], sil[:, w * 8:(w + 1) * 8], [B("sil")], [bConst])

            psrr = [0]

            def next_ps(lo, hi):
                i = lo + psrr[0] % (hi - lo)
                psrr[0] += 1
                return i

            def s1_steps(l2):
                cm = CM[l2 % 2]
                bcm = bCM[l2 % 2]
                badac = v(OFF_PH + 27 * KB, [48])
                em.dma("sp", badac, b_ada_d[l2], writes=[B("badac")])
                def s1_load(s_):
                    W_ = v(OFF_PH + (s_ % 2) * SLOT, [8, 512], BF16)
                    bW_ = B("s1ring", s_ % 2)
                    em.dma("pool", W_, w_ada_d[l2, s_], writes=[bW_])
                    return W_, bW_
                q0 = []
                if l2 == 0:
                    for s_ in range(5):
                        q0.append(self.ring_load(w_ada_d[0, s_], [8, 512]))
                for s_ in range(12):
                    if l2 == 0:
                        if s_ + 5 < 12:
                            q0.append(self.ring_load(w_ada_d[0, s_ + 5], [8, 512]))
                        W, bW = q0.pop(0)
                    else:
                        if s_ % 4 == 0:
                            nxt = s1_load(s_)
                            yield
                        W, bW = nxt
                        if (s_ + 1) % 4 != 0:
                            nxt = s1_load(s_ + 1)
                    pm = next_ps(4, 8)
                    for cc in range(4):
                        for kc in range(8):
                            self.mm(PS[pm][:, 2 * cc:2 * cc + 2], W[:, kc, cc * 128:(cc + 1) * 128],
                                    conds_bf[:, kc, :], kc == 0, kc == 7, [bW, bConst], [PB[pm]])
                    psv = PS[pm][:, 0:8].rearrange("p (a b) -> p a b", b=2)
                    for w in range(2):
                        self.tt("dve", cm[:, 4 * s_:4 * s_ + 4, w], psv[:, :, w], badac[:, 4 * s_:4 * s_ + 4], ALU.add,
                                [PB[pm], B("badac")], [bcm])
                    yield
                for m in (1, 4):
                    self.ts("dve", cm[:, m * 8:(m + 1) * 8, :], cm[:, m * 8:(m + 1) * 8, :], 1.0, None,
                            ALU.add, ALU.bypass, [bcm], [bcm])
                yield

            def gate_bcast(colmods, bCol, m):
                diag = v(OFF_PH + 26 * KB, [2, 128])
                for w in range(2):
                    for hf in range(2):
                        pi = next_ps(6, 8)
                        for q in range(4):
                            cc = hf * 4 + q
                            dd = diag[:, (cc + w) % 2, :]
                            bd = B("diag", (cc + w) % 2)
                            self.ts("dve", dd, ident, colmods[:, m * 8 + cc, w:w + 1], None, ALU.mult, ALU.bypass,
                                    [bConst, bCol], [bd])
                            self.mm(PS[pi][:, q * 128:(q + 1) * 128], ones_f, dd, True, True,
                                    [bConst, bd], [PB[pi]])
                        self.cp("act", GBv[w][:, hf * 512:(hf + 1) * 512], PS[pi][:, :], [PB[pi]], [bGB])

            def layer(l):
                ctx_out = l < DEPTH - 1
                last = (l == L - 1)
                act_chunks = CHUNKS if ctx_out else CHUNKS[1:]
                act_tiles = list(range(NT)) if ctx_out else list(range(2, NT))

                colmods = CM[l % 2]
                bCol = bCM[l % 2]
                em.dma("sp", ncol, ncol_d[l], writes=[bNcol])
                if l == 0:
                    for _ in s1_steps(0):
                        pass
                s1gen = s1_steps(l + 1) if l + 1 < L else None
                self.dump("colmods%d" % l, colmods, [128, 48, 2], [bCol])
                gate_bcast(colmods, bCol, 2)

                def build_T(dst_is_ft):
                    mS, mB = (4, 3) if dst_is_ft else (1, 0)
                    for i in (act_tiles if dst_is_ft else range(NT)):
                        w = which(i)
                        for hf in range(2):
                            pi = next_ps(4, 6) if not dst_is_ft else next_ps(4, 6)
                            for q in range(4):
                                kc = hf * 4 + q
                                self.em.op("pe", lambda: nc.tensor.transpose(
                                    out=PS[pi][:, q * 128:(q + 1) * 128],
                                    in_=Xs[:, i, kc * 128:(kc + 1) * 128], identity=ident),
                                    [bX[i], bConst], [PB[pi]])
                            for q in range(4):
                                kc = hf * 4 + q
                                sc = colmods[:, mS * 8 + kc, w:w + 1]
                                bi = colmods[:, mB * 8 + kc, w:w + 1]
                                src = PS[pi][:, q * 128:(q + 1) * 128]
                                if dst_is_ft:
                                    f32t = self.F32T[:, (i % 2) * 8 + kc, :]
                                    bf = B("f32t", i % 2, kc)
                                    self.act(f32t, src, AF.Identity, [PB[pi], bCol], [bf], bias=bi, scale=sc)
                                    self.cp("dve", HT[:, kc, i * 128:(i + 1) * 128], f32t, [bf], [bHT[i]])
                                else:
                                    dst = HT[:, kc, i * 128:(i + 1) * 128]
                                    if hf == 0:
                                        self.act(dst, src, AF.Identity, [PB[pi], bCol], [bHT[i]], bias=bi, scale=sc)
                                    else:
                                        self.ts("dve", dst, src, sc, bi, ALU.mult, ALU.add, [PB[pi], bCol], [bHT[i]])
                        if dst_is_ft:
                            for kc in range(8):
                                self.mm(PS[6][:, i * 16:(i + 1) * 16], self.F32T[:, (i % 2) * 8 + kc, :],
                                        wrouter[:, kc, :], kc == 0, kc == 7,
                                        [B("f32t", i % 2, kc), bConst], [PB[6]])
                        if dst_is_ft:
                            self.em.op("act", lambda: nc.scalar.mul(out=Xs[:, i, :], in_=Xs[:, i, :], mul=ALPHA),
                                       [bX[i]], [bX[i]])
                        else:
                            self.ts("pool", Xs[:, i, :], Xs[:, i, :], ALPHA, 0.0, ALU.mult, ALU.add, [bX[i]], [bX[i]])
                        if not dst_is_ft:
                            em.dma("sp", XD[i], Xs[:, i, :], reads=[bX[i]], writes=[bXD[i]])

                def load_a():
                    AU = self.ring_load(wA_d[l, 0], [8, 512])
                    AV = self.ring_load(wA_d[l, 1], [8, 512])
                    WS = self.ring_load(wsT_d[l], [4, 128])
                    em.dma("pool", bs_row[0:1, :], bs_d[l], writes=[B("bsrow")])
                    em.dma("sp", alnG, aln_d[l, 0], writes=[B("aln")])
                    em.dma("sp", alnB, aln_d[l, 1], writes=[B("aln")])
                    return AU, AV, WS

                hold0 = load_a()
                gb0 = ([self.ring_load(wG_d[l, hf], [8, 512]) for hf in range(2)],
                       self.ring_load(wbr_d[l, 0], [4, D]))
                build_T(False)
                self.dump("HT%d" % l, HT, [128, 8, T], bHT, BF16)
                em.barrier()

                def load_merge(k):
                    G = [self.ring_load(wG_d[l, 2 * k + hf], [8, 512]) for hf in range(2)]
                    BR = self.ring_load(wbr_d[l, k], [4, D])
                    WO = [self.ring_load(wo_d[l, hf], [8, 512]) for hf in range(2)]
                    return G, BR, WO

                def merge(k, Wm, pre_barrier=None):
                    YT = self.YT
                    G, BR, WO = Wm
                    base = OFF_X + 18 * KB
                    Mb = [v(base + r * 8 * KB, [8, 512], BF16) for r in range(2)]
                    Xt = [v(base + 16 * KB + r * 4 * KB, [D]) for r in range(3)]
                    sgb = [v(base + 28 * KB + r * 2 * KB, [512]) for r in range(2)]
                    tmpb = [v(base + 32 * KB + r * 2 * KB, [512]) for r in range(2)]
                    cnt = 0
                    xcnt = 0
                    xl = [0]

                    def xload():
                        if xl[0] < len(act_tiles):
                            ii = act_tiles[xl[0]]
                            em.dma("sp", Xt[xl[0] % 3], XD[ii], reads=[bXD[ii]], writes=[B("xt", xl[0] % 3)])
                            xl[0] += 1
                    xload()
                    xload()
                    for ci, (c0, n) in enumerate(act_chunks):
                        tl = tiles_of(c0, n)
                        M = Mb[ci % 2]
                        bM = B("M", ci % 2)
                        rdh = [bHT[i] for i in tl]
                        rdy = [bYT[i] for i in tl]
                        for dc in range(8):
                            hf, q = dc // 4, dc % 4
                            pg = next_ps(0, 2)
                            pb = next_ps(2, 4)
                            for kc in range(8):
                                self.mm(PS[pg][:, 0:n], G[hf][0][:, kc, q * 128:(q + 1) * 128], HT[:, kc, c0:c0 + n],
                                        kc == 0, kc == 7, [G[hf][1]] + rdh, [PB[pg]])
                            for jj in range(4):
                                self.mm(PS[pb][:, 0:n], BR[0][:, jj, dc * 128:(dc + 1) * 128], YT[:, jj, c0:c0 + n],
                                        jj == 0, jj == 3, [BR[1]] + rdy, [PB[pb]])
                            sg = sgb[cnt % 2]
                            bsg = B("sg", cnt % 2)
                            cnt += 1
                            self.act(sg[:, 0:n], PS[pg][:, 0:n], AF.Sigmoid, [PB[pg]], [bsg])
                            self.tt("dve", M[:, dc, 0:n], sg[:, 0:n], PS[pb][:, 0:n], ALU.mult, [bsg, PB[pb]], [bM])
                        for t, i in enumerate(tl):
                            w = which(i)
                            xt = Xt[xcnt % 3]
                            bxt = B("xt", xcnt % 3)
                            xcnt += 1
                            for hf in range(2):
                                py = next_ps(4, 8)
                                for kc in range(8):
                                    self.mm(PS[py][:, :], M[:, kc, t * 128:(t + 1) * 128], WO[hf][0][:, kc, :],
                                            kc == 0, kc == 7, [bM, WO[hf][1]], [PB[py]])
                                tmp = tmpb[(cnt) % 2]
                                btmp = B("mtmp", cnt % 2)
                                cnt += 1
                                self.tt("dve", tmp, PS[py][:, :], GBv[w][:, hf * 512:(hf + 1) * 512], ALU.mult,
                                        [PB[py], bGB], [btmp])
                                self.tt("pool", xt[:, hf * 512:(hf + 1) * 512], xt[:, hf * 512:(hf + 1) * 512], tmp,
                                        ALU.add, [btmp, bxt], [bxt])
                            xload()
                            em.dma("sp", XD[i], xt, reads=[bxt], writes=[bXD[i]])
                        if s1gen is not None:
                            next(s1gen, None)
                    if s1gen is not None and k == 2:
                        for _ in s1gen:
                            pass
                    if pre_barrier is not None:
                        pre_barrier()
                    em.barrier()

                self.YT = v(OFF_X, [4, T], BF16)
                YT = self.YT
                IB = OFF_X + 18 * KB

                def mixer_a(Wa, pre_barrier=None):
                    AU, AV, WS = Wa
                    UTb = [v(IB + r * 4 * KB, [4, 512], BF16) for r in range(2)]
                    vgb = [v(IB + 8 * KB + r * 2 * KB, [512]) for r in range(4)]
                    vnb = [v(IB + 16 * KB + r * 2 * KB, [512]) for r in range(4)]
                    vbb = [v(IB + 24 * KB + r * KB, [512], BF16) for r in range(4)]
                    tcnt = 0
                    pendq = []

                    def spatial(pd):
                        i, t, r, UT, bUT = pd
                        vb, bvb = vbb[r], B("vb", r)
                        pm_ = next_ps(4, 8)
                        for g in range(4):
                            self.mm(PS[pm_][:, g * 128:(g + 1) * 128], vb[:, g * 128:(g + 1) * 128], WS[0][:, g, :],
                                    True, False, [bvb, WS[1]], [PB[pm_]])
                            self.mm(PS[pm_][:, g * 128:(g + 1) * 128], ones_bf[0:1, :], bs_row[0:1, g * 128:(g + 1) * 128],
                                    False, True, [bConst, B("bsrow")], [PB[pm_]])
                        self.tt("dve", YT[:, :, i * 128:(i + 1) * 128],
                                PS[pm_][:, :].rearrange("p (a b) -> p a b", b=128),
                                UT[:, :, t * 128:(t + 1) * 128], ALU.mult, [PB[pm_], bUT], [bYT[i]])

                    for ci, (c0, n) in enumerate(act_chunks):
                        tl = tiles_of(c0, n)
                        UT = UTb[ci % 2]
                        bUT = B("UT", ci % 2)
                        rdh = [bHT[i] for i in tl]
                        for g in range(4):
                            pi = next_ps(0, 2)
                            for kc in range(8):
                                self.mm(PS[pi][:, 0:n], AU[0][:, kc, g * 128:(g + 1) * 128], HT[:, kc, c0:c0 + n],
                                        kc == 0, kc == 7, [AU[1]] + rdh, [PB[pi]])
                            self.act(UT[:, g, 0:n], PS[pi][:, 0:n], AF.Gelu_apprx_tanh, [PB[pi]], [bUT])
                        for t, i in enumerate(tl):
                            r = tcnt % 4
                            tcnt += 1
                            pv = next_ps(2, 4)
                            for kc in range(8):
                                self.mm(PS[pv][:, :], HT[:, kc, i * 128:(i + 1) * 128], AV[0][:, kc, :],
                                        kc == 0, kc == 7, [AV[1], bHT[i]], [PB[pv]])
                            vg, vn, vb = vgb[r], vnb[r], vbb[r]
                            bvg, bvn, bvb, bst = B("vg", r), B("vn", r), B("vb", r), B("stA", r)
                            sa = stat[:, r, :]
                            self.act(vg, PS[pv][:, :], AF.Gelu_apprx_tanh, [PB[pv]], [bvg])
                            em.op("dve", lambda: nc.vector.bn_stats(out=sa[:, 0:6], in_=vg), [bvg], [bst])
                            em.op("dve", lambda: nc.vector.bn_aggr(out=sa[:, 6:8], in_=sa[:, 0:6]), [bst], [bst])
                            self.act(sa[:, 8:9], sa[:, 7:8], AF.Sqrt, [bst], [bst], bias=epsc, scale=1.0)
                            em.op("dve", lambda: nc.vector.reciprocal(out=sa[:, 9:10], in_=sa[:, 8:9]), [bst], [bst])
                            self.ts("dve", vn, vg, sa[:, 6:7], sa[:, 9:10], ALU.subtract, ALU.mult, [bvg, bst], [bvn])
                            self.tt("pool", vn, vn, alnG, ALU.mult, [bvn, B("aln")], [bvn])
                            self.tt("pool", vb, vn, alnB, ALU.add, [bvn, B("aln")], [bvb])
                            pendq.append((i, t, r, UT, bUT))
                            if len(pendq) > 2:
                                spatial(pendq.pop(0))
                    while pendq:
                        spatial(pendq.pop(0))
                    self.dump("ya%d" % l, YT, [128, 4, T], bYT, BF16)
                    if pre_barrier is not None:
                        pre_barrier()
                    em.barrier()

                def load_b():
                    WB = self.ring_load(wB_d[l], [8, 416])
                    offq, bWQ = self.ring_take()
                    WUQ = v(offq, [2, 768], BF16)
                    WUKV = v(offq + 6 * KB, [D], BF16)
                    em.dma("pool", WUQ, wuq_d[l], writes=[bWQ])
                    em.dma("pool", WUKV, wukv_d[l], writes=[bWQ])
                    return WB, offq, bWQ

                def mixer_b(Wb, pre_barrier=None):
                    WB, offq, bWQ = Wb
                    WUQ = v(offq, [2, 768], BF16)
                    WUQS = v(offq + 3 * KB, [2, 768], BF16)
                    WUKV = v(offq + 6 * KB, [D], BF16)
                    COS = v(IB, [T])
                    SIN = v(IB + 9 * KB, [T])
                    bCS = B("cossin")
                    em.dma("sp", COS, cossin_d[0], writes=[bCS])
                    em.dma("sp", SIN, cossin_d[1], writes=[bCS])
                    CQN = v(IB + 18 * KB, [2, T], BF16)
                    CKN = v(IB + 27 * KB, [T], BF16)
                    KROPE = v(IB + 31 * KB + 512, [T], BF16)
                    KRW = v(IB + 36 * KB, [8, 96], BF16)
                    KRS = v(IB + 37 * KB + 512, [8, 96], BF16)
                    cqf = v(IB + 39 * KB, [2, 512])
                    sq = v(IB + 43 * KB, [2, 512])
                    rstd = v(IB + 47 * KB, [512])
                    t1 = v(IB + 49 * KB, [512])
                    t2 = v(IB + 51 * KB, [512])
                    QTh = v(IB + 53 * KB, [T], BF16)
                    KTh = v(IB + 57 * KB + 512, [T], BF16)
                    VAb = [v(IB + 62 * KB + r * 2560, [NT, 65], BF16) for r in range(2)]
                    PTb = [v(IB + 67 * KB + r * KB, [512], BF16) for r in range(4)]
                    YBp = v(IB + 71 * KB, [NT, 128], BF16)
                    rden = v(IB + 76 * KB, [8])
                    bKR, bWQS = B("KRW"), B("WUQS")
                    self.memset("pool", KRW, 0.0, [bKR])
                    self.memset("pool", KRS, 0.0, [bKR])
                    self.memset("pool", WUQS, 0.0, [bWQS])
                    self.cp("pool", KRW[:, :, 64:96], WB[0][:, :, 384:416], [WB[1], bKR], [bKR])
                    for a in range(2):
                        o = 64 + a * 16
                        s_ = 384 + a * 16
                        self.ts("pool", KRS[:, :, o:o + 8], WB[0][:, :, s_ + 8:s_ + 16], -1.0, 0.0, ALU.mult, ALU.add,
                                [WB[1], bKR], [bKR])
                        self.cp("pool", KRS[:, :, o + 8:o + 16], WB[0][:, :, s_:s_ + 8], [WB[1], bKR], [bKR])
                    wq4 = WUQ.rearrange("p j (h e) -> p j h e", e=96)
                    ws4 = WUQS.rearrange("p j (h e) -> p j h e", e=96)
                    for j in range(2):
                        for a in range(2):
                            o = 64 + a * 16
                            self.ts("pool", ws4[:, j, :, o:o + 8], wq4[:, j, :, o + 8:o + 16], -1.0, 0.0,
                                    ALU.mult, ALU.add, [bWQ, bWQS], [bWQS])
                            self.cp("pool", ws4[:, j, :, o + 8:o + 16], wq4[:, j, :, o:o + 8], [bWQ, bWQS], [bWQS])
                    for r in range(2):
                        self.memset("pool", VAb[r][:, :, 64:65], 1.0, [B("VA", r)])
                    qg = ncol[:, 0:2]
                    kvg = ncol[:, 2:3]
                    bCQN = [B("CQN", c) for c in range(5)]
                    bCKN = [B("CKN", c) for c in range(5)]
                    bKRP = [B("KROPE", c) for c in range(5)]
                    for ci, (c0, n) in enumerate(CHUNKS):
                        tl = tiles_of(c0, n)
                        rdh = [bHT[i] for i in tl]
                        bcq, bsq, brs = B("cqf"), B("sq"), B("rstd")

                        def rms(nchunks, colbase, inv_n, gcol, dst_fn, bdst):
                            pss = []
                            for j in range(nchunks):
                                pi = next_ps(0, 4)
                                pss.append(pi)
                                for kc in range(8):
                                    self.mm(PS[pi][:, 0:n], WB[0][:, kc, colbase + j * 128:colbase + (j + 1) * 128],
                                            HT[:, kc, c0:c0 + n], kc == 0, kc == 7, [WB[1]] + rdh, [PB[pi]])
                                self.cp("act", cqf[:, j, 0:n], PS[pi][:, 0:n], [PB[pi]], [bcq])
                                self.act(sq[:, j, 0:n], PS[pi][:, 0:n], AF.Square, [PB[pi]], [bsq])
                            pq = next_ps(4, 6)
                            for j in range(nchunks):
                                self.mm(PS[pq][:, 0:n], ones_f, sq[:, j, 0:n], j == 0, j == nchunks - 1,
                                        [bConst, bsq], [PB[pq]])
                            self.act(rstd[:, 0:n], PS[pq][:, 0:n], AF.Sqrt, [PB[pq]], [brs], bias=epsc, scale=inv_n)
                            em.op("dve", lambda: nc.vector.reciprocal(out=rstd[:, 0:n], in_=rstd[:, 0:n]), [brs], [brs])
                            for j in range(nchunks):
                                self.stt(dst_fn(j), cqf[:, j, 0:n], gcol[:, j:j + 1], rstd[:, 0:n], ALU.mult, ALU.mult,
                                         [bcq, brs, bNcol], [bdst])

                        rms(2, 0, 1.0 / 256, qg, lambda j: CQN[:, j, c0:c0 + n], bCQN[ci])
                        rms(1, 256, 1.0 / 128, kvg, lambda j: CKN[:, c0:c0 + n], bCKN[ci])
                        pk = next_ps(0, 4)
                        pks = next_ps(0, 4)
                        for kc in range(8):
                            self.mm(PS[pk][0:96, 0:n], KRW[:, kc, :], HT[:, kc, c0:c0 + n], kc == 0, kc == 7,
                                    [bKR] + rdh, [PB[pk]])
                        for kc in range(8):
                            self.mm(PS[pks][0:96, 0:n], KRS[:, kc, :], HT[:, kc, c0:c0 + n], kc == 0, kc == 7,
                                    [bKR] + rdh, [PB[pks]])
                        bt1, bt2 = B("t1"), B("t2")
                        self.tt("dve", t1[64:96, 0:n], PS[pk][64:96, 0:n], COS[64:96, c0:c0 + n], ALU.mult,
                                [PB[pk], bCS], [bt1])
                        self.tt("dve", t2[64:96, 0:n], PS[pks][64:96, 0:n], SIN[64:96, c0:c0 + n], ALU.mult,
                                [PB[pks], bCS], [bt2])
                        self.tt("pool", KROPE[64:96, c0:c0 + n], t1[64:96, 0:n], t2[64:96, 0:n], ALU.add,
                                [bt1, bt2], [bKRP[ci]])
                    q_chunks = act_chunks
                    QT2 = v(IB + 39 * KB, [T], BF16)
                    KT2 = v(IB + 43 * KB + 512, [T], BF16)
                    alias_b = [B("cqf"), B("sq"), B("rstd")]
                    QTs = [QTh, QT2]
                    KTs = [KTh, KT2]
                    bQTs = [[B("QTh")], alias_b]
                    bKTs = [[B("KTh")], alias_b]
                    ptc = [0]

                    SBANKS = (0, 1, 6)
                    sbc = [0]

                    def prologue(h):
                        VA = VAb[h % 2]
                        bVA = B("VA", h % 2)
                        QT, KT = QTs[h % 2], KTs[h % 2]
                        bQT, bKT = bQTs[h % 2], bKTs[h % 2]
                        self.cp("pool", KT[64:96, :], KROPE[64:96, :], bKRP, bKT)
                        yield
                        for ci, (c0, n) in enumerate(CHUNKS):
                            pi = next_ps(7, 8)
                            self.mm(PS[pi][0:64, 0:n], WUKV[:, h * 128:h * 128 + 64], CKN[:, c0:c0 + n], True, True,
                                    [bWQ, bCKN[ci]], [PB[pi]])
                            self.cp("dve", KT[0:64, c0:c0 + n], PS[pi][0:64, 0:n], [PB[pi]], bKT)
                            yield
                        for g0 in range(0, NT, 8):
                            g1 = min(NT, g0 + 8)
                            pi = next_ps(7, 8)
                            for i in range(g0, g1):
                                self.mm(PS[pi][:, (i - g0) * 64:(i - g0 + 1) * 64], CKN[:, i * 128:(i + 1) * 128],
                                        WUKV[:, h * 128 + 64:h * 128 + 128], True, True,
                                        [bWQ, bCKN[min(4, (i + 2) // 4)]], [PB[pi]])
                            self.cp("dve", VA[:, g0:g1, 0:64],
                                    PS[pi][:, 0:(g1 - g0) * 64].rearrange("p (a b) -> p a b", b=64), [PB[pi]], [bVA])
                            yield
                        for ci, (c0, n) in enumerate(q_chunks):
                            cidx = CHUNKS.index((c0, n))
                            pq_ = 7
                            pqs = 7
                            for j in range(2):
                                self.mm(PS[pq_][0:96, 0:n], WUQ[:, j, h * 96:(h + 1) * 96], CQN[:, j, c0:c0 + n],
                                        j == 0, j == 1, [bWQ, bCQN[cidx]], [PB[pq_]])
                            bt1, bt2 = B("t1"), B("t2")
                            self.cp("dve", QT[0:64, c0:c0 + n], PS[pq_][0:64, 0:n], [PB[pq_]], bQT)
                            self.tt("dve", t1[64:96, 0:n], PS[pq_][64:96, 0:n], COS[64:96, c0:c0 + n], ALU.mult,
                                    [PB[pq_], bCS], [bt1])
                            yield
                            for j in range(2):
                                self.mm(PS[pqs][0:96, 0:n], WUQS[:, j, h * 96:(h + 1) * 96], CQN[:, j, c0:c0 + n],
                                        j == 0, j == 1, [bWQS, bWQ, bCQN[cidx]], [PB[pqs]])
                            self.tt("dve", t2[64:96, 0:n], PS[pqs][64:96, 0:n], SIN[64:96, c0:c0 + n], ALU.mult,
                                    [PB[pqs], bCS], [bt2])
                            self.tt("pool", QT[64:96, c0:c0 + n], t1[64:96, 0:n], t2[64:96, 0:n], ALU.add,
                                    [bt1, bt2], bQT)
                            yield

                    pgen = [None]
                    pit = [0]

                    def attend(h, ci, c0, n):
                        hsub = h % 2
                        VA = VAb[h % 2]
                        bVA = B("VA", h % 2)
                        QT, KT = QTs[h % 2], KTs[h % 2]
                        bQT, bKT = bQTs[h % 2], bKTs[h % 2]
                        keys = list(range(NT)) if c0 >= NCTX else [0, 1]
                        nq = n // 128
                        pend = []

                        def pv(pd):
                            ki, i, PT, bPT = pd
                            for qs in range(nq):
                                self.mm(PS[2 + qs][:, 0:65], PT[:, qs * 128:(qs + 1) * 128], VA[:, i, 0:65],
                                        ki == 0, ki == len(keys) - 1, [bPT, bVA], [PB[2 + qs]])
                        for ki, i in enumerate(keys):
                            ps_ = SBANKS[sbc[0] % 3]
                            sbc[0] += 1
                            self.mm(PS[ps_][:, 0:n], KT[0:96, i * 128:(i + 1) * 128], QT[0:96, c0:c0 + n],
                                    True, True, bKT + bQT, [PB[ps_]])
                            if len(pend) >= 2:
                                pv(pend.pop(0))
                            PT = PTb[ptc[0] % 4]
                            bPT = B("PT", ptc[0] % 4)
                            ptc[0] += 1
                            self.act(PT[:, 0:n], PS[ps_][:, 0:n], AF.Exp, [PB[ps_]], [bPT], scale=ATT_SCALE)
                            pend.append((ki, i, PT, bPT))
                            pit[0] += 1
                            if pgen[0] is not None and pit[0] % 3 == 0:
                                next(pgen[0], None)
                        while pend:
                            pv(pend.pop(0))
                        for qs in range(nq):
                            iq = c0 // 128 + qs
                            rslot = (qs % 4) + 4 * (ci % 2)
                            rd = rden[:, rslot:rslot + 1]
                            brd = B("rden", rslot)
                            em.op("dve", lambda: nc.vector.reciprocal(out=rd, in_=PS[2 + qs][:, 64:65]),
                                  [PB[2 + qs]], [brd])
                            self.ts("dve", YBp[:, iq, hsub * 64:(hsub + 1) * 64], PS[2 + qs][:, 0:64], rd, None,
                                    ALU.mult, ALU.bypass, [PB[2 + qs], brd], [B("YBp", iq)])

                    for _ in prologue(0):
                        pass
                    for h in range(8):
                        jpair, hsub = h // 2, h % 2
                        pgen[0] = prologue(h + 1) if h + 1 < 8 else None
                        for ci, (c0, n) in enumerate(q_chunks):
                            attend(h, ci, c0, n)
                        if pgen[0] is not None:
                            for _ in pgen[0]:
                                pass
                        if hsub == 1:
                            for g0 in range(0, NT, 4):
                                pi = next_ps(7, 8)
                                psb16 = PS[pi][:, 0:256].bitcast(BF16)
                                tls = [i for i in range(g0, min(NT, g0 + 4)) if i in act_tiles]
                                for i in tls:
                                    em.op("pe", lambda: nc.tensor.transpose(
                                        out=psb16[:, (i - g0) * 128:(i - g0 + 1) * 128], in_=YBp[:, i, :],
                                        identity=ident_bf), [B("YBp", i), bConst], [PB[pi]])
                                for i in tls:
                                    self.cp("act", YT[:, jpair, i * 128:(i + 1) * 128],
                                            psb16[:, (i - g0) * 128:(i - g0 + 1) * 128], [PB[pi]], [bYT[i]])
                    self.dump("yb%d" % l, YT, [128, 4, T], bYT, BF16)
                    if pre_barrier is not None:
                        pre_barrier()
                    em.barrier()

                def load_c():
                    CX = self.ring_load(wC_d[l, 0], [8, 512])
                    CG = self.ring_load(wC_d[l, 1], [8, 512])
                    WRI = self.ring_load(wri_d[l], [16, 128])
                    return CX, CG, WRI

                def mixer_c(Wc, pre_barrier=None):
                    CX, CG, WRI = Wc
                    XR = v(IB, [T])
                    XC = v(IB + 9 * KB, [T])
                    XCB = v(IB + 18 * KB, [T], BF16)
                    Ad = [v(IB + 23 * KB, [T]), v(IB + 41 * KB, [T])]
                    Bd = [v(IB + 32 * KB, [T]), v(IB + 50 * KB, [T])]
                    Hf = v(IB + 59 * KB, [T])
                    Hb = v(IB + 68 * KB, [T])
                    tb = [v(IB + 77 * KB + r * 2 * KB, [512]) for r in range(2)]
                    lam = ncol[:, 40:48]
                    bcc = B("ccol")
                    self.act(etmp, lam, AF.Exp, [bNcol], [bcc], scale=-1.0)
                    self.act(etmp, etmp, AF.Ln, [bcc], [bcc], bias=onec, scale=1.0)
                    self.ts("dve", ccol, etmp, -8.0, None, ALU.mult, ALU.bypass, [bcc], [bcc])
                    convw = ncol[:, 4:20].rearrange("p (j k) -> p j k", k=4)
                    convb = ncol[:, 20:24]
                    bri = ncol[:, 24:40]
                    bXR, bXC, bXCB = B("XR"), B("XC"), B("XCB")
                    bAd = [B("Ad", 0), B("Ad", 1)]
                    bBd = [B("Bd", 0), B("Bd", 1)]
                    bHf, bHb = B("Hf"), B("Hb")
                    tmps = [Hf, Hb]
                    btmps = [bHf, bHb]
                    tcn = [0]

                    def st1a(j):
                        for ci, (c0, n) in enumerate(CHUNKS):
                            tl = tiles_of(c0, n)
                            pi = next_ps(0, 4)
                            for kc in range(8):
                                self.mm(PS[pi][:, 0:n], CX[0][:, kc, j * 128:(j + 1) * 128], HT[:, kc, c0:c0 + n],
                                        kc == 0, kc == 7, [CX[1]] + [bHT[i] for i in tl], [PB[pi]])
                            self.cp("dve", XR[:, c0:c0 + n], PS[pi][:, 0:n], [PB[pi]], [bXR])
                        for (s_, e_) in ((0, NCTX), (NCTX, T)):
                            self.ts("dve", XC[:, s_:e_], XR[:, s_:e_], convw[:, j, 2:3], convb[:, j:j + 1], ALU.mult, ALU.add,
                                    [bXR, bNcol], [bXC])
                            self.stt(XC[:, s_ + 1:e_], XR[:, s_:e_ - 1], convw[:, j, 1:2], XC[:, s_ + 1:e_], ALU.mult, ALU.add,
                                     [bXR, bXC, bNcol], [bXC])
                            self.stt(XC[:, s_ + 2:e_], XR[:, s_:e_ - 2], convw[:, j, 0:1], XC[:, s_ + 2:e_], ALU.mult, ALU.add,
                                     [bXR, bXC, bNcol], [bXC])
                            self.stt(XC[:, s_:e_ - 1], XR[:, s_ + 1:e_], convw[:, j, 3:4], XC[:, s_:e_ - 1], ALU.mult, ALU.add,
                                     [bXR, bXC, bNcol], [bXC])
                        self.cp("pool", XCB, XC, [bXC], [bXCB])

                    def st1b(j):
                        for d in range(2):
                            ir = (d * 2 + 0) * 4 + j
                            ii = (d * 2 + 1) * 4 + j
                            for ci, (c0, n) in enumerate(CHUNKS):
                                pr = next_ps(4, 8)
                                pi_ = next_ps(4, 8)
                                self.mm(PS[pr][:, 0:n], WRI[0][:, ir, :], XCB[:, c0:c0 + n], True, True,
                                        [WRI[1], bXCB], [PB[pr]])
                                self.mm(PS[pi_][:, 0:n], WRI[0][:, ii, :], XCB[:, c0:c0 + n], True, True,
                                        [WRI[1], bXCB], [PB[pi_]])
                                self.act(Ad[d][:, c0:c0 + n], PS[pr][:, 0:n], AF.Sigmoid, [PB[pr], bNcol], [bAd[d]],
                                         bias=bri[:, ir:ir + 1], scale=1.0)
                                self.act(Bd[d][:, c0:c0 + n], PS[pi_][:, 0:n], AF.Sigmoid, [PB[pi_], bNcol], [bBd[d]],
                                         bias=bri[:, ii:ii + 1], scale=1.0)
                        for d in range(2):
                            self.tt("pool", Bd[d], Bd[d], XC, ALU.mult, [bBd[d], bXC], [bBd[d]])

                    def st2(j, mid=None):
                        for d in range(2):
                            self.act(Ad[d], Ad[d], AF.Exp, [bAd[d], bcc], [bAd[d]],
                                     scale=ccol[:, d * 4 + j:d * 4 + j + 1])
                        for d in range(2):
                            self.tt("dve", tmps[d], Ad[d], Ad[d], ALU.mult, [bAd[d]], [btmps[d]])
                        if mid is not None:
                            mid()
                        for d in range(2):
                            self.act(tmps[d], tmps[d], AF.Sqrt, [btmps[d]], [btmps[d]], bias=onec, scale=-1.0)
                        for d in range(2):
                            self.tt("pool", Bd[d], Bd[d], tmps[d], ALU.mult, [bBd[d], btmps[d]], [bBd[d]])
                        em.op("dve", lambda: nc.vector.tensor_tensor_scan(
                            out=Hf[:, 0:T], data0=Ad[0][:, 0:T], data1=Bd[0][:, 0:T], initial=0.0,
                            op0=ALU.mult, op1=ALU.add), [bAd[0], bBd[0]], [bHf])
                        em.op("dve", lambda: nc.vector.tensor_tensor_scan(
                            out=Hb[:, 0:NCTX][:, ::-1], data0=Ad[1][:, 0:NCTX][:, ::-1],
                            data1=Bd[1][:, 0:NCTX][:, ::-1], initial=0.0,
                            op0=ALU.mult, op1=ALU.add), [bAd[1], bBd[1]], [bHb])
                        em.op("dve", lambda: nc.vector.tensor_tensor_scan(
                            out=Hb[:, NCTX:T][:, ::-1], data0=Ad[1][:, NCTX:T][:, ::-1],
                            data1=Bd[1][:, NCTX:T][:, ::-1], initial=Hb[:, 0:1],
                            op0=ALU.mult, op1=ALU.add), [bAd[1], bBd[1], bHb], [bHb])
                        self.tt("pool", Hf, Hf, Hb, ALU.add, [bHf, bHb], [bHf])
                        for ci, (c0, n) in enumerate(act_chunks):
                            tl = tiles_of(c0, n)
                            pi = next_ps(0, 4)
                            for kc in range(8):
                                self.mm(PS[pi][:, 0:n], CG[0][:, kc, j * 128:(j + 1) * 128], HT[:, kc, c0:c0 + n],
                                        kc == 0, kc == 7, [CG[1]] + [bHT[i] for i in tl], [PB[pi]])
                            tg = tb[tcn[0] % 2]
                            btg = B("tb", tcn[0] % 2)
                            tcn[0] += 1
                            self.act(tg[:, 0:n], PS[pi][:, 0:n], AF.Gelu_apprx_tanh, [PB[pi]], [btg])
                            self.tt("dve", YT[:, j, c0:c0 + n], tg[:, 0:n], Hf[:, c0:c0 + n], ALU.mult, [btg, bHf],
                                    [bYT[i] for i in tl])

                    st1a(0)
                    st1b(0)
                    for j in range(4):
                        st2(j, (lambda jj=j: st1a(jj + 1)) if j + 1 < 4 else None)
                        if j + 1 < 4:
                            st1b(j + 1)
                    self.dump("yc%d" % l, YT, [128, 4, T], bYT, BF16)
                    if pre_barrier is not None:
                        pre_barrier()
                    em.barrier()

                def load_expert(e_):
                    return (self.ring_load(wg_d[l, e_], [8, 512]), self.ring_load(wu_d[l, e_], [8, 512]),
                            self.ring_load(wd_d[l, e_], [4, D]))
                stop = getattr(self, "stop", None)
                hold = {"A": hold0}

                def pf(name, fn):
                    def go():
                        hold[name] = fn()
                    return go
                mixer_a(hold.pop("A"), pf("m0", lambda: (gb0[0], gb0[1],
                                                         [self.ring_load(wo_d[l, hf], [8, 512]) for hf in range(2)])))
                if stop == "A":
                    merge(0, hold.pop("m0"))
                else:
                    merge(0, hold.pop("m0"), pf("B", load_b))
                    mixer_b(hold.pop("B"), pf("m1", lambda: load_merge(1)))
                    if stop == "B":
                        merge(1, hold.pop("m1"))
                    else:
                        merge(1, hold.pop("m1"), pf("C", load_c))
                        mixer_c(hold.pop("C"), pf("m2", lambda: load_merge(2)))
                        merge(2, hold.pop("m2"), pf("E0", lambda: load_expert(0)) if stop is None else None)

                def layer_norm(pidx, tiles):
                    Lg = v(OFF_PH, [D])
                    Lb = v(OFF_PH + 4 * KB, [D])
                    bL = B("lnp")
                    em.dma("sp", Lg, lnp_d[l, pidx], writes=[bL])
                    em.dma("sp", Lb, lnp_d[l, pidx + 1], writes=[bL])
                    for n_, i in enumerate(tiles):
                        r = n_ % 4
                        sa = stat[:, r, :]
                        bst = B("stL", r)
                        for hf in range(2):
                            em.op("dve", lambda: nc.vector.bn_stats(out=sa[:, hf * 6:(hf + 1) * 6],
                                                                    in_=Xs[:, i, hf * 512:(hf + 1) * 512]),
                                  [bX[i]], [bst])
                        em.op("dve", lambda: nc.vector.bn_aggr(out=sa[:, 12:14], in_=sa[:, 0:12]), [bst], [bst])
                        self.act(sa[:, 14:15], sa[:, 13:14], AF.Sqrt, [bst], [bst], bias=epsc, scale=1.0)
                        em.op("dve", lambda: nc.vector.reciprocal(out=sa[:, 15:16], in_=sa[:, 14:15]), [bst], [bst])
                        self.ts("dve", sa[:, 14:15], sa[:, 12:13], sa[:, 15:16], -1.0, ALU.mult, ALU.mult, [bst], [bst])
                        self.act(Xs[:, i, :], Xs[:, i, :], AF.Identity, [bX[i], bst], [bX[i]],
                                 bias=sa[:, 14:15], scale=sa[:, 15:16])
                        self.tt("dve", Xs[:, i, :], Xs[:, i, :], Lg, ALU.mult, [bX[i], bL], [bX[i]])
                        self.tt("pool", Xs[:, i, :], Xs[:, i, :], Lb, ALU.add, [bX[i], bL], [bX[i]])

                for i in act_tiles:
                    em.dma("sp", Xs[:, i, :], XD[i], reads=[bXD[i]], writes=[bX[i]])
                layer_norm(0, act_tiles)
                self.dump("x1_%d" % l, Xs, [128, NT, D], bX)
                if stop in ("A", "B", "C"):
                    return act_tiles

                self.F32T = v(OFF_PH + 8 * KB, [16, 128])
                gate_bcast(colmods, bCol, 5)
                build_T(True)
                RB = OFF_PH + 16 * KB
                SC = v(RB, [NT * 16])
                SEL = v(RB + 1152, [NT * 16])
                PR = v(RB + 2304, [6, NT * 4])
                GS = v(RB + 4096, [NT * 4])
                GM = v(RB + 4416, [NT])
                OG = v(RB + 4512, [NT * 4])
                PEN = v(RB + 4800, [NT * 4])
                SM = v(RB + 5120, [NT * 16])
                CNT = v(RB + 6272, [NT * 16])
                CMP = v(RB + 7424, [NT * 4])
                WM = v(RB + 7712, [NT * 16])
                DEN = v(RB + 8864, [NT])
                bR = B("route")
                bCW = B("CW")
                self.act(SC, PS[6][:, 0:NT * 16], AF.Sigmoid, [PB[6]], [bR])
                self.tt("dve", SEL, SC, rbias, ALU.add, [bR, bConst], [bR])
                sel4 = SEL.rearrange("p (a b) -> p a b", b=4)
                pairs = [(0, 1), (0, 2), (0, 3), (1, 2), (1, 3), (2, 3)]
                for pi_, (a, b_) in enumerate(pairs):
                    self.tt("dve", PR[:, pi_, :], sel4[:, :, a], sel4[:, :, b_], ALU.add, [bR], [bR])
                self.tt("dve", GS, PR[:, 0, :], PR[:, 1, :], ALU.max, [bR], [bR])
                for pi_ in range(2, 6):
                    self.tt("dve", GS, GS, PR[:, pi_, :], ALU.max, [bR], [bR])
                gs3 = GS.rearrange("p (a b) -> p a b", b=4)
                self.tt("dve", GM, gs3[:, :, 0], gs3[:, :, 1], ALU.max, [bR], [bR])
                for g in range(2, 4):
                    self.tt("dve", GM, GM, gs3[:, :, g], ALU.max, [bR], [bR])
                og3 = OG.rearrange("p (a b) -> p a b", b=4)
                for g in range(4):
                    self.tt("dve", og3[:, :, g], gs3[:, :, g], GM, ALU.is_ge, [bR], [bR])
                self.ts("dve", PEN, OG, BIG, -BIG, ALU.mult, ALU.add, [bR], [bR])
                sm4 = SM.rearrange("p (a b) -> p a b", b=4)
                cnt4 = CNT.rearrange("p (a b) -> p a b", b=4)
                for e_ in range(4):
                    self.tt("dve", sm4[:, :, e_], sel4[:, :, e_], PEN, ALU.add, [bR], [bR])
                for e_ in range(4):
                    first = True
                    for e2 in range(4):
                        if e2 == e_:
                            continue
                        if first:
                            self.tt("dve", cnt4[:, :, e_], sm4[:, :, e2], sm4[:, :, e_], ALU.is_gt, [bR], [bR])
                            first = False
                        else:
                            self.tt("dve", CMP, sm4[:, :, e2], sm4[:, :, e_], ALU.is_gt, [bR], [bR])
                            self.tt("dve", cnt4[:, :, e_], cnt4[:, :, e_], CMP, ALU.add, [bR], [bR])
                wm4 = WM.rearrange("p (a b) -> p a b", b=4)
                sc4 = SC.rearrange("p (a b) -> p a b", b=4)
                for e_ in range(4):
                    self.ts("dve", CMP, cnt4[:, :, e_], 1.5, None, ALU.is_lt, ALU.bypass, [bR], [bR])
                    self.tt("dve", CMP, CMP, OG, ALU.mult, [bR], [bR])
                    self.tt("dve", wm4[:, :, e_], sc4[:, :, e_], CMP, ALU.mult, [bR], [bR])
                wm16 = WM.rearrange("p (a b) -> p a b", b=16)
                em.op("dve", lambda: nc.vector.tensor_reduce(out=DEN, in_=wm16, axis=AX.X, op=ALU.add), [bR], [bR])
                em.op("dve", lambda: nc.vector.reciprocal(out=DEN, in_=DEN), [bR], [bR])
                for e_ in range(16):
                    self.stt(CWt[:, :, e_], wm16[:, :, e_], 2.5, DEN, ALU.mult, ALU.mult, [bR], [bCW])
                self.dump("cw%d" % l, CWt, [128, NT, 16], [bCW])

                MB = OFF_PH + 8 * KB
                ATb = [v(MB + r * 4 * KB, [4, 512], BF16) for r in range(2)]
                bATl = [[B("f32t", r, kc) for kc in range(8)] for r in range(2)]
                sgm = [v(RB + r * 2 * KB, [512]) for r in range(2)]
                tmm = [v(RB + 4 * KB + r * 2 * KB, [512]) for r in range(2)]
                claimed = set()

                def claim(key):
                    if key in claimed:
                        return []
                    claimed.add(key)
                    return [bR]

                cnt = 0
                Wn = hold.pop("E0")
                for e_ in range(NEXP):
                    WG, WU, WD = Wn
                    if e_ + 1 < NEXP:
                        Wn = load_expert(e_ + 1)
                    for ci, (c0, n) in enumerate(act_chunks):
                        tl = tiles_of(c0, n)
                        rdh = [bHT[i] for i in tl]
                        AT = ATb[(e_ * 5 + ci) % 2]
                        bATs = bATl[(e_ * 5 + ci) % 2]
                        for j in range(4):
                            pg = next_ps(0, 2)
                            pu = next_ps(2, 4)
                            for kc in range(8):
                                self.mm(PS[pg][:, 0:n], WG[0][:, kc, j * 128:(j + 1) * 128], HT[:, kc, c0:c0 + n],
                                        kc == 0, kc == 7, [WG[1]] + rdh, [PB[pg]])
                            for kc in range(8):
                                self.mm(PS[pu][:, 0:n], WU[0][:, kc, j * 128:(j + 1) * 128], HT[:, kc, c0:c0 + n],
                                        kc == 0, kc == 7, [WU[1]] + rdh, [PB[pu]])
                            sg = sgm[cnt % 2]
                            bsg = B("sgm", cnt % 2)
                            ck = claim(("sgm", cnt % 2))
                            cnt += 1
                            self.act(sg[:, 0:n], PS[pg][:, 0:n], AF.Silu, [PB[pg]], [bsg] + ck)
                            self.tt("dve", AT[:, j, 0:n], sg[:, 0:n], PS[pu][:, 0:n], ALU.mult, [bsg, PB[pu]], bATs)
                        for t, i in enumerate(tl):
                            w = which(i)
                            for hf in range(2):
                                py = next_ps(4, 8)
                                for j in range(4):
                                    self.mm(PS[py][:, :], AT[:, j, t * 128:(t + 1) * 128],
                                            WD[0][:, j, hf * 512:(hf + 1) * 512], j == 0, j == 3,
                                            bATs + [WD[1]], [PB[py]])
                                tmp = tmm[cnt % 2]
                                btmp = B("tmm", cnt % 2)
                                ck = claim(("tmm", cnt % 2))
                                cnt += 1
                                self.stt(tmp, PS[py][:, :], CWt[:, i, e_:e_ + 1], GBv[w][:, hf * 512:(hf + 1) * 512],
                                         ALU.mult, ALU.mult, [PB[py], bCW, bGB], [btmp] + ck)
                                self.tt("pool", Xs[:, i, hf * 512:(hf + 1) * 512], Xs[:, i, hf * 512:(hf + 1) * 512],
                                        tmp, ALU.add, [btmp, bX[i]], [bX[i]])
                layer_norm(2, act_tiles)
                self.dump("x2_%d" % l, Xs, [128, NT, D], bX)
                return act_tiles

            epsc = v(OFF_CONST + 2304, [1])
            onec = v(OFF_CONST + 2308, [1])
            self.memset("dve", epsc, EPS, [bConst])
            self.memset("dve", onec, 1.0, [bConst])

            for l in range(L):
                layer(l)

            for i in range(2, NT):
                em.dma("sp", out_d[(i - 2) * 128:(i - 1) * 128, :], Xs[:, i, :], reads=[bX[i]])
            em.finish("sp")
            self.stats = dict(ninst=dict(em.ninst), nwait=em.nwait, nsem=em.nsem)
        return nc


def _rope_tables():
    rows = NLAT // 64
    row = np.repeat(np.arange(rows, dtype=np.float32), 64)
    col = np.tile(np.arange(64, dtype=np.float32), rows)
    n_freq = 8
    inv = (np.float32(10000.0) ** (-np.arange(n_freq, dtype=np.float32) / np.float32(n_freq))).astype(np.float32)
    ang = np.stack([row[:, None] * inv, col[:, None] * inv], axis=1).astype(np.float32)
    cos = np.cos(ang).astype(np.float32)
    sin = np.sin(ang).astype(np.float32)
    tab = np.zeros((2, 128, T), np.float32)
    tab[0] = 1.0
    for a in range(2):
        for hf in range(2):
            r0 = 64 + a * 16 + hf * 8
            tab[0, r0:r0 + 8, NCTX:] = cos[:, a, :].T
            tab[1, r0:r0 + 8, NCTX:] = sin[:, a, :].T
    return tab


def _kc(w):
    *lead, K, N = w.shape
    w = w.reshape(*lead, K // 128, 128, N)
    nd = w.ndim
    perm = list(range(nd - 3)) + [nd - 2, nd - 3, nd - 1]
    return np.ascontiguousarray(w.transpose(perm))


def _col(vv):
    *lead, n = vv.shape
    return np.ascontiguousarray(np.swapaxes(vv.reshape(*lead, n // 128, 128), -1, -2))


def prep_shared(inp):
    f = lambda a: np.ascontiguousarray(np.asarray(a, dtype=np.float32))
    sh = {}
    sh["ident"] = np.eye(128, dtype=np.float32)
    sh["cossin"] = _rope_tables()
    sh["rbias"] = np.ascontiguousarray(np.broadcast_to(np.tile(f(inp["router_bias"]), NT)[None, :], (128, NT * 16)))
    sh["wrouter"] = _kc(f(inp["w_router"]))
    w_ada = f(inp["w_ada"])
    wa = _kc(w_ada)
    sh["w_ada_h"] = np.ascontiguousarray(wa.reshape(DEPTH, 128, 8, 12, 512).transpose(0, 3, 1, 2, 4))
    sh["b_ada_col"] = _col(f(inp["b_ada"]))
    w_in = _kc(f(inp["w_in"]))
    sh["wA"] = np.ascontiguousarray(np.stack([w_in[..., 0:512], w_in[..., 512:1024]], axis=1))
    sh["wB"] = np.ascontiguousarray(w_in[..., 1024:1440])
    sh["wC"] = np.ascontiguousarray(np.stack([w_in[..., 1440:1952], w_in[..., 1952:2464]], axis=1))
    sh["wG"] = np.ascontiguousarray(np.stack([w_in[..., 2464 + s * 512:2464 + (s + 1) * 512] for s in range(6)], axis=1))
    aln = np.stack([f(inp["a_ln_g"]), f(inp["a_ln_b"])], axis=1)
    sh["aln"] = np.ascontiguousarray(np.broadcast_to(aln[:, :, None, :], (DEPTH, 2, 128, 512)))
    sh["wsT"] = np.ascontiguousarray(f(inp["w_s"]).transpose(0, 3, 1, 2))
    sh["bs_row"] = np.ascontiguousarray(f(inp["b_s"]).reshape(DEPTH, 1, 512))
    ncol = np.zeros((DEPTH, 128, 48), np.float32)
    ncol[:, :, 0:2] = _col(f(inp["q_norm_g"]))
    ncol[:, :, 2:3] = _col(f(inp["kv_norm_g"]))
    cw = f(inp["conv_w"])
    ncol[:, :, 4:20] = np.ascontiguousarray(cw.reshape(DEPTH, 4, 4, 128).transpose(0, 3, 2, 1)).reshape(DEPTH, 128, 16)
    ncol[:, :, 20:24] = _col(f(inp["conv_b"]))
    br = _col(f(inp["b_r"]))
    bi = _col(f(inp["b_i"]))
    for d in range(2):
        ncol[:, :, 24 + (d * 2 + 0) * 4:24 + (d * 2 + 0) * 4 + 4] = br[:, d]
        ncol[:, :, 24 + (d * 2 + 1) * 4:24 + (d * 2 + 1) * 4 + 4] = bi[:, d]
    lam = _col(f(inp["lru_lambda"]))
    for d in range(2):
        ncol[:, :, 40 + d * 4:44 + d * 4] = lam[:, d]
    sh["ncol"] = ncol
    sh["wuq"] = _kc(f(inp["w_uq"]))
    sh["wukv"] = f(inp["w_ukv"])
    wri = np.zeros((DEPTH, 128, 16, 128), np.float32)
    for d in range(2):
        for ri, name in enumerate(("w_r", "w_i")):
            w = f(inp[name])[:, d]
            for j in range(4):
                for bb in range(2):
                    wri[:, bb * 64:(bb + 1) * 64, (d * 2 + ri) * 4 + j, bb * 64:(bb + 1) * 64] = w[:, 2 * j + bb]
    sh["wri"] = wri
    sh["wbr"] = _kc(f(inp["w_br"]))
    wo = _kc(f(inp["w_o"]))
    sh["wo"] = np.ascontiguousarray(np.stack([wo[..., 0:512], wo[..., 512:1024]], axis=1))
    lnp = np.stack([f(inp["ln1_g"]), f(inp["ln1_b"]), f(inp["ln2_g"]), f(inp["ln2_b"])], axis=1)
    sh["lnp"] = np.ascontiguousarray(np.broadcast_to(lnp[:, :, None, :], (DEPTH, 4, 128, D)))
    sh["wg"] = _kc(f(inp["w_gate"]))
    sh["wu"] = _kc(f(inp["w_up"]))
    sh["wd"] = _kc(f(inp["w_down"]))
    return sh


def prep_core(inp, b):
    f = lambda a: np.asarray(a, dtype=np.float32)
    xin = np.ascontiguousarray(np.concatenate([f(inp["ctx"])[b], f(inp["x"])[b]], axis=0))
    condT = np.concatenate([_col(f(inp["c"])[b]), _col(f(inp["c_ctx"]))], axis=1)
    return {"xin": xin, "condT": np.ascontiguousarray(condT)}


_CACHE = {}


def kernel(**inputs):
    nb = inputs["x"].shape[0]
    if "prog" not in _CACHE:
        p = Prog()
        p.build()
        _CACHE["prog"] = p
    p = _CACHE["prog"]
    sh = prep_shared(inputs)
    in_maps = []
    for b in range(nb):
        m = dict(sh)
        m.update(prep_core(inputs, b))
        in_maps.append(m)
    res = run_bass_kernel_spmd(p.nc, in_maps, core_ids=list(range(nb)))
    out = np.stack([np.asarray(r["out"], dtype=np.float32) for r in res.results], axis=0)
    return out
```
